# Optimizing a Trainium2 kernel written in Bass

```python
import jax
import jax.numpy as jnp
from jax import lax
import numpy as np

D_MODEL = 1024
BATCH = 2
SEQ = 16384
DEPTH = 4

D_MIX = 2 * D_MODEL
SSD_WIDTH = D_MIX // 2
SSD_HEAD_DIM = 64
SSD_HEADS = SSD_WIDTH // SSD_HEAD_DIM
SSD_GROUPS = 2
SSD_STATE = 64
SSD_CONV = 4
SSD_CHUNK = 128
D_XBC = SSD_WIDTH + 2 * SSD_GROUPS * SSD_STATE
D_SSD_IN = SSD_WIDTH + D_XBC + SSD_HEADS
MLSTM_WIDTH = D_MIX // 4
MLSTM_HEAD_DIM = 128
MLSTM_HEADS = MLSTM_WIDTH // MLSTM_HEAD_DIM
MLSTM_CONV = 4
MLSTM_CHUNK = 128
D_MLSTM_IN = 2 * MLSTM_WIDTH + 2 * MLSTM_HEADS
RWKV_WIDTH = D_MIX // 4
RWKV_HEAD_DIM = 64
RWKV_HEADS = RWKV_WIDTH // RWKV_HEAD_DIM
RWKV_DECAY_RANK = 64
RWKV_AAA_RANK = 64
RWKV_GATE_RANK = 128
D_RWKV_IN = 3 * RWKV_WIDTH + RWKV_DECAY_RANK + RWKV_AAA_RANK + RWKV_GATE_RANK
D_IN = D_SSD_IN + D_MLSTM_IN + D_RWKV_IN
D_FF = 2816
FFN_HALF = 0.5
EPS = 1e-6
RWKV_GN_EPS = 64e-5

kernel_name = 'hybrid_ssd_mlstm_rwkv7_macaron'


def rms_norm(x, g):
    xf = x.astype(jnp.float32)
    xf = xf * lax.rsqrt(jnp.mean(xf * xf, axis=-1, keepdims=True) + EPS)
    return (xf * g).astype(x.dtype)


def group_rms_norm(y, g, n_groups):
    b, t, w = y.shape
    yf = y.astype(jnp.float32).reshape(b, t, n_groups, w // n_groups)
    yf = yf * lax.rsqrt(jnp.mean(yf * yf, axis=-1, keepdims=True) + EPS)
    return yf.reshape(b, t, w) * g


def swiglu(x, w_gate_up, w_down):
    gate, up = jnp.split(x @ w_gate_up, 2, axis=-1)
    return (jax.nn.silu(gate) * up) @ w_down


def causal_depthwise_conv(x, w, b):
    k = w.shape[0]
    y = lax.conv_general_dilated(x, w[:, None, :].astype(x.dtype), window_strides=(1,),
                                 padding=[(k - 1, 0)], dimension_numbers=('NWC', 'WIO', 'NWC'),
                                 feature_group_count=x.shape[-1])
    return y + b


def causal_mask(n):
    return jnp.tril(jnp.ones((n, n), dtype=bool))


def ssd_mixer(u, conv_w, conv_b, dt_bias, a_log, d_skip, norm_g):
    bsz, t, _ = u.shape
    nc, L, G, J, P, N = t // SSD_CHUNK, SSD_CHUNK, SSD_GROUPS, SSD_HEADS // SSD_GROUPS, SSD_HEAD_DIM, SSD_STATE
    z, xbc, dt_raw = jnp.split(u, [SSD_WIDTH, SSD_WIDTH + D_XBC], axis=-1)
    xbc = jax.nn.silu(causal_depthwise_conv(xbc, conv_w, conv_b).astype(jnp.float32))
    xs, b_in, c_in = jnp.split(xbc, [SSD_WIDTH, SSD_WIDTH + G * N], axis=-1)
    dt = jax.nn.softplus((dt_raw + dt_bias).astype(jnp.float32)).reshape(bsz, nc, L, G, J)
    a = dt * (-jnp.exp(a_log.astype(jnp.float32))).reshape(G, J)
    a_cum = jnp.cumsum(a, axis=2)
    X = xs.reshape(bsz, nc, L, G, J, P)
    Bc = b_in.reshape(bsz, nc, L, G, N)
    Cc = c_in.reshape(bsz, nc, L, G, N)
    x_dt = X * dt[..., None]
    seg = a_cum[:, :, :, None] - a_cum[:, :, None, :]
    decay_ls = jnp.exp(jnp.where(causal_mask(L)[:, :, None, None], seg, -jnp.inf))
    cb = jnp.einsum('bclgn,bcsgn->bclsg', Cc, Bc)
    y_diag = jnp.einsum('bclsgj,bcsgjp->bclgjp', cb[..., None] * decay_ls, x_dt)
    decay_to_end = jnp.exp(a_cum[:, :, -1:] - a_cum)
    states = jnp.einsum('bcsgn,bcsgjp->bcgjpn', Bc, x_dt * decay_to_end[..., None])
    chunk_decay = jnp.exp(a_cum[:, :, -1])

    def step(s, inp):
        st, dec = inp
        return s * dec[..., None, None] + st, s

    _, s_prev = lax.scan(step, jnp.zeros_like(states[:, 0]),
                         (jnp.moveaxis(states, 1, 0), jnp.moveaxis(chunk_decay, 1, 0)))
    s_prev = jnp.moveaxis(s_prev, 0, 1)
    y_off = jnp.einsum('bclgn,bcgjpn->bclgjp', Cc, s_prev) * jnp.exp(a_cum)[..., None]
    y = y_diag + y_off + X * d_skip.astype(jnp.float32).reshape(G, J, 1)
    y = y.reshape(bsz, t, SSD_WIDTH) * jax.nn.silu(z.astype(jnp.float32))
    return group_rms_norm(y, norm_g, G).astype(u.dtype)


def mlstm_mixer(u, conv_w, conv_b, wq, wk, wv, gate_bias, norm_g):
    bsz, t, _ = u.shape
    H, Dh, L = MLSTM_HEADS, MLSTM_HEAD_DIM, MLSTM_CHUNK
    nc = t // L
    xm, o_pre, if_pre = jnp.split(u, [MLSTM_WIDTH, 2 * MLSTM_WIDTH], axis=-1)
    xc = jax.nn.silu(causal_depthwise_conv(xm, conv_w, conv_b).astype(jnp.float32))
    xc_h = xc.reshape(bsz, t, H, Dh)
    xm_h = xm.astype(jnp.float32).reshape(bsz, t, H, Dh)
    q = jnp.einsum('bthd,hde->bthe', xc_h, wq)
    k = jnp.einsum('bthd,hde->bthe', xc_h, wk) * (Dh ** -0.5)
    v = jnp.einsum('bthd,hde->bthe', xm_h, wv)
    gates = (if_pre + gate_bias).astype(jnp.float32)
    log_i = gates[..., :H].reshape(bsz, nc, L, H)
    log_f = jax.nn.log_sigmoid(gates[..., H:]).reshape(bsz, nc, L, H)
    o = jax.nn.sigmoid(o_pre.astype(jnp.float32)).reshape(bsz, t, H, Dh)
    qc = q.reshape(bsz, nc, L, H, Dh)
    kc = k.reshape(bsz, nc, L, H, Dh)
    vc = v.reshape(bsz, nc, L, H, Dh)
    bcum = jnp.cumsum(log_f, axis=2)
    d_log = bcum[:, :, :, None] - bcum[:, :, None, :] + log_i[:, :, None, :]
    d_log = jnp.where(causal_mask(L)[:, :, None], d_log, -jnp.inf)
    m_intra = jnp.max(d_log, axis=3)
    end_log = bcum[:, :, -1:] - bcum + log_i
    m_loc = jnp.max(end_log, axis=2)
    w_end = jnp.exp(end_log - m_loc[:, :, None])
    c_loc = jnp.einsum('bcshd,bcshe->bchde', vc * w_end[..., None], kc)
    n_loc = jnp.einsum('bcsh,bcshe->bche', w_end, kc)
    b_end = bcum[:, :, -1]

    def step(carry, inp):
        c_st, n_st, m_st = carry
        cl, nl, ml, be = inp
        m_new = jnp.maximum(be + m_st, ml)
        s_old = jnp.exp(be + m_st - m_new)
        s_new = jnp.exp(ml - m_new)
        c_new = c_st * s_old[..., None, None] + cl * s_new[..., None, None]
        n_new = n_st * s_old[..., None] + nl * s_new[..., None]
        return (c_new, n_new, m_new), (c_st, n_st, m_st)

    init = (jnp.zeros((bsz, H, Dh, Dh), jnp.float32), jnp.zeros((bsz, H, Dh), jnp.float32),
            jnp.zeros((bsz, H), jnp.float32))
    _, (c_prev, n_prev, m_prev) = lax.scan(
        step, init, (jnp.moveaxis(c_loc, 1, 0), jnp.moveaxis(n_loc, 1, 0),
                     jnp.moveaxis(m_loc, 1, 0), jnp.moveaxis(b_end, 1, 0)))
    c_prev = jnp.moveaxis(c_prev, 0, 1)
    n_prev = jnp.moveaxis(n_prev, 0, 1)
    m_prev = jnp.moveaxis(m_prev, 0, 1)
    inter_log = bcum + m_prev[:, :, None]
    m_t = jnp.maximum(inter_log, m_intra)
    w_intra = jnp.exp(d_log - m_t[:, :, :, None])
    w_inter = jnp.exp(inter_log - m_t)
    qk = jnp.einsum('bclhe,bcshe->bclsh', qc, kc) * w_intra
    num = (jnp.einsum('bclsh,bcshd->bclhd', qk, vc)
           + jnp.einsum('bclhe,bchde->bclhd', qc, c_prev) * w_inter[..., None])
    den = jnp.sum(qk, axis=3) + jnp.einsum('bclhe,bche->bclh', qc, n_prev) * w_inter
    h = num / jnp.maximum(jnp.abs(den), jnp.exp(-m_t))[..., None]
    h = h.reshape(bsz, t, H, Dh) * o
    return group_rms_norm(h.reshape(bsz, t, MLSTM_WIDTH), norm_g, H).astype(u.dtype)


def rwkv7_mixer(u, shift_mu, w0, w_up, a0, a_up, g_up, k_k, k_a, r_k, ln_w, ln_b):
    bsz, t, _ = u.shape
    H, N, W = RWKV_HEADS, RWKV_HEAD_DIM, RWKV_WIDTH
    uf = u.astype(jnp.float32)
    u_prev = jnp.pad(uf, ((0, 0), (1, 0), (0, 0)))[:, :-1]
    us = uf + (u_prev - uf) * shift_mu
    r, k, v, wl, al, gl = jnp.split(
        us, [W, 2 * W, 3 * W, 3 * W + RWKV_DECAY_RANK, 3 * W + RWKV_DECAY_RANK + RWKV_AAA_RANK], axis=-1)
    w_log = -jax.nn.softplus(-(w0 + jnp.tanh(wl) @ w_up)) - 0.5
    decay = jnp.exp(-jnp.exp(w_log))
    a = jax.nn.sigmoid(a0 + al @ a_up)
    g = jax.nn.sigmoid(gl) @ g_up
    kk = (k * k_k).reshape(bsz, t, H, N)
    kk = kk / jnp.maximum(jnp.sqrt(jnp.sum(kk * kk, axis=-1, keepdims=True)), 1e-6)
    k = k * (1.0 + (a - 1.0) * k_a)
    rh, kh, vh = (z.reshape(bsz, t, H, N) for z in (r, k, v))
    dh, ah = decay.reshape(bsz, t, H, N), a.reshape(bsz, t, H, N)

    def tm(z):
        return jnp.moveaxis(z, 1, 0)

    def step(s, inp):
        r_t, w_t, k_t, v_t, kk_t, a_t = inp
        sa = jnp.einsum('bhvk,bhk->bhv', s, -kk_t)
        s = (s * w_t[:, :, None, :] + sa[..., None] * (kk_t * a_t)[:, :, None, :]
             + v_t[..., None] * k_t[:, :, None, :])
        return s, jnp.einsum('bhvk,bhk->bhv', s, r_t)

    _, y = lax.scan(step, jnp.zeros((bsz, H, N, N), jnp.float32),
                    (tm(rh), tm(dh), tm(kh), tm(vh), tm(kk), tm(ah)))
    y = jnp.moveaxis(y, 0, 1)
    mu = jnp.mean(y, axis=-1, keepdims=True)
    var = jnp.mean(jnp.square(y - mu), axis=-1, keepdims=True)
    y = (y - mu) * lax.rsqrt(var + RWKV_GN_EPS) * ln_w.reshape(H, N) + ln_b.reshape(H, N)
    y = y + jnp.sum(rh * kh * r_k, axis=-1, keepdims=True) * vh
    return (y.reshape(bsz, t, W) * g).astype(u.dtype)


def setup_inputs(seed: int = 0) -> dict:
    key = jax.random.key(seed)
    keys = iter(jax.random.split(key, 48))
    L, D = DEPTH, D_MODEL

    def nrm(shape, scale):
        return scale * jax.random.normal(next(keys), shape, jnp.float32)

    def unif(shape, lo, hi):
        return jax.random.uniform(next(keys), shape, jnp.float32, lo, hi)

    dt_init = jnp.exp(unif((L, SSD_HEADS), float(np.log(1e-3)), float(np.log(1e-1))))
    return {
        'x': nrm((BATCH, SEQ, D), 1.0),
        'ffn1_norm': 1.0 + nrm((L, D), 0.02),
        'ffn1_w_gate_up': nrm((L, D, 2 * D_FF), D ** -0.5),
        'ffn1_w_down': nrm((L, D_FF, D), D_FF ** -0.5),
        'mix_norm': 1.0 + nrm((L, D), 0.02),
        'w_in': nrm((L, D, D_IN), D ** -0.5),
        'ssd_conv_w': nrm((L, SSD_CONV, D_XBC), SSD_CONV ** -0.5),
        'ssd_conv_b': nrm((L, D_XBC), 0.02),
        'ssd_dt_bias': dt_init + jnp.log(-jnp.expm1(-dt_init)),
        'ssd_a_log': jnp.log(unif((L, SSD_HEADS), 1.0, 16.0)),
        'ssd_d': 1.0 + nrm((L, SSD_HEADS), 0.02),
        'ssd_norm': 1.0 + nrm((L, SSD_WIDTH), 0.02),
        'mlstm_conv_w': nrm((L, MLSTM_CONV, MLSTM_WIDTH), MLSTM_CONV ** -0.5),
        'mlstm_conv_b': nrm((L, MLSTM_WIDTH), 0.02),
        'mlstm_wq': nrm((L, MLSTM_HEADS, MLSTM_HEAD_DIM, MLSTM_HEAD_DIM), MLSTM_HEAD_DIM ** -0.5),
        'mlstm_wk': nrm((L, MLSTM_HEADS, MLSTM_HEAD_DIM, MLSTM_HEAD_DIM), MLSTM_HEAD_DIM ** -0.5),
        'mlstm_wv': nrm((L, MLSTM_HEADS, MLSTM_HEAD_DIM, MLSTM_HEAD_DIM), MLSTM_HEAD_DIM ** -0.5),
        'mlstm_gate_bias': jnp.concatenate(
            [nrm((L, MLSTM_HEADS), 0.1),
             jnp.linspace(3.0, 6.0, MLSTM_HEADS, dtype=jnp.float32) + nrm((L, MLSTM_HEADS), 0.1)], axis=-1),
        'mlstm_norm': 1.0 + nrm((L, MLSTM_WIDTH), 0.02),
        'rwkv_shift_mu': unif((L, D_RWKV_IN), 0.0, 1.0),
        'rwkv_w0': jnp.linspace(-6.0, -1.0, RWKV_WIDTH, dtype=jnp.float32) + nrm((L, RWKV_WIDTH), 0.1),
        'rwkv_w_up': nrm((L, RWKV_DECAY_RANK, RWKV_WIDTH), 0.5 * RWKV_DECAY_RANK ** -0.5),
        'rwkv_a0': nrm((L, RWKV_WIDTH), 0.1),
        'rwkv_a_up': nrm((L, RWKV_AAA_RANK, RWKV_WIDTH), 0.5 * RWKV_AAA_RANK ** -0.5),
        'rwkv_g_up': nrm((L, RWKV_GATE_RANK, RWKV_WIDTH), RWKV_GATE_RANK ** -0.5),
        'rwkv_k_k': 0.85 + nrm((L, RWKV_WIDTH), 0.02),
        'rwkv_k_a': 1.0 + nrm((L, RWKV_WIDTH), 0.02),
        'rwkv_r_k': nrm((L, RWKV_HEADS, RWKV_HEAD_DIM), 0.1),
        'rwkv_ln_w': 1.0 + nrm((L, RWKV_WIDTH), 0.02),
        'rwkv_ln_b': nrm((L, RWKV_WIDTH), 0.02),
        'w_out': nrm((L, D_MIX, D), D_MIX ** -0.5),
        'ffn2_norm': 1.0 + nrm((L, D), 0.02),
        'ffn2_w_gate_up': nrm((L, D, 2 * D_FF), D ** -0.5),
        'ffn2_w_down': nrm((L, D_FF, D), D_FF ** -0.5),
        'final_norm': 1.0 + nrm((D,), 0.02),
    }


def reference(x, ffn1_norm, ffn1_w_gate_up, ffn1_w_down, mix_norm, w_in,
              ssd_conv_w, ssd_conv_b, ssd_dt_bias, ssd_a_log, ssd_d, ssd_norm,
              mlstm_conv_w, mlstm_conv_b, mlstm_wq, mlstm_wk, mlstm_wv, mlstm_gate_bias, mlstm_norm,
              rwkv_shift_mu, rwkv_w0, rwkv_w_up, rwkv_a0, rwkv_a_up, rwkv_g_up, rwkv_k_k, rwkv_k_a,
              rwkv_r_k, rwkv_ln_w, rwkv_ln_b, w_out, ffn2_norm, ffn2_w_gate_up, ffn2_w_down, final_norm):
    for l in range(DEPTH):
        x = x + FFN_HALF * swiglu(rms_norm(x, ffn1_norm[l]), ffn1_w_gate_up[l], ffn1_w_down[l])
        u = rms_norm(x, mix_norm[l]) @ w_in[l]
        u_ssd, u_ml, u_rw = jnp.split(u, [D_SSD_IN, D_SSD_IN + D_MLSTM_IN], axis=-1)
        y_ssd = ssd_mixer(u_ssd, ssd_conv_w[l], ssd_conv_b[l], ssd_dt_bias[l], ssd_a_log[l],
                          ssd_d[l], ssd_norm[l])
        y_ml = mlstm_mixer(u_ml, mlstm_conv_w[l], mlstm_conv_b[l], mlstm_wq[l], mlstm_wk[l],
                           mlstm_wv[l], mlstm_gate_bias[l], mlstm_norm[l])
        y_rw = rwkv7_mixer(u_rw, rwkv_shift_mu[l], rwkv_w0[l], rwkv_w_up[l], rwkv_a0[l], rwkv_a_up[l],
                           rwkv_g_up[l], rwkv_k_k[l], rwkv_k_a[l], rwkv_r_k[l], rwkv_ln_w[l], rwkv_ln_b[l])
        y = jnp.concatenate([y_ssd, y_ml, y_rw], axis=-1)
        x = x + y @ w_out[l]
        x = x + FFN_HALF * swiglu(rms_norm(x, ffn2_norm[l]), ffn2_w_gate_up[l], ffn2_w_down[l])
    return rms_norm(x, final_norm)
```

```python
import numpy as np
import ml_dtypes
from contextlib import ExitStack
import concourse.bass as bass
import concourse.mybir as mybir
from concourse.bass_utils import run_bass_kernel_spmd

F32 = mybir.dt.float32
BF16 = mybir.dt.bfloat16
AF = mybir.ActivationFunctionType
ALU = mybir.AluOpType

D_MODEL = 1024
DEPTH = 4
D_FF = 2816
EPS = 1e-6
WQ = "sp"


class Buf:
    __slots__ = ("name", "w", "r", "dsem", "dcnt", "excl")

    def __init__(self, name, excl=False):
        self.name = name
        self.excl = excl
        self.w = None
        self.r = {}
        self.dsem = None
        self.dcnt = 0


class Em:
    def __init__(self, nc, es):
        self.nc = nc
        self.es = es
        self.eng = {"pe": nc.tensor, "act": nc.scalar, "dve": nc.vector, "pool": nc.gpsimd, "sp": nc.sync}
        self.sem = {k: es.enter_context(nc.semaphore("s_" + k)) for k in ["pe", "act", "dve", "pool"]}
        self.cnt = {k: 0 for k in self.sem}
        self.known = {}
        self.nsem = 0
        self.nins = 0
        self.pes = None
        self.prefix = ""
        self.sempool = []
        self.dma_bufs = []
        self.extra = []

    def sbuf(self, name, shape, dt):
        st = self.pes if self.pes is not None else self.es
        return st.enter_context(self.nc.sbuf_tensor(self.prefix + name, list(shape), dt))

    def barrier(self):
        evs = [(self.sem[k], self.cnt[k]) for k in self.sem if self.cnt[k] > 0]
        evs += [(b.dsem, b.dcnt) for b in self.dma_bufs]
        evs += [(s_, c_) for (s_, c_) in self.extra if c_ > 0]
        for e in self.eng:
            self._wait(e, evs)

    def end_phase(self):
        for b in self.dma_bufs:
            self.sempool.append((b.dsem, b.dcnt))
            b.dsem = None
        self.dma_bufs = []

    def psum(self, name, shape, dt=F32):
        return self.es.enter_context(self.nc.psum_tensor(name, list(shape), dt))

    def _deps(self, reads, writes):
        evs = []
        for b in reads:
            if b.w is not None:
                evs.append(b.w)
            if b.excl:
                evs.extend(b.r.values())
        for b in writes:
            if b.w is not None:
                evs.append(b.w)
            evs.extend(b.r.values())
        return evs

    def _wait(self, e, evs):
        need = {}
        for (s, v) in evs:
            k = id(s)
            if v > self.known.get((e, k), 0):
                if k not in need or need[k][1] < v:
                    need[k] = (s, v)
        for k, (s, v) in need.items():
            self.eng[e].wait_ge(s, v)
            self.known[(e, k)] = v

    def _mark(self, ev, reads, writes):
        k = id(ev[0])
        for b in reads:
            b.r[k] = ev
        for b in writes:
            b.w = ev
            b.r = {}

    def op(self, e, fn, reads=(), writes=()):
        evs = self._deps(reads, writes)
        if e == "pe":
            evs = [ev for ev in evs if ev[0] is not self.sem["pe"]]
        self._wait(e, evs)
        ins = fn(self.eng[e])
        self.cnt[e] += 1
        ins.then_inc(self.sem[e], 1)
        self._mark((self.sem[e], self.cnt[e]), reads, writes)
        self.nins += 1

    def dma(self, q, out, in_, prim, reads=(), writes=()):
        evs = self._deps(reads, writes)
        self._wait(q, evs)
        if prim.dsem is None:
            if self.sempool:
                prim.dsem, prim.dcnt = self.sempool.pop()
            else:
                prim.dsem = self.es.enter_context(self.nc.semaphore("d%d" % self.nsem))
                prim.dcnt = 0
                self.nsem += 1
            self.dma_bufs.append(prim)
        prim.dcnt += 16
        self.eng[q].dma_start(out=out, in_=in_).then_inc(prim.dsem, 16)
        self._mark((prim.dsem, prim.dcnt), reads, writes)
        self.nins += 1

    def wait_bufs(self, q, bufs):
        evs = []
        for b in bufs:
            if b.w is not None:
                evs.append(b.w)
            evs.extend(b.r.values())
        self._wait(q, evs)


def build_tok(ntok, TT, has_y, n_ffn, out_h, out_final, ctx=None):
    NT = ntok // TT
    D, KC, FC = D_MODEL, 8, D_FF // 128
    if ctx is None:
        nc = bass.Bass("TRN2", target_bir_lowering=False)
        x_in = nc.dram_tensor("x_in", [D, ntok], F32, kind="ExternalInput").ap()
        x_out = nc.dram_tensor("x_out", [D, ntok], F32, kind="ExternalOutput").ap()
        y_load = None
        if has_y:
            y_in = nc.dram_tensor("y_in", [2048, ntok], BF16, kind="ExternalInput").ap()
            w_o = nc.dram_tensor("w_o", [2048, D], F32, kind="ExternalInput").ap()
            g_y = nc.dram_tensor("g_y", [128, 8], F32, kind="ExternalInput").ap()
        wgu, wd, gn = [], [], []
        for i in range(n_ffn):
            wgu.append(nc.dram_tensor("wgu%d" % i, [D, 2 * D_FF], F32, kind="ExternalInput").ap())
            wd.append(nc.dram_tensor("wd%d" % i, [D_FF, D], F32, kind="ExternalInput").ap())
            gn.append(nc.dram_tensor("gn%d" % i, [128, 8], F32, kind="ExternalInput").ap())
        if out_h:
            h_out = nc.dram_tensor("h_out", [D, ntok], BF16, kind="ExternalOutput").ap()
        if out_final:
            g_f = nc.dram_tensor("g_f", [128, 8], F32, kind="ExternalInput").ap()
            o_fin = nc.dram_tensor("o_fin", [D, ntok], F32, kind="ExternalOutput").ap()
    else:
        nc = ctx["nc"]
        x_in, x_out = ctx["x_in"], ctx["x_out"]
        y_load = ctx.get("y_load")
        w_o, g_y = ctx.get("w_o"), ctx.get("g_y")
        wgu, wd, gn = ctx["wgu"], ctx["wd"], ctx["gn"]
        h_out, g_f, o_fin = ctx.get("h_out"), ctx.get("g_f"), ctx.get("o_fin")

    xin_v = x_in.rearrange("(c p) n -> p c n", p=128)
    xout_v = x_out.rearrange("(c p) n -> p c n", p=128)

    es = ExitStack()
    with es:
        if ctx is None:
            em = Em(nc, es)
        else:
            em = ctx["em"]
            em.pes = es
        W1 = em.sbuf("W1", [128, KC, 2 * D_FF], BF16)
        W2 = em.sbuf("W2", [128, FC, D], BF16)
        SC = 1408
        NSTG = 3
        stg = [em.sbuf("stg%d" % i, [128, SC], F32) for i in range(NSTG)]
        bStg = [Buf("stg%d" % i) for i in range(NSTG)]
        xt = [em.sbuf("xt%d" % i, [128, KC, TT], F32) for i in range(2)]
        bX = [Buf("xt%d" % i) for i in range(2)]
        hs = [em.sbuf("hs%d" % i, [128, KC, TT], BF16) for i in range(2)]
        bH = [Buf("hs%d" % i) for i in range(2)]
        NACT = 4
        actb = em.sbuf("actb", [128, NACT, TT], BF16)
        bAct = [Buf("act%d" % i) for i in range(NACT)]
        sgt = em.sbuf("sgt", [128, 2, TT], F32)
        bSg = [Buf("sg%d" % i) for i in range(2)]
        rstd = em.sbuf("rstd", [128, 2, TT], F32)
        bR = [Buf("rstd%d" % i) for i in range(2)]
        ones = em.sbuf("ones", [128, 128], BF16)
        bOnes = Buf("ones")
        gsb = em.sbuf("gsb", [128, 4, 8], F32)
        bG = Buf("gsb")
        if has_y:
            yt = em.sbuf("yt", [128, 16, TT], BF16)
            bY = Buf("yt")
        if out_final:
            fo = em.sbuf("fo", [128, KC, TT], F32)
            bFo = Buf("fo")
        assert TT <= 256
        if ctx is None:
            pbank = [em.psum("pb%d" % i, [128, 512], F32) for i in range(8)]
        else:
            pbank = ctx["banks"]

        def ph(i):
            return pbank[i // 2][:, (i % 2) * 256:(i % 2) * 256 + TT]
        bPb = [Buf("pb%d" % i, True) for i in range(8)] if ctx is None else ctx["bbanks"]
        bP = [bPb[i // 2] for i in range(16)]
        bW1 = [Buf("W1p%d" % i) for i in range(4)]
        bW2 = [Buf("W2p%d" % i) for i in range(2)]
        bDx = [Buf("dx%d" % i) for i in range(NT)]

        em.op("dve", lambda e: e.memset(ones[:], 1.0), writes=[bOnes])
        em.dma("sp", gsb[:, 0, :], (g_y if has_y else gn[0])[:, :], bG, writes=[bG])
        for i in range(n_ffn):
            em.dma("sp", gsb[:, 1 + i, :], gn[i][:, :], bG, writes=[bG])
        if out_final:
            em.dma("sp", gsb[:, 3, :], g_f[:, :], bG, writes=[bG])

        stg_i = [0]

        def load_piece(dst_ap, src_ap, ncols, scale_ap, wbuf):
            s = stg_i[0] % NSTG
            stg_i[0] += 1
            em.dma(WQ, stg[s][:, 0:ncols], src_ap, bStg[s], writes=[bStg[s]])
            if scale_ap is None:
                em.op("pool", lambda e: e.tensor_copy(dst_ap, stg[s][:, 0:ncols]), reads=[bStg[s]], writes=[wbuf])
            else:
                em.op("pool", lambda e: e.tensor_scalar(dst_ap, stg[s][:, 0:ncols], scale_ap, None, ALU.mult),
                      reads=[bStg[s], bG], writes=[wbuf])

        def load_ffn_w1_piece(i, pc):
            for c in range(KC):
                load_piece(W1[:, c, pc * SC:(pc + 1) * SC], wgu[i][c * 128:(c + 1) * 128, pc * SC:(pc + 1) * SC], SC,
                           gsb[:, 1 + i, c:c + 1], bW1[pc])

        def load_ffn_w2_piece(i, pc):
            for j in range(pc * 11, pc * 11 + 11):
                load_piece(W2[:, j, :], wd[i][j * 128:(j + 1) * 128, :], D, None, bW2[pc])

        def load_wout():
            for j in range(16):
                load_piece(W2[:, j, :], w_o[j * 128:(j + 1) * 128, :], D,
                           gsb[:, 0, j:j + 1] if j < 8 else None, bW2[j // 8])

        xcnt = [0]

        def stats(slot, rs, nparts=1, src=None, srcbuf=None):
            src = xt[slot] if src is None else src
            srcbuf = bX[slot] if srcbuf is None else srcbuf
            hslot = slot
            em.op("act", lambda e: e.activation(out=hs[hslot][:, :, :], in_=src[:, 0:KC, :], func=AF.Square),
                  reads=[srcbuf], writes=[bH[hslot]])
            for c in range(KC):
                em.op("pe", lambda e, c=c: e.matmul(ph(14 + rs), lhsT=ones[:, :], rhs=hs[hslot][:, c, :],
                                                  start=(c == 0), stop=(c == KC - 1)),
                      reads=[bH[hslot], bOnes], writes=[bP[14 + rs]])
            em.op("act", lambda e: e.activation(out=rstd[:, rs, :], in_=ph(14 + rs), func=AF.Sqrt, bias=EPS,
                                                scale=1.0 / D), reads=[bP[14 + rs]], writes=[bR[rs]])
            em.op("dve", lambda e: e.reciprocal(rstd[:, rs, :], rstd[:, rs, :]), reads=[bR[rs]], writes=[bR[rs]])

        def make_h(slot, rs):
            em.op("pool", lambda e: e.tensor_tensor(hs[slot][:, :, :], xt[slot][:, :, :],
                                                    rstd[:, rs, :].unsqueeze(1).to_broadcast([128, KC, TT]), ALU.mult),
                  reads=[bX[slot], bR[rs]], writes=[bH[slot]])

        def load_x(phase_idx, i, slot):
            if phase_idx == 0:
                em.dma("sp", xt[slot][:, :, :], xin_v[:, :, i * TT:(i + 1) * TT], bX[slot], writes=[bX[slot]])
            else:
                em.dma("sp", xt[slot][:, :, :], xout_v[:, :, i * TT:(i + 1) * TT], bX[slot], reads=[bDx[i]],
                       writes=[bX[slot]])

        def store_x(i, slot):
            em.dma("sp", xout_v[:, :, i * TT:(i + 1) * TT], xt[slot][:, :, :], bX[slot], reads=[bX[slot]],
                   writes=[bDx[i]])

        def post(i, slot):
            if out_h:
                stats(slot, slot)
                make_h(slot, slot)
                em.dma("sp", h_out.rearrange("(c p) n -> p c n", p=128)[:, :, i * TT:(i + 1) * TT], hs[slot][:, :, :],
                       bH[slot], reads=[bH[slot]])
            if out_final:
                stats(slot, slot)
                for c in range(KC):
                    em.op("dve", lambda e, c=c: e.scalar_tensor_tensor(out=fo[:, c, :], in0=xt[slot][:, c, :],
                                                                     scalar=gsb[:, 3, c:c + 1], in1=rstd[:, slot, :],
                                                                     op0=ALU.mult, op1=ALU.mult),
                          reads=[bX[slot], bR[slot], bG], writes=[bFo])
                em.dma("sp", o_fin.rearrange("(c p) n -> p c n", p=128)[:, :, i * TT:(i + 1) * TT], fo[:, :, :],
                       bFo, reads=[bFo])

        phases = (["wout"] if has_y else []) + [("ffn", i) for i in range(n_ffn)]
        for pi, phs in enumerate(phases):
            last = (pi == len(phases) - 1)
            if phs == "wout":
                load_wout()
                for i in range(NT):
                    slot = xcnt[0] % 2
                    xcnt[0] += 1
                    load_x(pi, i, slot)
                    if y_load is None:
                        em.dma("sp", yt[:, :, :], y_in.rearrange("(c p) n -> p c n", p=128)[:, :, i * TT:(i + 1) * TT],
                               bY, writes=[bY])
                    else:
                        y_load(em, yt, bY, i)
                    em.op("act", lambda e: e.activation(out=hs[slot][:, :, :], in_=yt[:, 0:8, :], func=AF.Square),
                          reads=[bY], writes=[bH[slot]])
                    for g in range(2):
                        for c in range(4):
                            em.op("pe", lambda e, g=g, c=c: e.matmul(ph(14 + g), lhsT=ones[:, :],
                                                                    rhs=hs[slot][:, 4 * g + c, :], start=(c == 0),
                                                                    stop=(c == 3)),
                                  reads=[bH[slot], bOnes], writes=[bP[14 + g]])
                        em.op("act", lambda e, g=g: e.activation(out=rstd[:, g, :], in_=ph(14 + g), func=AF.Sqrt,
                                                                 bias=EPS, scale=1.0 / 512), reads=[bP[14 + g]],
                              writes=[bR[g]])
                        em.op("dve", lambda e, g=g: e.reciprocal(rstd[:, g, :], rstd[:, g, :]), reads=[bR[g]],
                              writes=[bR[g]])
                        em.op("dve", lambda e, g=g: e.tensor_tensor(
                            yt[:, 4 * g:4 * g + 4, :], yt[:, 4 * g:4 * g + 4, :],
                            rstd[:, g, :].unsqueeze(1).to_broadcast([128, 4, TT]), ALU.mult),
                            reads=[bY, bR[g]], writes=[bY])
                    for d in range(8):
                        for j in range(16):
                            em.op("pe", lambda e, d=d, j=j: e.matmul(ph(d), lhsT=W2[:, j, d * 128:(d + 1) * 128],
                                                                    rhs=yt[:, j, :], start=(j == 0), stop=(j == 15)),
                                  reads=[bY, bW2[j // 8]], writes=[bP[d]])
                        em.op("dve", lambda e, d=d: e.tensor_tensor(xt[slot][:, d, :], xt[slot][:, d, :], ph(d),
                                                                     ALU.add), reads=[bP[d], bX[slot]],
                              writes=[bX[slot]])
                    if last:
                        post(i, slot)
                    store_x(i, slot)
            else:
                fi = phs[1]
                for pc in (0, 2):
                    load_ffn_w1_piece(fi, pc)
                load_ffn_w2_piece(fi, 0)
                for pc in (1, 3):
                    load_ffn_w1_piece(fi, pc)
                load_ffn_w2_piece(fi, 1)

                def prep(i):
                    slot = xcnt[0] % 2
                    xcnt[0] += 1
                    load_x(pi, i, slot)
                    stats(slot, slot)
                    make_h(slot, slot)
                    return slot

                slot_next = prep(0)
                for i in range(NT):
                    slot = slot_next
                    for j in range(FC + 1):
                        if j < FC:
                            pr = 8 + 2 * (j % 3)
                            for half, off in ((0, 0), (1, D_FF)):
                                for c in range(KC):
                                    em.op("pe", lambda e, c=c, half=half, off=off, j=j, pr=pr: e.matmul(
                                        ph(pr + half), lhsT=W1[:, c, off + j * 128:off + (j + 1) * 128],
                                        rhs=hs[slot][:, c, :], start=(c == 0), stop=(c == KC - 1)),
                                        reads=[bH[slot], bW1[2 * half + (j // 11)]], writes=[bP[pr + half]])
                            sg = j % 2
                            em.op("act", lambda e, pr=pr, sg=sg: e.activation(out=sgt[:, sg, :], in_=ph(pr),
                                                                            func=AF.Silu), reads=[bP[pr]],
                                  writes=[bSg[sg]])
                            a = j % NACT
                            em.op("dve", lambda e, pr=pr, sg=sg, a=a: e.tensor_tensor(actb[:, a, :], sgt[:, sg, :],
                                                                                   ph(pr + 1), ALU.mult),
                                  reads=[bSg[sg], bP[pr + 1]], writes=[bAct[a]])
                        if j >= 1:
                            jj = j - 1
                            a = jj % NACT
                            for d in range(8):
                                em.op("pe", lambda e, d=d, jj=jj, a=a: e.matmul(
                                    ph(d), lhsT=W2[:, jj, d * 128:(d + 1) * 128], rhs=actb[:, a, :],
                                    start=(jj == 0 and d % 2 == 0), stop=(jj == FC - 1 and d % 2 == 1)),
                                    reads=[bAct[a], bW2[jj // 11]], writes=[bP[d]])
                        if j == 10 and i + 1 < NT:
                            slot_next = prep(i + 1)
                    for d in range(8):
                        em.op("dve", lambda e, d=d: e.scalar_tensor_tensor(out=xt[slot][:, d, :], in0=ph(d),
                                                                         scalar=0.5, in1=xt[slot][:, d, :],
                                                                         op0=ALU.mult, op1=ALU.add),
                              reads=[bP[d], bX[slot]], writes=[bX[slot]])
                    if last:
                        post(i, slot)
                    store_x(i, slot)
        allb = bX + bH + bDx + ([bFo] if out_final else [])
        em.wait_bufs("sp", allb)
        if ctx is not None:
            em.barrier()
            em.end_phase()
            em.pes = None
    return nc


ST = 512
NCOL = 1542
CHUNKS = [("z0", 0, 128), ("z1", 128, 128), ("x0", 256, 128), ("x1", 384, 128), ("B", 512, 64), ("C", 576, 64),
          ("dt", 640, 4), ("mx", 644, 128), ("mo", 772, 128), ("mi", 900, 1), ("mf", 901, 1),
          ("rr", 902, 128), ("rk", 1030, 128), ("rv", 1158, 128), ("rwl", 1286, 64), ("ral", 1350, 64),
          ("rgl", 1414, 128)]
KDEC = float(np.exp(-0.5))
STAGES = {"ssd", "ml", "rw"}
CUT = 99


class _Cut(Exception):
    pass


def _ck(k):
    if CUT == k:
        raise _Cut()


def _cf_layout():
    items = [("mask128", 128), ("identf", 128), ("onesf", 128), ("sel4", 512), ("m128", ST), ("m64", ST),
             ("rneg", ST), ("maskAB", 512), ("nmasklo", 128), ("signm", 128), ("blkf", 128)]
    off, o = {}, 0
    for n, w in items:
        off[n] = (o, w)
        o += w
    return off, o


def _prm_layout():
    items = [("cwx", 8), ("cbx", 2), ("cwB", 4), ("cbB", 1), ("cwC", 4), ("cbC", 1), ("dtb", 1), ("alog", 1),
             ("Dcol", 2), ("cwm", 4), ("cbm", 1), ("bi", 1), ("bf", 1), ("nm", 1), ("mu3", 3), ("mu2", 2),
             ("mug", 1), ("w0", 1), ("a0", 1), ("kk", 1), ("ka", 1), ("rkc", 1), ("lnw", 1), ("lnb", 1)]
    off, o = {}, 0
    for n, w in items:
        off[n] = (o, w)
        o += w
    return off, o


def mix_consts():
    off, tot = _cf_layout()
    cf = np.zeros((128, tot), np.float32)

    def put(n, a):
        o, w = off[n]
        cf[0:a.shape[0], o:o + w] = a
    i = np.arange(128)
    put("mask128", (i[None, :] >= i[:, None]).astype(np.float32))
    put("identf", np.eye(128, dtype=np.float32))
    put("onesf", np.ones((128, 128), np.float32))
    sel = np.zeros((4, 4, 128), np.float32)
    for h in range(4):
        sel[h, h, :] = 1.0
    put("sel4", sel.reshape(4, 512))
    t = np.arange(ST)
    put("m128", np.tile((t % 128 != 0).astype(np.float32)[None, :], (128, 1)))
    put("m64", np.tile((t % 64 != 0).astype(np.float32)[None, :], (128, 1)))
    put("rneg", np.tile(np.where(t % 128 == 0, -1e30, 0.0).astype(np.float32)[None, :], (128, 1)))
    j = np.arange(64)
    strict = (j[None, :] > j[:, None]).astype(np.float32)
    incl = (j[None, :] >= j[:, None]).astype(np.float32)
    one = np.concatenate([strict, incl, strict, incl], 1)
    put("maskAB", np.concatenate([np.concatenate([one, one], 0), np.zeros((128, 256), np.float32)], 1))
    lo = -(j[None, :] < j[:, None]).astype(np.float32)
    put("nmasklo", np.concatenate([np.concatenate([lo, lo], 0), np.zeros((128, 64), np.float32)], 1))
    put("signm", np.concatenate([np.ones((128, 64), np.float32), -np.ones((128, 64), np.float32)], 1))
    blk = np.zeros((128, 128), np.float32)
    blk[0:64, 0:64] = 1.0
    blk[64:, 64:] = 1.0
    put("blkf", blk)
    return cf


class _Tile:
    def __init__(self, t, b):
        self.t = t
        self.b = b

    def f(self, p=128, n=ST, off=0, p0=0):
        return self.t[p0:p0 + p, off:off + n]

    def h(self, p=128, n=ST, off=0, p0=0):
        return self.t[:, :].bitcast(BF16)[p0:p0 + p, off:off + n]


def build_mix(T, ctx=None):
    NS = T // ST
    cfo, cftot = _cf_layout()
    pro, prtot = _prm_layout()
    if ctx is None:
        nc = bass.Bass("TRN2", target_bir_lowering=False)
        h_in = nc.dram_tensor("h_in", [D_MODEL, T], BF16, kind="ExternalInput").ap()
        w_sel = nc.dram_tensor("w_sel", [D_MODEL, NCOL], F32, kind="ExternalInput").ap()
        g_mix = nc.dram_tensor("g_mix", [128, 8], F32, kind="ExternalInput").ap()
        cf_d = nc.dram_tensor("cf", [128, cftot], F32, kind="ExternalInput").ap()
        prm_d = nc.dram_tensor("prm", [128, prtot], F32, kind="ExternalInput").ap()
        wsm_d = nc.dram_tensor("wsm", [128, 768], F32, kind="ExternalInput").ap()
        y_out = nc.dram_tensor("y_out", [512, T], BF16, kind="ExternalOutput").ap()
        hin_v = h_in.rearrange("(c p) n -> p c n", p=128)

        def h_src(t):
            return hin_v[:, :, t * ST:(t + 1) * ST]

        def y_dst(r0, r1, t):
            return y_out[r0:r1, t * ST:(t + 1) * ST]
    else:
        nc = ctx["nc"]
        w_sel, g_mix, cf_d, prm_d, wsm_d, y_out = (ctx[k] for k in ("w_sel", "g_mix", "cf", "prm", "wsm", "y_out"))
        h_src = ctx["h_src"]
        y_dst = ctx["y_dst"]

    es = ExitStack()
    with es:
        if ctx is None:
            em = Em(nc, es)
        else:
            em = ctx["em"]
            em.pes = es
        V = em.op
        Win = em.sbuf("Win", [128, 8, NCOL], BF16)
        bWin = Buf("Win")
        ht = [em.sbuf("ht%d" % i, [128, 8, ST], BF16) for i in range(2)]
        bHt = [Buf("ht%d" % i) for i in range(2)]
        cf = em.sbuf("cfs", [128, cftot], F32)
        bC = Buf("cf")
        prm = em.sbuf("prms", [128, prtot + 16], F32)
        bPr = Buf("prm")
        wsm = em.sbuf("wsms", [128, 768], BF16)
        bWs = Buf("wsm")
        cb = em.sbuf("cbs", [128, 448], BF16)
        bCb = Buf("cb")
        gm = em.sbuf("gm", [128, 8], F32)
        bGm = Buf("gm")
        halo = em.sbuf("halo", [128, 16, 4], F32)
        bHalo = [Buf("halo%d" % i) for i in range(16)]
        NG = 56
        TW = ST + 8
        gt = [_Tile(em.sbuf("g%d" % i, [128, TW], F32), Buf("g%d" % i)) for i in range(NG)]
        if ctx is None:
            banks = [em.psum("pb%d" % i, [128, 512], F32) for i in range(8)]
            bBk = [Buf("pb%d" % i, True) for i in range(8)]
        else:
            banks, bBk = ctx["banks"], ctx["bbanks"]
        bki = [0]
        reserved = set()

        def bank(keep=False):
            while True:
                i = bki[0] % 8
                bki[0] += 1
                if i not in reserved:
                    break
            if keep:
                reserved.add(i)
            return banks[i], bBk[i]

        def unkeep(bb):
            reserved.discard(bBk.index(bb))
        from collections import deque
        free = deque(gt)
        live = []

        def G():
            g = free.popleft()
            live.append(g)
            return g

        def R(*ts):
            for g in ts:
                live.remove(g)
                free.append(g)

        def C(name, p=128, p0=0):
            o, w = cfo[name]
            return cf[p0:p0 + p, o:o + w]

        def P(name, j=0, p=128, p0=0):
            o, w = pro[name]
            return prm[p0:p0 + p, o + j:o + j + 1]
        identb, onesb, blkb = cb[:, 0:128], cb[:, 128:256], cb[:, 256:384]

        em.dma("sp", cf[:, :], cf_d[:, :], bC, writes=[bC])
        em.dma("sp", prm[:, 0:prtot], prm_d[:, :], bPr, writes=[bPr])
        em.dma("sp", gm[:, :], g_mix[:, :], bGm, writes=[bGm])
        V("dve", lambda e: e.tensor_copy(cb[:, 0:128], C("identf")), [bC], [bCb])
        V("dve", lambda e: e.tensor_copy(cb[:, 128:256], C("onesf")), [bC], [bCb])
        V("dve", lambda e: e.tensor_copy(cb[:, 256:384], C("blkf")), [bC], [bCb])
        em.dma("sp", gt[0].t[:, 0:384], wsm_d[:, 0:384], gt[0].b, writes=[gt[0].b])
        em.dma("sp", gt[1].t[:, 0:384], wsm_d[:, 384:768], gt[1].b, writes=[gt[1].b])
        V("dve", lambda e: e.tensor_copy(wsm[:, 0:384], gt[0].t[:, 0:384]), [gt[0].b], [bWs])
        V("dve", lambda e: e.tensor_copy(wsm[:, 384:768], gt[1].t[:, 0:384]), [gt[1].b], [bWs])
        wq_b, wk_b, wv_b = wsm[:, 0:128], wsm[:, 128:256], wsm[:, 256:384]
        wup_b, aup_b, gup_b = wsm[0:64, 384:512], wsm[0:64, 512:640], wsm[:, 640:768]
        for c in range(8):
            for hf in range(3):
                g = gt[2 + (c * 3 + hf) % 4]
                c0 = hf * 514
                em.dma("sp", g.t[:, 0:514], w_sel[c * 128:(c + 1) * 128, c0:c0 + 514], g.b, writes=[g.b])
                V(["dve", "pool", "act"][hf] if hf < 2 else "dve",
                  lambda e, g=g, c=c, c0=c0: e.tensor_scalar(Win[:, c, c0:c0 + 514], g.t[:, 0:514], gm[:, c:c + 1],
                                                           None, ALU.mult), [g.b, bGm], [bWin])
        o_mu3 = pro["mu3"][0]
        V("dve", lambda e: e.tensor_scalar(prm[:, prtot:prtot + 6], prm[:, o_mu3:o_mu3 + 6], -1.0, 1.0, ALU.mult,
                                          ALU.add), [bPr], [bPr])
        V("act", lambda e: e.activation(out=prm[0:4, prtot + 6:prtot + 7], in_=P("alog", 0, 4), func=AF.Exp), [bPr],
          [bPr])
        V("dve", lambda e: e.tensor_scalar(prm[0:4, prtot + 6:prtot + 7], prm[0:4, prtot + 6:prtot + 7], -1.0, None,
                                          ALU.mult), [bPr], [bPr])
        V("dve", lambda e: e.tensor_scalar(prm[0:1, prtot + 7:prtot + 8], P("bf", 0, 1), -1.0, None, ALU.mult),
          [bPr], [bPr])
        negA = prm[0:4, prtot + 6:prtot + 7]
        nbf = prm[0:1, prtot + 7:prtot + 8]

        def OMU(j, p=128):
            return prm[0:p, prtot + j:prtot + j + 1]

        stt = em.sbuf("stt", [128, 1024], F32)
        bSs, bSm, bSr, bMc = Buf("Sssd"), Buf("Sml"), Buf("Srw"), Buf("mcar")
        S_f = stt[0:64, 0:256]
        Cst = stt[:, 256:385]
        Srw = stt[:, 400:464]
        mcar = stt[0:1, 480:481]
        stb = em.sbuf("stb", [128, 512], BF16)
        bSsb, bSrb = Buf("Sssdb"), Buf("Srwb")
        S_b = stb[0:64, 0:256]
        Srw_b = stb[:, 256:320]
        vtok = em.sbuf("vtok", [128, 4, 256], BF16)
        bVt = Buf("vtok")
        nbc = em.sbuf("nbc", [128, 128], BF16)
        Cst_b = stb[:, 320:448]
        bSmb = Buf("Smlb")
        bNb = Buf("nbc")
        V("dve", lambda e: e.memset(stt[:, :], 0.0), [], [bSs, bSm, bSr, bMc])
        V("dve", lambda e: e.memset(stb[:, :], 0.0), [], [bSsb, bSrb, bSmb])
        V("dve", lambda e: e.memset(vtok[:, :, :], 1.0), [], [bVt])
        V("dve", lambda e: e.memset(nbc[:, :], 0.0), [], [bNb])
        V("dve", lambda e: e.memset(halo[:, :, :], 0.0), [], bHalo)

        def load_h(t, slot):
            em.dma("sp", ht[slot][:, :, :], h_src(t), bHt[slot], writes=[bHt[slot]])

        iden2 = em.sbuf("iden2", [128, 64], F32)
        bI2 = Buf("iden2")
        V("dve", lambda e: e.tensor_tensor(iden2[:, :], C("identf")[:, 0:64], C("identf")[:, 64:128], ALU.add), [bC],
          [bI2])
        V("dve", lambda e: e.tensor_copy(cb[:, 384:448], iden2[:, :]), [bI2], [bCb])

        def mm(out, lhsT, rhs, r, w, start=True, stop=True):
            V("pe", lambda e: e.matmul(out, lhsT=lhsT, rhs=rhs, start=start, stop=stop), r, w)

        def v3(ap, l):
            return ap.rearrange("p (c l) -> p c l", l=l)

        load_h(0, 0)
        for t in range(NS):
            slot = t % 2
            if t + 1 < NS:
                load_h(t + 1, 1 - slot)
            tsl = slice(t * ST, (t + 1) * ST)
            raw = {}
            for ci, (name, c0, M) in enumerate(CHUNKS):
                pb, bb = bank()
                for c in range(8):
                    mm(pb[0:M, :], Win[:, c, c0:c0 + M], ht[slot][:, c, :], [bWin, bHt[slot]], [bb], c == 0, c == 7)
                g = G()
                raw[name] = g
                if name in ("z0", "z1"):
                    V("act", lambda e: e.activation(out=g.f(), in_=pb[:, :], func=AF.Silu), [bb], [g.b])
                elif name == "mo":
                    V("act", lambda e: e.activation(out=g.f(), in_=pb[:, :], func=AF.Sigmoid), [bb], [g.b])
                elif name == "dt":
                    V("act", lambda e: e.activation(out=g.f(4), in_=pb[0:4, :], func=AF.Exp, bias=P("dtb", 0, 4)),
                      [bb, bPr], [g.b])
                    V("act", lambda e: e.activation(out=g.f(4), in_=g.f(4), func=AF.Ln, bias=1.0), [g.b], [g.b])
                elif name == "mi":
                    V("act", lambda e: e.activation(out=g.f(1), in_=pb[0:1, :], func=AF.Identity, bias=P("bi", 0, 1)),
                      [bb, bPr], [g.b])
                elif name == "mf":
                    V("act", lambda e: e.activation(out=g.f(1), in_=pb[0:1, :], func=AF.Exp, scale=-1.0, bias=nbf),
                      [bb, bPr], [g.b])
                    V("act", lambda e: e.activation(out=g.f(1), in_=g.f(1), func=AF.Ln, bias=1.0), [g.b], [g.b])
                else:
                    hw = 3 if name in ("x0", "x1", "B", "C", "mx") else 1
                    hb = bHalo[ci % 16]
                    V("pool", lambda e: e.tensor_copy(g.f(M, hw, 4 - hw), halo[0:M, ci % 16, 4 - hw:4]), [hb], [g.b])
                    if ci % 2:
                        V("act", lambda e: e.copy(g.f(M, ST, 4), pb[0:M, :]), [bb], [g.b])
                    else:
                        V("dve", lambda e: e.tensor_copy(g.f(M, ST, 4), pb[0:M, :]), [bb], [g.b])
                    V("pool", lambda e: e.tensor_copy(halo[0:M, ci % 16, 4 - hw:4], g.f(M, hw, 4 + ST - hw)), [g.b],
                      [hb])
            conv_in = [("x0", 128, lambda k: P("cwx", k), P("cbx", 0)),
                       ("x1", 128, lambda k: P("cwx", 4 + k), P("cbx", 1)),
                       ("B", 64, lambda k: P("cwB", k, 64), P("cbB", 0, 64)),
                       ("C", 64, lambda k: P("cwC", k, 64), P("cbC", 0, 64)),
                       ("mx", 128, lambda k: P("cwm", k), P("cbm", 0))]
            conv_out = {}
            for (name, M, wk, bcol) in conv_in:
                r = raw[name]
                acc = G()
                V("pool", lambda e: e.tensor_scalar(acc.f(M), r.f(M, ST, 4), wk(3), bcol, ALU.mult, ALU.add),
                  [r.b, bPr], [acc.b])
                tmpc_ = G()
                for k in range(3):
                    V("pool", lambda e: e.tensor_scalar(tmpc_.f(M), r.f(M, ST, 1 + k), wk(k), None, ALU.mult),
                      [r.b, bPr], [tmpc_.b])
                    V("pool", lambda e: e.tensor_tensor(acc.f(M), acc.f(M), tmpc_.f(M), ALU.add), [acc.b, tmpc_.b],
                      [acc.b])
                R(tmpc_)
                o = G()
                V("act", lambda e: e.activation(out=o.h(M), in_=acc.f(M), func=AF.Silu), [acc.b], [o.b])
                conv_out[name] = o
                R(acc)
                if name != "mx":
                    R(r)
            if "ssd" in STAGES:
                NCH = ST // 128
                sz = [raw["z0"], raw["z1"]]
                xs_b = [conv_out["x0"], conv_out["x1"]]
                B_b, C_b, xc_b = conv_out["B"], conv_out["C"], conv_out["mx"]
                dt = raw["dt"]
                a_t, acum = G(), G()
                V("dve", lambda e: e.tensor_scalar(a_t.f(4), dt.f(4), negA, None, ALU.mult), [dt.b, bPr], [a_t.b])
                V("dve", lambda e: e.tensor_tensor_scan(acum.f(4), C("m128", 4), a_t.f(4), 0.0, ALU.mult, ALU.add),
                  [a_t.b, bC], [acum.b])
                R(a_t)
                pcol, bcol_ = bank()
                for c in range(NCH):
                    mm(pcol[:, c * 4:c * 4 + 4], acum.f(4, 128, c * 128), C("identf", 4)[:, 0:4], [acum.b, bC], [bcol_])
                    mm(pcol[:, 16 + c * 4:16 + c * 4 + 4], dt.f(4, 128, c * 128), C("identf", 4)[:, 0:4], [dt.b, bC],
                       [bcol_])
                R(dt)
                cols = G()
                V("dve", lambda e: e.tensor_copy(cols.f(128, 32), pcol[:, 0:32]), [bcol_], [cols.b])
                V("dve", lambda e: e.tensor_scalar(cols.f(128, 16, 96), cols.f(128, 16, 0), -1.0, None, ALU.mult),
                  [cols.b], [cols.b])
                abc = []
                for h in range(4):
                    pa, ba = bank()
                    mm(pa[:, :], C("sel4", 4)[:, h * 128:(h + 1) * 128], acum.f(4), [acum.b, bC], [ba])
                    ab = G()
                    V("act", lambda e: e.copy(ab.f(), pa[:, :]), [ba], [ab.b])
                    abc.append(ab)
                    V("dve", lambda e: e.tensor_copy(cols.t[:, 32 + h:32 + h + 13:4], ab.t[:, 127:512:128]), [ab.b],
                      [cols.b])
                R(acum)
                V("dve", lambda e: e.tensor_tensor(cols.f(128, 16, 48), cols.f(128, 16, 32), cols.f(128, 16, 0),
                                                  ALU.subtract), [cols.b], [cols.b])
                V("act", lambda e: e.activation(out=cols.f(128, 16, 48), in_=cols.f(128, 16, 48), func=AF.Exp), [cols.b],
                  [cols.b])
                V("act", lambda e: e.activation(out=cols.f(128, 16, 80), in_=cols.f(128, 16, 32), func=AF.Exp), [cols.b],
                  [cols.b])
                V("dve", lambda e: e.tensor_tensor(cols.f(128, 16, 64), cols.f(128, 16, 48), cols.f(128, 16, 16),
                                                  ALU.mult), [cols.b], [cols.b])
                cdec = []
                for h in range(4):
                    ex, cd = G(), G()
                    V("act", lambda e: e.activation(out=ex.f(64), in_=abc[h].f(64), func=AF.Exp), [abc[h].b], [ex.b])
                    V("dve", lambda e: e.tensor_tensor(cd.h(64), ex.f(64), C_b.h(64), ALU.mult), [ex.b, C_b.b], [cd.b])
                    R(ex)
                    cdec.append(cd)
                pcb, bcb = bank()
                for c in range(NCH):
                    mm(pcb[:, c * 128:(c + 1) * 128], B_b.h(64, 128, c * 128), C_b.h(64, 128, c * 128), [B_b.b, C_b.b],
                       [bcb])
                CBm = G()
                V("dve", lambda e: e.tensor_tensor(v3(CBm.f(), 128), v3(pcb[:, :], 128),
                                                  C("mask128").unsqueeze(1).to_broadcast([128, NCH, 128]), ALU.mult),
                  [bcb, bC], [CBm.b])
                xdt, xdte, Btok = G(), G(), G()
                for half in range(2):
                    px, bx = bank()
                    pxb = px[:, :].bitcast(BF16)
                    for cc in range(2):
                        c = half * 2 + cc
                        for i in range(2):
                            V("pe", lambda e: e.transpose(pxb[:, cc * 256 + i * 128:cc * 256 + (i + 1) * 128],
                                                          xs_b[i].h(128, 128, c * 128), identb), [xs_b[i].b, bCb], [bx])
                        V("pe", lambda e: e.transpose(pxb[:, 512 + cc * 64:512 + (cc + 1) * 64],
                                                      B_b.h(64, 128, c * 128), identb[0:64, 0:64]), [B_b.b, bCb], [bx])
                    for (dst, coff) in ((xdt, 16), (xdte, 64)):
                        V("dve", lambda e: e.tensor_tensor(
                            v3(dst.h(128, 512, half * 512), 64), v3(pxb[:, 0:512], 64),
                            cols.f(128, 8, coff + half * 8).unsqueeze(2).to_broadcast([128, 8, 64]), ALU.mult),
                            [bx, cols.b], [dst.b])
                    V("act", lambda e: e.copy(Btok.h(128, 128, half * 128), pxb[:, 512:640]), [bx], [Btok.b])
                R(B_b)
                py = [bank(True), bank(True)]
                for c in range(NCH):
                    mts = []
                    for h in range(4):
                        et, mt = G(), G()
                        V("dve", lambda e: e.tensor_scalar(et.f(128, 128), abc[h].f(128, 128, c * 128),
                                                          cols.f(128, 1, 96 + c * 4 + h), 0.0, ALU.add, ALU.min),
                          [abc[h].b, cols.b], [et.b])
                        V("act", lambda e: e.activation(out=et.f(128, 128), in_=et.f(128, 128), func=AF.Exp), [et.b],
                          [et.b])
                        V("pool", lambda e: e.tensor_tensor(mt.h(128, 128), et.f(128, 128), CBm.f(128, 128, c * 128),
                                                            ALU.mult), [et.b, CBm.b], [mt.b])
                        R(et)
                        mts.append(mt)
                    for h in range(4):
                        pyb, byb = py[h // 2]
                        po = (h % 2) * 64
                        mm(pyb[po:po + 64, c * 128:(c + 1) * 128], xdt.h(128, 64, (c * 4 + h) * 64), mts[h].h(128, 128),
                           [xdt.b, mts[h].b], [byb], True, False)
                        mm(pyb[po:po + 64, c * 128:(c + 1) * 128], S_b[:, h * 64:(h + 1) * 64],
                           cdec[h].h(64, 128, c * 128), [bSsb, cdec[h].b], [byb], False, True)
                    R(*mts)
                    ps_, bs_ = bank()
                    for h in range(4):
                        mm(ps_[0:64, h * 64:(h + 1) * 64], Btok.h(128, 64, c * 64), xdte.h(128, 64, (c * 4 + h) * 64),
                           [Btok.b, xdte.b], [bs_])
                    for h in range(4):
                        V("dve", lambda e: e.scalar_tensor_tensor(
                            out=S_f[:, h * 64:(h + 1) * 64], in0=S_f[:, h * 64:(h + 1) * 64],
                            scalar=cols.f(64, 1, 80 + c * 4 + h), in1=ps_[0:64, h * 64:(h + 1) * 64], op0=ALU.mult,
                            op1=ALU.add), [bSs, cols.b, bs_], [bSs])
                    V("act", lambda e: e.copy(S_b, S_f), [bSs], [bSsb])
                R(xdt, xdte, Btok, CBm, cols, C_b, *abc, *cdec)
                for i in range(2):
                    pyb, byb = py[i]
                    tmp, yo = G(), G()
                    V("dve", lambda e: e.scalar_tensor_tensor(out=tmp.f(), in0=xs_b[i].h(), scalar=P("Dcol", i),
                                                              in1=pyb[:, :], op0=ALU.mult, op1=ALU.add),
                      [xs_b[i].b, bPr, byb], [tmp.b])
                    unkeep(byb)
                    V("pool", lambda e: e.tensor_tensor(yo.h(), tmp.f(), sz[i].f(), ALU.mult), [tmp.b, sz[i].b], [yo.b])
                    em.dma("sp", y_dst(i * 128, (i + 1) * 128, t), yo.h(), yo.b, reads=[yo.b])
                    R(tmp, yo, xs_b[i], sz[i])
            if "ml" in STAGES:
                try:
                    osig, li, sp = raw["mo"], raw["mi"], raw["mf"]
                    xc_b = conv_out["mx"]
                    NCH = ST // 128
                    rmx = raw["mx"]
                    mxb = G()
                    V("pool", lambda e: e.tensor_copy(mxb.h(), rmx.f(128, ST, 4)), [rmx.b], [mxb.b])
                    R(rmx)
                    pq, bq = bank()
                    mm(pq[:, :], wq_b, xc_b.h(), [bWs, xc_b.b], [bq])
                    q_b = G()
                    V("act", lambda e: e.copy(q_b.h(), pq[:, :]), [bq], [q_b.b])
                    pk, bk = bank()
                    mm(pk[:, :], wk_b, xc_b.h(), [bWs, xc_b.b], [bk])
                    k_b = G()
                    V("act", lambda e: e.activation(out=k_b.h(), in_=pk[:, :], func=AF.Identity, scale=128.0 ** -0.5), [bk],
                      [k_b.b])
                    _ck(1)
                    bcum, g_, cmx, mint, sm, il, mt = G(), G(), G(), G(), G(), G(), G()
                    V("dve", lambda e: e.tensor_tensor_scan(bcum.f(1), C("m128", 1), sp.f(1), 0.0, ALU.mult, ALU.subtract),
                      [sp.b, bC], [bcum.b])
                    V("dve", lambda e: e.tensor_tensor(g_.f(1), li.f(1), bcum.f(1), ALU.subtract), [li.b, bcum.b], [g_.b])
                    V("dve", lambda e: e.tensor_tensor_scan(cmx.f(1), C("rneg", 1), g_.f(1), 0.0, ALU.add, ALU.max),
                      [g_.b, bC], [cmx.b])
                    V("dve", lambda e: e.tensor_tensor(mint.f(1), bcum.f(1), cmx.f(1), ALU.add), [bcum.b, cmx.b], [mint.b])
                    V("dve", lambda e: e.tensor_copy(sm.f(1, 4, 0), bcum.t[0:1, 127:512:128]), [bcum.b], [sm.b])
                    V("dve", lambda e: e.tensor_copy(sm.f(1, 4, 4), cmx.t[0:1, 127:512:128]), [cmx.b], [sm.b])
                    V("dve", lambda e: e.tensor_tensor(sm.f(1, 4, 8), sm.f(1, 4, 0), sm.f(1, 4, 4), ALU.add), [sm.b], [sm.b])
                    V("dve", lambda e: e.tensor_copy(sm.f(1, 1, 16), mcar), [bMc, sm.b], [sm.b])
                    for c in range(4):
                        V("dve", lambda e: e.tensor_tensor(sm.f(1, 1, 12 + c), sm.f(1, 1, 16 + c), sm.f(1, 1, c), ALU.add),
                          [sm.b], [sm.b])
                        V("dve", lambda e: e.tensor_tensor(sm.f(1, 1, 12 + c), sm.f(1, 1, 12 + c), sm.f(1, 1, 8 + c),
                                                          ALU.max), [sm.b], [sm.b])
                        if c < 3:
                            V("dve", lambda e: e.tensor_copy(sm.f(1, 1, 17 + c), sm.f(1, 1, 12 + c)), [sm.b], [sm.b])
                    V("dve", lambda e: e.tensor_copy(mcar, sm.f(1, 1, 15)), [sm.b], [bMc])
                    V("dve", lambda e: e.tensor_tensor(sm.f(1, 4, 20), sm.f(1, 4, 0), sm.f(1, 4, 16), ALU.add), [sm.b], [sm.b])
                    V("dve", lambda e: e.tensor_tensor(sm.f(1, 4, 20), sm.f(1, 4, 20), sm.f(1, 4, 12), ALU.subtract), [sm.b],
                      [sm.b])
                    V("dve", lambda e: e.tensor_tensor(sm.f(1, 4, 24), sm.f(1, 4, 8), sm.f(1, 4, 12), ALU.subtract), [sm.b],
                      [sm.b])
                    V("act", lambda e: e.activation(out=sm.f(1, 8, 20), in_=sm.f(1, 8, 20), func=AF.Exp), [sm.b], [sm.b])
                    V("dve", lambda e: e.tensor_tensor(v3(il.f(1), 128), v3(bcum.f(1), 128),
                                                      sm.f(1, 4, 16).unsqueeze(2).to_broadcast([1, 4, 128]), ALU.add),
                      [bcum.b, sm.b], [il.b])
                    V("dve", lambda e: e.tensor_tensor(mt.f(1), il.f(1), mint.f(1), ALU.max), [il.b, mint.b], [mt.b])
                    V("dve", lambda e: e.tensor_tensor(mint.f(1), bcum.f(1), mt.f(1), ALU.subtract), [bcum.b, mt.b], [mint.b])
                    V("dve", lambda e: e.tensor_tensor(il.f(1), il.f(1), mt.f(1), ALU.subtract), [il.b, mt.b], [il.b])
                    V("dve", lambda e: e.tensor_scalar(mt.f(1), mt.f(1), -1.0, None, ALU.mult), [mt.b], [mt.b])
                    V("dve", lambda e: e.tensor_tensor(v3(cmx.f(1), 128), v3(g_.f(1), 128),
                                                      sm.f(1, 4, 4).unsqueeze(2).to_broadcast([1, 4, 128]), ALU.subtract),
                      [g_.b, sm.b], [cmx.b])
                    R(bcum, li, sp)
                    _ck(2)
                    onerow = C("onesf", 1)
                    pc_, bc_ = bank()
                    for c in range(NCH):
                        mm(pc_[:, 32 + 2 * c:34 + 2 * c], g_.f(1, 128, c * 128), onerow[:, 0:2], [g_.b, bC], [bc_])
                        mm(pc_[:, 40 + 2 * c:42 + 2 * c], cmx.f(1, 128, c * 128), onerow[:, 0:2], [cmx.b, bC], [bc_])
                    mm(pc_[:, 8:16], onerow[:, 0:128], sm.f(1, 8, 20), [sm.b, bC], [bc_])
                    mcol = G()
                    V("dve", lambda e: e.tensor_copy(mcol.f(128, 8, 8), pc_[:, 8:16]), [bc_], [mcol.b])
                    V("dve", lambda e: e.tensor_copy(mcol.f(128, 8, 0), pc_[:, 32:48:2]), [bc_], [mcol.b])
                    V("act", lambda e: e.activation(out=mcol.f(128, 4, 4), in_=mcol.f(128, 4, 4), func=AF.Exp), [mcol.b],
                      [mcol.b])
                    V("dve", lambda e: e.tensor_scalar(mcol.f(128, 4, 16), mcol.f(128, 4, 4), 128.0 ** -0.5, None, ALU.mult),
                      [mcol.b], [mcol.b])
                    R(g_, cmx, sm)
                    _ck(3)
                    pR1, bR1 = bank(True)
                    mm(pR1[:, :], onerow[:, 0:128], mint.f(1), [mint.b, bC], [bR1])
                    pR2, bR2 = bank()
                    mm(pR2[:, :], onerow[:, 0:128], il.f(1), [il.b, bC], [bR2])
                    wint, qw, emt = G(), G(), G()
                    V("act", lambda e: e.activation(out=wint.f(), in_=pR2[:, :], func=AF.Exp), [bR2], [wint.b])
                    V("dve", lambda e: e.tensor_tensor(qw.h(), q_b.h(), wint.f(), ALU.mult), [q_b.b, wint.b], [qw.b])
                    pR3, bR3 = bank()
                    mm(pR3[:, :], onerow[:, 0:128], mt.f(1), [mt.b, bC], [bR3])
                    V("act", lambda e: e.activation(out=emt.f(), in_=pR3[:, :], func=AF.Exp), [bR3], [emt.b])
                    R(wint, mint, il, mt)
                    _ck(4)
                    kwt = G()
                    pnum, bnum = bank(True)
                    pden, bden = bank(True)
                    for c in range(NCH):
                        cs = slice(c * 128, (c + 1) * 128)
                        pv, bv = bank()
                        mm(pv[:, 0:128], mxb.h(128, 128, c * 128), wv_b, [mxb.b, bWs], [bv])
                        mm(pv[:, 128:256], xc_b.h(128, 128, c * 128), wk_b, [xc_b.b, bWs], [bv])
                        V("act", lambda e: e.copy(vtok[:, c, 0:128], pv[:, 0:128]), [bv], [bVt])
                        V("dve", lambda e: e.tensor_scalar(kwt.h(128, 128, c * 128), pv[:, 128:256],
                                                          mcol.f(128, 1, 16 + c), None, ALU.mult), [bv, mcol.b],
                          [kwt.b])
                        _ck(6)
                        et, wm, qkw, tmpc = G(), G(), G(), G()
                        V("dve", lambda e: e.tensor_scalar(et.f(128, 128), pR1[:, cs], mcol.f(128, 1, c), 0.0, ALU.add,
                                                          ALU.min), [bR1, mcol.b], [et.b])
                        V("act", lambda e: e.activation(out=et.f(128, 128), in_=et.f(128, 128), func=AF.Exp), [et.b], [et.b])
                        V("pool", lambda e: e.tensor_tensor(wm.f(128, 128), et.f(128, 128), C("mask128"), ALU.mult),
                          [et.b, bC], [wm.b])
                        _ck(7)
                        pqk, bqk = bank()
                        mm(pqk[:, 0:128], k_b.h(128, 128, c * 128), q_b.h(128, 128, c * 128), [k_b.b, q_b.b], [bqk])
                        V("dve", lambda e: e.tensor_tensor(qkw.h(128, 128), pqk[:, 0:128], wm.f(128, 128), ALU.mult),
                          [bqk, wm.b], [qkw.b])
                        _ck(8)
                        mm(pnum[:, cs], vtok[:, c, 0:128], qkw.h(128, 128), [bVt, qkw.b], [bnum], True, False)
                        mm(pnum[:, cs], Cst_b[:, 0:128], qw.h(128, 128, c * 128), [bSmb, qw.b], [bnum], False, True)
                        mm(pden[:, cs], onesb, qkw.h(128, 128), [bCb, qkw.b], [bden], True, False)
                        mm(pden[:, cs], nbc[:, :], qw.h(128, 128, c * 128), [bNb, qw.b], [bden], False, True)
                        _ck(9)
                        pcl, bcl = bank()
                        mm(pcl[:, 0:160], kwt.h(128, 128, c * 128), vtok[:, c, 0:160], [kwt.b, bVt], [bcl])
                        V("dve", lambda e: e.tensor_scalar(tmpc.f(128, 129), pcl[:, 0:129], mcol.f(128, 1, 12 + c), None,
                                                          ALU.mult), [bcl, mcol.b], [tmpc.b])
                        V("dve", lambda e: e.scalar_tensor_tensor(out=Cst, in0=Cst, scalar=mcol.f(128, 1, 8 + c),
                                                                  in1=tmpc.f(128, 129), op0=ALU.mult, op1=ALU.add),
                          [bSm, mcol.b, tmpc.b], [bSm])
                        V("pool", lambda e: e.tensor_copy(nbc[:, :], Cst[:, 128:129].to_broadcast([128, 128])), [bSm],
                          [bNb])
                        V("act", lambda e: e.copy(Cst_b, Cst[:, 0:128]), [bSm], [bSmb])
                        R(et, wm, qkw, tmpc)
                    unkeep(bR1)
                    _ck(5)
                    R(kwt, mxb, xc_b, q_b, k_b, qw, mcol)
                    dsb, hh, sq, rs, yo = G(), G(), G(), G(), G()
                    V("act", lambda e: e.copy(dsb.f(), pden[:, :]), [bden], [dsb.b])
                    unkeep(bden)
                    V("dve", lambda e: e.scalar_tensor_tensor(out=dsb.f(), in0=dsb.f(), scalar=-1.0, in1=dsb.f(),
                                                              op0=ALU.mult, op1=ALU.max), [dsb.b], [dsb.b])
                    V("dve", lambda e: e.tensor_tensor(dsb.f(), dsb.f(), emt.f(), ALU.max), [dsb.b, emt.b], [dsb.b])
                    V("dve", lambda e: e.reciprocal(dsb.f(), dsb.f()), [dsb.b], [dsb.b])
                    V("dve", lambda e: e.tensor_tensor(hh.f(), pnum[:, :], dsb.f(), ALU.mult), [bnum, dsb.b], [hh.b])
                    unkeep(bnum)
                    V("pool", lambda e: e.tensor_tensor(hh.f(), hh.f(), osig.f(), ALU.mult), [hh.b, osig.b], [hh.b])
                    V("act", lambda e: e.activation(out=sq.h(), in_=hh.f(), func=AF.Square), [hh.b], [sq.b])
                    pss, bss = bank()
                    mm(pss[:, :], onesb, sq.h(), [bCb, sq.b], [bss])
                    V("act", lambda e: e.activation(out=rs.f(), in_=pss[:, :], func=AF.Sqrt, bias=EPS, scale=1.0 / 128),
                      [bss], [rs.b])
                    V("dve", lambda e: e.reciprocal(rs.f(), rs.f()), [rs.b], [rs.b])
                    V("dve", lambda e: e.scalar_tensor_tensor(out=yo.h(), in0=hh.f(), scalar=P("nm", 0), in1=rs.f(),
                                                              op0=ALU.mult, op1=ALU.mult), [hh.b, bPr, rs.b], [yo.b])
                    em.dma("sp", y_dst(256, 384, t), yo.h(), yo.b, reads=[yo.b])
                    R(dsb, hh, sq, rs, yo, emt, osig)
                except _Cut:
                    reserved.clear()

            if "rw" in STAGES:
                try:
                    NC8 = ST // 64

                    def shiftmix(r, M, mu_ap, omu_ap, eng="pool"):
                        o = G()
                        V(eng, lambda e: e.tensor_scalar(o.f(M), r.f(M, ST, 4), omu_ap, None, ALU.mult), [r.b, bPr], [o.b])
                        if eng == "dve":
                            V(eng, lambda e: e.scalar_tensor_tensor(out=o.f(M), in0=r.f(M, ST, 3), scalar=mu_ap, in1=o.f(M),
                                                                    op0=ALU.mult, op1=ALU.add), [r.b, bPr, o.b], [o.b])
                        else:
                            tm_ = G()
                            V(eng, lambda e: e.tensor_scalar(tm_.f(M), r.f(M, ST, 3), mu_ap, None, ALU.mult), [r.b, bPr],
                              [tm_.b])
                            V(eng, lambda e: e.tensor_tensor(o.f(M), o.f(M), tm_.f(M), ALU.add), [o.b, tm_.b], [o.b])
                            R(tm_)
                        R(r)
                        return o
                    r_s = shiftmix(raw["rr"], 128, P("mu3", 0), OMU(0))
                    k_s = shiftmix(raw["rk"], 128, P("mu3", 1), OMU(1), "dve")
                    v_s = shiftmix(raw["rv"], 128, P("mu3", 2), OMU(2))
                    wl_s = shiftmix(raw["rwl"], 64, P("mu2", 0, 64), OMU(3, 64), "dve")
                    al_s = shiftmix(raw["ral"], 64, P("mu2", 1, 64), OMU(4, 64))
                    gl_s = shiftmix(raw["rgl"], 128, P("mug", 0), OMU(5), "dve")
                    tw_b, al_b, sg_b = G(), G(), G()
                    V("act", lambda e: e.activation(out=tw_b.h(64), in_=wl_s.f(64), func=AF.Tanh), [wl_s.b], [tw_b.b])
                    V("act", lambda e: e.copy(al_b.h(64), al_s.f(64)), [al_s.b], [al_b.b])
                    V("act", lambda e: e.activation(out=sg_b.h(), in_=gl_s.f(), func=AF.Sigmoid), [gl_s.b], [sg_b.b])
                    R(wl_s, al_s, gl_s)
                    _ck(11)
                    sgm, a_f, g_f = G(), G(), G()
                    pw, bw = bank()
                    mm(pw[:, :], wup_b, tw_b.h(64), [bWs, tw_b.b], [bw])
                    V("act", lambda e: e.activation(out=sgm.f(), in_=pw[:, :], func=AF.Sigmoid, bias=P("w0", 0)), [bw, bPr],
                      [sgm.b])
                    pa_, ba_ = bank()
                    mm(pa_[:, :], aup_b, al_b.h(64), [bWs, al_b.b], [ba_])
                    V("act", lambda e: e.activation(out=a_f.f(), in_=pa_[:, :], func=AF.Sigmoid, bias=P("a0", 0)), [ba_, bPr],
                      [a_f.b])
                    pg, bg = bank()
                    mm(pg[:, :], gup_b, sg_b.h(), [bWs, sg_b.b], [bg])
                    V("act", lambda e: e.copy(g_f.f(), pg[:, :]), [bg], [g_f.b])
                    R(tw_b, al_b, sg_b)
                    _ck(12)
                    kkf, sq, nrm = G(), G(), G()
                    V("dve", lambda e: e.tensor_scalar(kkf.f(), k_s.f(), P("kk", 0), None, ALU.mult), [k_s.b, bPr], [kkf.b])
                    V("act", lambda e: e.activation(out=sq.h(), in_=kkf.f(), func=AF.Square), [kkf.b], [sq.b])
                    pss, bss = bank()
                    mm(pss[:, :], blkb, sq.h(), [bCb, sq.b], [bss])
                    V("act", lambda e: e.activation(out=nrm.f(), in_=pss[:, :], func=AF.Sqrt), [bss], [nrm.b])
                    V("dve", lambda e: e.tensor_scalar(nrm.f(), nrm.f(), 1e-6, None, ALU.max), [nrm.b], [nrm.b])
                    V("dve", lambda e: e.reciprocal(nrm.f(), nrm.f()), [nrm.b], [nrm.b])
                    V("dve", lambda e: e.tensor_tensor(kkf.f(), kkf.f(), nrm.f(), ALU.mult), [kkf.b, nrm.b], [kkf.b])
                    R(sq, nrm)
                    _ck(13)
                    kp, beta, csg, cx = G(), G(), G(), G()
                    V("dve", lambda e: e.tensor_scalar(kp.f(), a_f.f(), -1.0, P("ka", 0), ALU.add, ALU.mult), [a_f.b, bPr],
                      [kp.b])
                    V("dve", lambda e: e.scalar_tensor_tensor(out=kp.f(), in0=kp.f(), scalar=1.0, in1=k_s.f(), op0=ALU.add,
                                                              op1=ALU.mult), [kp.b, k_s.b], [kp.b])
                    V("pool", lambda e: e.tensor_tensor(beta.f(), kkf.f(), a_f.f(), ALU.mult), [kkf.b, a_f.b], [beta.b])
                    V("dve", lambda e: e.tensor_tensor_scan(csg.f(), C("m64"), sgm.f(), 0.0, ALU.mult, ALU.add), [sgm.b, bC],
                      [csg.b])
                    V("pool", lambda e: e.tensor_tensor(cx.f(), csg.f(), sgm.f(), ALU.subtract), [csg.b, sgm.b], [cx.b])
                    eP, eN, ePx = G(), G(), G()
                    V("act", lambda e: e.activation(out=eP.f(), in_=csg.f(), func=AF.Exp, scale=-KDEC), [csg.b], [eP.b])
                    V("act", lambda e: e.activation(out=eN.f(), in_=csg.f(), func=AF.Exp, scale=KDEC), [csg.b], [eN.b])
                    V("act", lambda e: e.activation(out=ePx.f(), in_=cx.f(), func=AF.Exp, scale=-KDEC), [cx.b], [ePx.b])
                    KR, Bc_b, Kc_b, v_b, pl = G(), G(), G(), G(), G()
                    KRv = KR.h(128, 1024).rearrange("p (c two l) -> p c two l", two=2, l=64)
                    V("dve", lambda e: e.tensor_tensor(KRv[:, :, 0, :], v3(kkf.f(), 64), v3(ePx.f(), 64), ALU.mult),
                      [kkf.b, ePx.b], [KR.b])
                    V("dve", lambda e: e.tensor_tensor(KRv[:, :, 1, :], v3(r_s.f(), 64), v3(eP.f(), 64), ALU.mult),
                      [r_s.b, eP.b], [KR.b])
                    V("pool", lambda e: e.tensor_tensor(Bc_b.h(), beta.f(), eN.f(), ALU.mult), [beta.b, eN.b], [Bc_b.b])
                    V("dve", lambda e: e.tensor_tensor(Kc_b.h(), kp.f(), eN.f(), ALU.mult), [kp.b, eN.b], [Kc_b.b])
                    V("act", lambda e: e.copy(v_b.h(), v_s.f()), [v_s.b], [v_b.b])
                    V("dve", lambda e: e.tensor_copy(pl.f(128, 8), eP.t[:, 63:512:64]), [eP.b], [pl.b])
                    R(kkf, a_f, k_s, beta, csg, cx, sgm, eP, eN, ePx)
                    _ck(14)
                    TK = [G(), G()]
                    for j2 in range(2):
                        px, bx = bank()
                        pxb = px[:, :].bitcast(BF16)
                        for cc in range(4):
                            c = 4 * j2 + cc
                            srcs = [(KR, c * 128), (Bc_b, c * 64), (Kc_b, c * 64), (v_b, c * 64)]
                            for q in range(4):
                                for hd in range(2):
                                    p0 = 64 * hd
                                    V("pe", lambda e: e.transpose(
                                        pxb[p0:p0 + 64, cc * 256 + q * 64:cc * 256 + (q + 1) * 64],
                                        srcs[q][0].h(64, 64, srcs[q][1], p0), identb[p0:p0 + 64, p0:p0 + 64]),
                                        [srcs[q][0].b, bCb], [bx])
                        if j2 % 2:
                            V("act", lambda e: e.copy(TK[j2].h(128, 1024), pxb[:, :]), [bx], [TK[j2].b])
                        else:
                            V("dve", lambda e: e.tensor_copy(TK[j2].h(128, 1024), pxb[:, :]), [bx], [TK[j2].b])
                    R(v_b)
                    _ck(15)

                    def tk(c, q, p0):
                        return TK[c // 4].h(64, 64, (c % 4) * 256 + q * 64, p0)
                    AB = [G(), G()]
                    Xa, XTa, Xb, XTb, TTm = G(), G(), G(), G(), G()
                    pA2, bA2 = bank(True)
                    for j2 in range(4):
                        pA, bA = bank()
                        for cc in range(2):
                            c = 2 * j2 + cc
                            for hd in range(2):
                                p0 = 64 * hd
                                krhs = KR.h(64, 128, c * 128, p0)
                                mm(pA[p0:p0 + 64, cc * 256:cc * 256 + 128], Bc_b.h(64, 64, c * 64, p0), krhs,
                                   [Bc_b.b, KR.b], [bA])
                                mm(pA[p0:p0 + 64, cc * 256 + 128:cc * 256 + 256], Kc_b.h(64, 64, c * 64, p0), krhs,
                                   [Kc_b.b, KR.b], [bA])
                                mm(pA2[p0:p0 + 64, c * 64:(c + 1) * 64], KR.h(64, 64, c * 128, p0),
                                   Bc_b.h(64, 64, c * 64, p0), [KR.b, Bc_b.b], [bA2])
                        abt = AB[j2 // 2]
                        V("dve", lambda e: e.tensor_tensor(
                            v3(abt.h(128, 512, (j2 % 2) * 512), 256), v3(pA[:, :], 256),
                            C("maskAB")[:, 0:256].unsqueeze(1).to_broadcast([128, 2, 256]), ALU.mult), [bA, bC],
                            [abt.b])
                    V("dve", lambda e: e.tensor_tensor(v3(Xa.h(128, 512), 64), v3(pA2[:, :], 64),
                                                      C("nmasklo")[:, 0:64].unsqueeze(1).to_broadcast([128, 8, 64]),
                                                      ALU.mult), [bA2, bC], [Xa.b])
                    unkeep(bA2)
                    for j2 in range(2):
                        V("pool", lambda e: e.tensor_scalar(
                            v3(XTa.h(128, 256, j2 * 256), 64),
                            AB[j2].h(128, 1024).rearrange("p (m w) -> p m w", w=256)[:, :, 0:64], -1.0, None,
                            ALU.mult), [AB[j2].b], [XTa.b])
                    V("pool", lambda e: e.tensor_tensor(
                        v3(TTm.h(128, 512), 64), v3(XTa.h(128, 512), 64),
                        cb[:, 384:448].unsqueeze(1).to_broadcast([128, 8, 64]), ALU.add), [XTa.b, bCb], [TTm.b])

                    def ab(c, w, p0):
                        return AB[c // 4].h(64, 64, (c % 4) * 256 + w * 64, p0)
                    _ck(16)
                    Xc, XTc, Xn, XTn = Xa, XTa, Xb, XTb
                    for lvl in range(1, 6):
                        for b4 in range(2):
                            pX, bX_ = bank()
                            for m in range(4):
                                mo_ = (b4 * 4 + m) * 64
                                for hd in range(2):
                                    p0 = 64 * hd
                                    mm(pX[p0:p0 + 64, m * 128:m * 128 + 64], XTc.h(64, 64, mo_, p0),
                                       Xc.h(64, 64, mo_, p0), [XTc.b, Xc.b], [bX_])
                                    if lvl < 5:
                                        mm(pX[p0:p0 + 64, m * 128 + 64:m * 128 + 128], Xc.h(64, 64, mo_, p0),
                                           XTc.h(64, 64, mo_, p0), [XTc.b, Xc.b], [bX_])
                            pXv = v3(pX[:, :], 128)
                            V("act", lambda e: e.copy(v3(Xn.h(128, 256, b4 * 256), 64), pXv[:, :, 0:64]), [bX_],
                              [Xn.b])
                            if lvl < 5:
                                V("dve", lambda e: e.tensor_copy(v3(XTn.h(128, 256, b4 * 256), 64), pXv[:, :, 64:128]),
                                  [bX_], [XTn.b])
                            pT, bT = bank()
                            for m in range(4):
                                mo_ = (b4 * 4 + m) * 64
                                for hd in range(2):
                                    p0 = 64 * hd
                                    mm(pT[p0:p0 + 64, m * 64:(m + 1) * 64], Xn.h(64, 64, mo_, p0),
                                       TTm.h(64, 64, mo_, p0), [Xn.b, TTm.b], [bT])
                            V("dve", lambda e: e.tensor_tensor(TTm.h(128, 256, b4 * 256), TTm.h(128, 256, b4 * 256),
                                                              pT[:, 0:256], ALU.add), [bT, TTm.b], [TTm.b])
                        Xc, XTc, Xn, XTn = Xn, XTn, Xc, XTc
                    R(Xa, XTa, Xb, XTb)
                    _ck(17)
                    AkV, WU = G(), G()
                    pK, bK = bank()
                    for c in range(NC8):
                        for hd in range(2):
                            p0 = 64 * hd
                            mm(pK[p0:p0 + 64, c * 64:(c + 1) * 64], ab(c, 2, p0), tk(c, 3, p0),
                               [AB[c // 4].b, TK[c // 4].b], [bK])
                    V("act", lambda e: e.copy(AkV.h(128, 512), pK[:, :]), [bK], [AkV.b])
                    for b4 in range(2):
                        pW, bW_ = bank()
                        for cc in range(4):
                            c = b4 * 4 + cc
                            for hd in range(2):
                                p0 = 64 * hd
                                mm(pW[p0:p0 + 64, cc * 128:cc * 128 + 64], TTm.h(64, 64, c * 64, p0), tk(c, 0, p0),
                                   [TTm.b, TK[c // 4].b], [bW_])
                                mm(pW[p0:p0 + 64, cc * 128 + 64:cc * 128 + 128], TTm.h(64, 64, c * 64, p0),
                                   AkV.h(64, 64, c * 64, p0), [TTm.b, AkV.b], [bW_])
                        V("dve", lambda e: e.tensor_tensor(v3(WU.h(128, 512, b4 * 512), 128), v3(pW[:, :], 128),
                                                          C("signm").unsqueeze(1).to_broadcast([128, 4, 128]),
                                                          ALU.mult), [bW_, bC], [WU.b])
                    R(AkV, TTm)
                    _ck(18)

                    def wu(c, w, p0):
                        return WU.h(64, 64, c * 128 + w * 64, p0)
                    G0T, Hpp, QT = G(), G(), G()
                    pG, bG_ = bank()
                    pH, bH_ = bank()
                    pQ, bQ_ = bank()
                    for c in range(NC8):
                        cs = slice(c * 64, (c + 1) * 64)
                        for hd in range(2):
                            p0 = 64 * hd
                            mm(pG[p0:p0 + 64, cs], wu(c, 0, p0), tk(c, 1, p0), [WU.b, TK[c // 4].b], [bG_])
                            mm(pH[p0:p0 + 64, cs], tk(c, 1, p0), wu(c, 1, p0), [WU.b, TK[c // 4].b], [bH_], True,
                               False)
                            mm(pH[p0:p0 + 64, cs], tk(c, 2, p0), tk(c, 3, p0), [TK[c // 4].b], [bH_], False, True)
                            mm(pQ[p0:p0 + 64, cs], wu(c, 0, p0), ab(c, 1, p0), [WU.b, AB[c // 4].b], [bQ_])
                    V("dve", lambda e: e.tensor_tensor(v3(G0T.f(), 64),
                                                      iden2[:, :].unsqueeze(1).to_broadcast([128, NC8, 64]),
                                                      v3(pG[:, :], 64), ALU.subtract), [bG_, bI2], [G0T.b])
                    V("dve", lambda e: e.tensor_tensor(v3(Hpp.f(), 64), v3(pH[:, :], 64),
                                                      pl.f(128, 8).unsqueeze(2).to_broadcast([128, NC8, 64]),
                                                      ALU.mult), [bH_, pl.b], [Hpp.b])
                    V("dve", lambda e: e.tensor_tensor(v3(QT.h(), 64), KRv[:, :, 1, :], v3(pQ[:, :], 64),
                                                      ALU.subtract), [bQ_, KR.b], [QT.b])
                    _ck(19)
                    pY, bY_ = bank(True)
                    for c in range(NC8):
                        cs = slice(c * 64, (c + 1) * 64)
                        for hd in range(2):
                            p0 = 64 * hd
                            mm(pY[p0:p0 + 64, cs], Srw_b[p0:p0 + 64, :], QT.h(64, 64, c * 64, p0), [bSrb, QT.b],
                               [bY_], True, False)
                            mm(pY[p0:p0 + 64, cs], wu(c, 1, p0), ab(c, 1, p0), [WU.b, AB[c // 4].b], [bY_], False,
                               False)
                            mm(pY[p0:p0 + 64, cs], tk(c, 3, p0), ab(c, 3, p0), [TK[c // 4].b, AB[c // 4].b], [bY_],
                               False, True)
                        pS, bS_ = bank()
                        for hd in range(2):
                            p0 = 64 * hd
                            mm(pS[p0:p0 + 64, 0:64], G0T.f(64, 64, c * 64, p0), Srw[p0:p0 + 64, :], [G0T.b, bSr],
                               [bS_])
                        V("dve", lambda e: e.scalar_tensor_tensor(out=Srw, in0=pS[:, 0:64], scalar=pl.f(128, 1, c),
                                                                  in1=Hpp.f(128, 64, c * 64), op0=ALU.mult,
                                                                  op1=ALU.add), [bS_, pl.b, Hpp.b, bSr], [bSr])
                        V("act", lambda e: e.copy(Srw_b, Srw), [bSr], [bSrb])
                    R(G0T, Hpp, QT, KR, Bc_b, Kc_b, pl, WU, *TK, *AB)
                    _ck(20)
                    y_f, y_b, sq, rs, rk_, rkr, yo = G(), G(), G(), G(), G(), G(), G()
                    V("dve", lambda e: e.tensor_copy(y_f.f(), pY[:, :]), [bY_], [y_f.b])
                    V("act", lambda e: e.copy(y_b.h(), pY[:, :]), [bY_], [y_b.b])
                    unkeep(bY_)
                    pm, bm = bank()
                    mm(pm[:, :], blkb, y_b.h(), [bCb, y_b.b], [bm])
                    V("dve", lambda e: e.scalar_tensor_tensor(out=y_f.f(), in0=pm[:, :], scalar=-1.0 / 64, in1=y_f.f(),
                                                              op0=ALU.mult, op1=ALU.add), [bm, y_f.b], [y_f.b])
                    V("act", lambda e: e.activation(out=sq.h(), in_=y_f.f(), func=AF.Square), [y_f.b], [sq.b])
                    pv_, bv_ = bank()
                    mm(pv_[:, :], blkb, sq.h(), [bCb, sq.b], [bv_])
                    V("act", lambda e: e.activation(out=rs.f(), in_=pv_[:, :], func=AF.Sqrt, bias=64e-5, scale=1.0 / 64),
                      [bv_], [rs.b])
                    V("dve", lambda e: e.reciprocal(rs.f(), rs.f()), [rs.b], [rs.b])
                    V("dve", lambda e: e.tensor_tensor(y_f.f(), y_f.f(), rs.f(), ALU.mult), [y_f.b, rs.b], [y_f.b])
                    V("dve", lambda e: e.tensor_scalar(y_f.f(), y_f.f(), P("lnw", 0), P("lnb", 0), ALU.mult, ALU.add),
                      [y_f.b, bPr], [y_f.b])
                    V("pool", lambda e: e.tensor_tensor(rk_.f(), r_s.f(), kp.f(), ALU.mult), [r_s.b, kp.b], [rk_.b])
                    V("pool", lambda e: e.tensor_scalar(rkr.h(), rk_.f(), P("rkc", 0), None, ALU.mult), [rk_.b, bPr], [rkr.b])
                    pb_, bb_ = bank()
                    mm(pb_[:, :], blkb, rkr.h(), [bCb, rkr.b], [bb_])
                    V("dve", lambda e: e.tensor_tensor(rk_.f(), pb_[:, :], v_s.f(), ALU.mult), [bb_, v_s.b], [rk_.b])
                    V("dve", lambda e: e.tensor_tensor(y_f.f(), y_f.f(), rk_.f(), ALU.add), [y_f.b, rk_.b], [y_f.b])
                    V("dve", lambda e: e.tensor_tensor(yo.h(), y_f.f(), g_f.f(), ALU.mult), [y_f.b, g_f.b], [yo.b])
                    em.dma("sp", y_dst(384, 512, t), yo.h(), yo.b, reads=[yo.b])
                    R(y_f, y_b, sq, rs, rk_, rkr, yo, r_s, v_s, kp, g_f)
                except _Cut:
                    reserved.clear()

            if len(STAGES) == 3:
                assert not live, [g.b.name for g in live]
            R(*list(live))
        em.wait_bufs("sp", [g.b for g in gt])
        if ctx is not None:
            em.barrier()
            em.end_phase()
            em.pes = None
    return nc


def _lay8(g):
    return np.ascontiguousarray(np.asarray(g, np.float32).reshape(8, 128).T)


def _mix_core_inputs(p, l, q):
    g = q // 2
    hs = [4 * q + i for i in range(4)]
    cols = []
    for h in hs:
        cols += list(range(64 * h, 64 * h + 64))
    for h in hs:
        cols += list(range(1024 + 64 * h, 1024 + 64 * h + 64))
    cols += list(range(2048 + 64 * g, 2048 + 64 * g + 64))
    cols += list(range(2176 + 64 * g, 2176 + 64 * g + 64))
    cols += [2304 + h for h in hs]
    cols += list(range(2320 + 128 * q, 2320 + 128 * q + 128))
    cols += list(range(2832 + 128 * q, 2832 + 128 * q + 128))
    cols += [3344 + q, 3348 + q]
    for base in (3352, 3864, 4376):
        cols += list(range(base + 128 * q, base + 128 * q + 128))
    cols += list(range(4888, 5144))
    assert len(cols) == NCOL
    w_sel = np.ascontiguousarray(p["w_in"][l][:, cols])
    pro, prtot = _prm_layout()
    prm = np.zeros((128, prtot), np.float32)

    def put(n, a, j=0):
        a = np.asarray(a, np.float32)
        if a.ndim == 1:
            a = a[:, None]
        o, w = pro[n]
        prm[0:a.shape[0], o + j:o + j + a.shape[1]] = a
    cw, cbv = p["ssd_conv_w"][l], p["ssd_conv_b"][l]
    for i in range(2):
        ch = list(range(64 * hs[2 * i], 64 * hs[2 * i] + 64)) + list(range(64 * hs[2 * i + 1], 64 * hs[2 * i + 1] + 64))
        put("cwx", cw[:, ch].T, 4 * i)
        put("cbx", cbv[ch], i)
        put("Dcol", np.repeat(p["ssd_d"][l][[hs[2 * i], hs[2 * i + 1]]], 64), i)
    chB = list(range(1024 + 64 * g, 1024 + 64 * g + 64))
    chC = list(range(1152 + 64 * g, 1152 + 64 * g + 64))
    put("cwB", cw[:, chB].T)
    put("cbB", cbv[chB])
    put("cwC", cw[:, chC].T)
    put("cbC", cbv[chC])
    put("dtb", p["ssd_dt_bias"][l][hs])
    put("alog", p["ssd_a_log"][l][hs])
    sl = slice(128 * q, 128 * q + 128)
    put("cwm", p["mlstm_conv_w"][l][:, sl].T)
    put("cbm", p["mlstm_conv_b"][l][sl])
    put("bi", p["mlstm_gate_bias"][l][q:q + 1])
    put("bf", p["mlstm_gate_bias"][l][4 + q:5 + q])
    put("nm", p["mlstm_norm"][l][sl])
    mu = p["rwkv_shift_mu"][l]
    for j, base in enumerate((0, 512, 1024)):
        put("mu3", mu[base + 128 * q:base + 128 * q + 128], j)
    put("mu2", mu[1536:1600], 0)
    put("mu2", mu[1600:1664], 1)
    put("mug", mu[1664:1792])
    put("w0", p["rwkv_w0"][l][sl])
    put("a0", p["rwkv_a0"][l][sl])
    put("kk", p["rwkv_k_k"][l][sl])
    put("ka", p["rwkv_k_a"][l][sl])
    put("rkc", p["rwkv_r_k"][l].reshape(-1)[sl])
    put("lnw", p["rwkv_ln_w"][l][sl])
    put("lnb", p["rwkv_ln_b"][l][sl])
    wsm = np.zeros((128, 768), np.float32)
    wsm[:, 0:128] = p["mlstm_wq"][l][q]
    wsm[:, 128:256] = p["mlstm_wk"][l][q]
    wsm[:, 256:384] = p["mlstm_wv"][l][q]
    wsm[0:64, 384:512] = p["rwkv_w_up"][l][:, sl]
    wsm[0:64, 512:640] = p["rwkv_a_up"][l][:, sl]
    wsm[:, 640:768] = p["rwkv_g_up"][l][:, sl]
    return {"w_sel": w_sel, "g_mix": _lay8(p["mix_norm"][l]), "prm": prm, "wsm": wsm}


def build_all(T, NTOK, TTK=256):
    nc = bass.Bass("TRN2", target_bir_lowering=False)
    D = D_MODEL
    L = DEPTH
    cfo, cftot = _cf_layout()
    pro, prtot = _prm_layout()

    def din(name, shape, dt=F32):
        return nc.dram_tensor(name, list(shape), dt, kind="ExternalInput").ap()
    x_in = din("x_in", [D, NTOK])
    wgu1 = din("wgu1", [L, D, 2 * D_FF])
    wd1 = din("wd1", [L, D_FF, D])
    gn1 = din("gn1", [L, 128, 8])
    wgu2 = din("wgu2", [L, D, 2 * D_FF])
    wd2 = din("wd2", [L, D_FF, D])
    gn2 = din("gn2", [L, 128, 8])
    w_o = din("w_o", [L, 2048, D])
    g_y = din("g_y", [L, 128, 8])
    g_f = din("g_f", [128, 8])
    w_sel = din("w_sel", [L, D, NCOL])
    g_mix = din("g_mix", [L, 128, 8])
    prm = din("prm", [L, 128, prtot])
    wsm = din("wsm", [L, 128, 768])
    cf = din("cf", [128, cftot])
    o_fin = nc.dram_tensor("o_fin", [D, NTOK], F32, kind="ExternalOutput").ap()
    x_res = nc.dram_tensor("x_res", [D, NTOK], F32).ap()
    h_loc = nc.dram_tensor("h_loc", [D, NTOK], BF16).ap()
    h_all = nc.dram_tensor("h_all", [8 * 512, NTOK], BF16).ap()
    y_loc = nc.dram_tensor("y_loc", [4 * 512, NTOK], BF16).ap()
    y_all = nc.dram_tensor("y_all", [16 * 512, NTOK], BF16).ap()
    y_mine = nc.dram_tensor("y_mine", [2048, NTOK], BF16).ap()
    groups = [[0, 1, 2, 3], [4, 5, 6, 7]]
    SPQ = NTOK // ST

    es = ExitStack()
    with es:
        em = Em(nc, es)
        banks = [em.psum("pb%d" % i, [128, 512], F32) for i in range(8)]
        bbanks = [Buf("pb%d" % i, True) for i in range(8)]
        cc_sem = es.enter_context(nc.semaphore("cc_sem"))
        cc = [cc_sem, 0]
        em.extra.append(cc)
        base = {"nc": nc, "em": em, "banks": banks, "bbanks": bbanks}

        def gather(src, dst, nblk):
            em.barrier()
            for j in range(nblk):
                nc.gpsimd.collective_compute("AllGather", ALU.bypass, replica_groups=groups,
                                             ins=[src[j * 128:(j + 1) * 128, :]],
                                             outs=[dst[j * 512:(j + 1) * 512, :]]).then_inc(cc_sem, 1)
                cc[1] += 1
            em.extra[0] = (cc_sem, cc[1])
            em.barrier()

        sqv = nc.sync.snap(nc.sync.partition_id() % 4, min_val=0, max_val=3)
        bYm = Buf("y_mine")

        def select_y():
            src = y_all.rearrange("(s m) n -> s m n", s=4)[bass.ds(sqv, 1), :, :]
            em.dma("sp", y_mine.rearrange("(o m) n -> o m n", o=1), src, bYm, writes=[bYm])

        def y_load(em_, yt, bY, i):
            cs_ = slice(i * TTK, (i + 1) * TTK)
            for kc, dst in ((0, yt[:, 0:8:2, :]), (1, yt[:, 1:8:2, :]), (2, yt[:, 8:12, :]), (3, yt[:, 12:16, :])):
                em_.dma("sp", dst, y_mine[kc * 512:(kc + 1) * 512, cs_].rearrange("(r p) n -> p r n", p=128), bY,
                        reads=[bYm], writes=[bY])

        def y_dst(r0, r1, t):
            sq_, tt = t // SPQ, t % SPQ
            return y_loc[sq_ * 512 + r0:sq_ * 512 + r1, tt * ST:(tt + 1) * ST]

        def h_src(t):
            r, tt = t // SPQ, t % SPQ
            return h_all.rearrange("(c r p) n -> p c r n", c=8, r=4)[:, :, r, tt * ST:(tt + 1) * ST]

        em.extra[0] = (cc_sem, 0)
        em.prefix = "t0_"
        build_tok(NTOK, TTK, False, 1, True, False,
                  dict(base, x_in=x_in, x_out=x_res, wgu=[wgu1[0]], wd=[wd1[0]], gn=[gn1[0]], h_out=h_loc))
        for l in range(L):
            gather(h_loc, h_all, 8)
            em.prefix = "m%d_" % l
            build_mix(T, dict(base, w_sel=w_sel[l], g_mix=g_mix[l], cf=cf, prm=prm[l], wsm=wsm[l], y_out=y_loc,
                              h_src=h_src, y_dst=y_dst))
            gather(y_loc, y_all, 16)
            select_y()
            em.prefix = "t%d_" % (l + 1)
            if l < L - 1:
                build_tok(NTOK, TTK, True, 2, True, False,
                          dict(base, x_in=x_res, x_out=x_res, y_load=y_load, w_o=w_o[l], g_y=g_y[l],
                               wgu=[wgu2[l], wgu1[l + 1]], wd=[wd2[l], wd1[l + 1]], gn=[gn2[l], gn1[l + 1]],
                               h_out=h_loc))
            else:
                build_tok(NTOK, TTK, True, 1, False, True,
                          dict(base, x_in=x_res, x_out=x_res, y_load=y_load, w_o=w_o[l], g_y=g_y[l],
                               wgu=[wgu2[l]], wd=[wd2[l]], gn=[gn2[l]], g_f=g_f, o_fin=o_fin))
        print("[build_all] instructions:", em.nins, "dma sems:", em.nsem)
    return nc


_PROGS = {}


def _prog(key, fn):
    if key not in _PROGS:
        _PROGS[key] = fn()
    return _PROGS[key]


def _run(nc, in_maps):
    return run_bass_kernel_spmd(nc, in_maps, core_ids=list(range(8))).results


def kernel(**inp):
    p = {k: np.asarray(v) for k, v in inp.items()}
    x = p["x"].astype(np.float32, copy=False)
    B, T, D = x.shape
    NTOK = B * T // 8
    xflat = x.reshape(B * T, D)
    nc = _prog(("all", T, NTOK), lambda: build_all(T, NTOK))
    f32 = lambda a: np.ascontiguousarray(a, dtype=np.float32)
    lay = lambda a: np.ascontiguousarray(np.stack([_lay8(a[l]) for l in range(DEPTH)]))
    shared = {"wgu1": f32(p["ffn1_w_gate_up"]), "wd1": f32(p["ffn1_w_down"]), "gn1": lay(p["ffn1_norm"]),
              "wgu2": f32(p["ffn2_w_gate_up"]), "wd2": f32(p["ffn2_w_down"]), "gn2": lay(p["ffn2_norm"]),
              "w_o": f32(p["w_out"]), "g_y": lay(p["ssd_norm"]), "g_f": _lay8(p["final_norm"]),
              "g_mix": lay(p["mix_norm"]), "cf": mix_consts()}
    perq = []
    for q in range(4):
        ds_ = [_mix_core_inputs(p, l, q) for l in range(DEPTH)]
        perq.append({k: np.ascontiguousarray(np.stack([d[k] for d in ds_])) for k in ("w_sel", "prm", "wsm")})
    im = []
    for c in range(8):
        d = dict(shared)
        d.update(perq[c % 4])
        d["x_in"] = np.ascontiguousarray(xflat[c * NTOK:(c + 1) * NTOK].T)
        im.append(d)
    res = _run(nc, im)
    out = np.concatenate([np.asarray(r["o_fin"]).T for r in res], axis=0).reshape(B, T, D)
    return np.ascontiguousarray(out, dtype=np.float32)
```

```python
import numpy as np
import ml_dtypes
from contextlib import ExitStack
import concourse.bass as bass
import concourse.mybir as mybir
from concourse.bass_utils import run_bass_kernel_spmd

F32 = mybir.dt.float32
BF16 = mybir.dt.bfloat16
AF = mybir.ActivationFunctionType
ALU = mybir.AluOpType

D_MODEL = 1024
DEPTH = 4
D_FF = 2816
EPS = 1e-6
WQ = "sp"


class Buf:
    __slots__ = ("name", "w", "r", "dsem", "dcnt", "excl")

    def __init__(self, name, excl=False):
        self.name = name
        self.excl = excl
        self.w = None
        self.r = {}
        self.dsem = None
        self.dcnt = 0


class Em:
    def __init__(self, nc, es):
        self.nc = nc
        self.es = es
        self.eng = {"pe": nc.tensor, "act": nc.scalar, "dve": nc.vector, "pool": nc.gpsimd, "sp": nc.sync}
        self.sem = {k: es.enter_context(nc.semaphore("s_" + k)) for k in ["pe", "act", "dve", "pool"]}
        self.cnt = {k: 0 for k in self.sem}
        self.known = {}
        self.nsem = 0
        self.nins = 0
        self.pes = None
        self.prefix = ""
        self.sempool = []
        self.dma_bufs = []
        self.extra = []

    def sbuf(self, name, shape, dt):
        st = self.pes if self.pes is not None else self.es
        return st.enter_context(self.nc.sbuf_tensor(self.prefix + name, list(shape), dt))

    def barrier(self):
        evs = [(self.sem[k], self.cnt[k]) for k in self.sem if self.cnt[k] > 0]
        evs += [(b.dsem, b.dcnt) for b in self.dma_bufs]
        evs += [(s_, c_) for (s_, c_) in self.extra if c_ > 0]
        for e in self.eng:
            self._wait(e, evs)

    def end_phase(self):
        for b in self.dma_bufs:
            self.sempool.append((b.dsem, b.dcnt))
            b.dsem = None
        self.dma_bufs = []

    def psum(self, name, shape, dt=F32):
        return self.es.enter_context(self.nc.psum_tensor(name, list(shape), dt))

    def _deps(self, reads, writes):
        evs = []
        for b in reads:
            if b.w is not None:
                evs.append(b.w)
            if b.excl:
                evs.extend(b.r.values())
        for b in writes:
            if b.w is not None:
                evs.append(b.w)
            evs.extend(b.r.values())
        return evs

    def _wait(self, e, evs):
        need = {}
        for (s, v) in evs:
            k = id(s)
            if v > self.known.get((e, k), 0):
                if k not in need or need[k][1] < v:
                    need[k] = (s, v)
        for k, (s, v) in need.items():
            self.eng[e].wait_ge(s, v)
            self.known[(e, k)] = v

    def _mark(self, ev, reads, writes):
        k = id(ev[0])
        for b in reads:
            b.r[k] = ev
        for b in writes:
            b.w = ev
            b.r = {}

    def op(self, e, fn, reads=(), writes=()):
        evs = self._deps(reads, writes)
        if e == "pe":
            evs = [ev for ev in evs if ev[0] is not self.sem["pe"]]
        self._wait(e, evs)
        ins = fn(self.eng[e])
        self.cnt[e] += 1
        ins.then_inc(self.sem[e], 1)
        self._mark((self.sem[e], self.cnt[e]), reads, writes)
        self.nins += 1

    def dma(self, q, out, in_, prim, reads=(), writes=()):
        evs = self._deps(reads, writes)
        self._wait(q, evs)
        if prim.dsem is None:
            if self.sempool:
                prim.dsem, prim.dcnt = self.sempool.pop()
            else:
                prim.dsem = self.es.enter_context(self.nc.semaphore("d%d" % self.nsem))
                prim.dcnt = 0
                self.nsem += 1
            self.dma_bufs.append(prim)
        prim.dcnt += 16
        self.eng[q].dma_start(out=out, in_=in_).then_inc(prim.dsem, 16)
        self._mark((prim.dsem, prim.dcnt), reads, writes)
        self.nins += 1

    def wait_bufs(self, q, bufs):
        evs = []
        for b in bufs:
            if b.w is not None:
                evs.append(b.w)
            evs.extend(b.r.values())
        self._wait(q, evs)


def build_tok(ntok, TT, has_y, n_ffn, out_h, out_final, ctx=None):
    NT = ntok // TT
    D, KC, FC = D_MODEL, 8, D_FF // 128
    if ctx is None:
        nc = bass.Bass("TRN2", target_bir_lowering=False)
        x_in = nc.dram_tensor("x_in", [D, ntok], F32, kind="ExternalInput").ap()
        x_out = nc.dram_tensor("x_out", [D, ntok], F32, kind="ExternalOutput").ap()
        y_load = None
        if has_y:
            y_in = nc.dram_tensor("y_in", [2048, ntok], BF16, kind="ExternalInput").ap()
            w_o = nc.dram_tensor("w_o", [2048, D], F32, kind="ExternalInput").ap()
            g_y = nc.dram_tensor("g_y", [128, 8], F32, kind="ExternalInput").ap()
        wgu, wd, gn = [], [], []
        for i in range(n_ffn):
            wgu.append(nc.dram_tensor("wgu%d" % i, [D, 2 * D_FF], F32, kind="ExternalInput").ap())
            wd.append(nc.dram_tensor("wd%d" % i, [D_FF, D], F32, kind="ExternalInput").ap())
            gn.append(nc.dram_tensor("gn%d" % i, [128, 8], F32, kind="ExternalInput").ap())
        if out_h:
            h_out = nc.dram_tensor("h_out", [D, ntok], BF16, kind="ExternalOutput").ap()
        if out_final:
            g_f = nc.dram_tensor("g_f", [128, 8], F32, kind="ExternalInput").ap()
            o_fin = nc.dram_tensor("o_fin", [D, ntok], F32, kind="ExternalOutput").ap()
    else:
        nc = ctx["nc"]
        x_in, x_out = ctx["x_in"], ctx["x_out"]
        y_load = ctx.get("y_load")
        w_o, g_y = ctx.get("w_o"), ctx.get("g_y")
        wgu, wd, gn = ctx["wgu"], ctx["wd"], ctx["gn"]
        h_out, g_f, o_fin = ctx.get("h_out"), ctx.get("g_f"), ctx.get("o_fin")

    xin_v = x_in.rearrange("(c p) n -> p c n", p=128)
    xout_v = x_out.rearrange("(c p) n -> p c n", p=128)

    es = ExitStack()
    with es:
        if ctx is None:
            em = Em(nc, es)
        else:
            em = ctx["em"]
            em.pes = es
        W1 = em.sbuf("W1", [128, KC, 2 * D_FF], BF16)
        W2 = em.sbuf("W2", [128, FC, D], BF16)
        SC = 1408
        NSTG = 3
        stg = [em.sbuf("stg%d" % i, [128, SC], F32) for i in range(NSTG)]
        bStg = [Buf("stg%d" % i) for i in range(NSTG)]
        xt = [em.sbuf("xt%d" % i, [128, KC, TT], F32) for i in range(2)]
        bX = [Buf("xt%d" % i) for i in range(2)]
        hs = [em.sbuf("hs%d" % i, [128, KC, TT], BF16) for i in range(2)]
        bH = [Buf("hs%d" % i) for i in range(2)]
        NACT = 4
        actb = em.sbuf("actb", [128, NACT, TT], BF16)
        bAct = [Buf("act%d" % i) for i in range(NACT)]
        sgt = em.sbuf("sgt", [128, 2, TT], F32)
        bSg = [Buf("sg%d" % i) for i in range(2)]
        rstd = em.sbuf("rstd", [128, 2, TT], F32)
        bR = [Buf("rstd%d" % i) for i in range(2)]
        ones = em.sbuf("ones", [128, 128], BF16)
        bOnes = Buf("ones")
        gsb = em.sbuf("gsb", [128, 4, 8], F32)
        bG = Buf("gsb")
        if has_y:
            yt = em.sbuf("yt", [128, 16, TT], BF16)
            bY = Buf("yt")
        if out_final:
            fo = em.sbuf("fo", [128, KC, TT], F32)
            bFo = Buf("fo")
        assert TT <= 256
        if ctx is None:
            pbank = [em.psum("pb%d" % i, [128, 512], F32) for i in range(8)]
        else:
            pbank = ctx["banks"]

        def ph(i):
            return pbank[i // 2][:, (i % 2) * 256:(i % 2) * 256 + TT]
        bPb = [Buf("pb%d" % i, True) for i in range(8)] if ctx is None else ctx["bbanks"]
        bP = [bPb[i // 2] for i in range(16)]
        bW1 = [Buf("W1p%d" % i) for i in range(4)]
        bW2 = [Buf("W2p%d" % i) for i in range(2)]
        bDx = [Buf("dx%d" % i) for i in range(NT)]

        em.op("dve", lambda e: e.memset(ones[:], 1.0), writes=[bOnes])
        em.dma("sp", gsb[:, 0, :], (g_y if has_y else gn[0])[:, :], bG, writes=[bG])
        for i in range(n_ffn):
            em.dma("sp", gsb[:, 1 + i, :], gn[i][:, :], bG, writes=[bG])
        if out_final:
            em.dma("sp", gsb[:, 3, :], g_f[:, :], bG, writes=[bG])

        stg_i = [0]

        def load_piece(dst_ap, src_ap, ncols, scale_ap, wbuf):
            s = stg_i[0] % NSTG
            stg_i[0] += 1
            q, ce = (("sp", "dve"), ("act", "act"), ("pool", "dve"))[s]
            em.dma(q, stg[s][:, 0:ncols], src_ap, bStg[s], writes=[bStg[s]])
            if ce == "act":
                if scale_ap is None:
                    em.op("act", lambda e: e.copy(dst_ap, stg[s][:, 0:ncols]), reads=[bStg[s]], writes=[wbuf])
                else:
                    em.op("act", lambda e: e.activation(out=dst_ap, in_=stg[s][:, 0:ncols], func=AF.Identity,
                                                        scale=scale_ap), reads=[bStg[s], bG], writes=[wbuf])
            elif scale_ap is None:
                em.op("dve", lambda e: e.tensor_copy(dst_ap, stg[s][:, 0:ncols]), reads=[bStg[s]], writes=[wbuf])
            else:
                em.op("dve", lambda e: e.tensor_scalar(dst_ap, stg[s][:, 0:ncols], scale_ap, None, ALU.mult),
                      reads=[bStg[s], bG], writes=[wbuf])

        def load_ffn_w1_piece(i, pc):
            for c in range(KC):
                load_piece(W1[:, c, pc * SC:(pc + 1) * SC], wgu[i][c * 128:(c + 1) * 128, pc * SC:(pc + 1) * SC], SC,
                           gsb[:, 1 + i, c:c + 1], bW1[pc])

        def load_ffn_w2_piece(i, pc):
            for j in range(pc * 11, pc * 11 + 11):
                load_piece(W2[:, j, :], wd[i][j * 128:(j + 1) * 128, :], D, None, bW2[pc])

        def load_wout():
            for j in range(16):
                load_piece(W2[:, j, :], w_o[j * 128:(j + 1) * 128, :], D,
                           gsb[:, 0, j:j + 1] if j < 8 else None, bW2[j // 8])

        xcnt = [0]

        def stats(slot, rs, nparts=1, src=None, srcbuf=None):
            src = xt[slot] if src is None else src
            srcbuf = bX[slot] if srcbuf is None else srcbuf
            hslot = slot
            em.op("act", lambda e: e.activation(out=hs[hslot][:, :, :], in_=src[:, 0:KC, :], func=AF.Square),
                  reads=[srcbuf], writes=[bH[hslot]])
            for c in range(KC):
                em.op("pe", lambda e, c=c: e.matmul(ph(14 + rs), lhsT=ones[:, :], rhs=hs[hslot][:, c, :],
                                                  start=(c == 0), stop=(c == KC - 1)),
                      reads=[bH[hslot], bOnes], writes=[bP[14 + rs]])
            em.op("act", lambda e: e.activation(out=rstd[:, rs, :], in_=ph(14 + rs), func=AF.Sqrt, bias=EPS,
                                                scale=1.0 / D), reads=[bP[14 + rs]], writes=[bR[rs]])
            em.op("dve", lambda e: e.reciprocal(rstd[:, rs, :], rstd[:, rs, :]), reads=[bR[rs]], writes=[bR[rs]])

        def make_h(slot, rs):
            em.op("dve", lambda e: e.tensor_tensor(hs[slot][:, :, :], xt[slot][:, :, :],
                                                    rstd[:, rs, :].unsqueeze(1).to_broadcast([128, KC, TT]), ALU.mult),
                  reads=[bX[slot], bR[rs]], writes=[bH[slot]])

        def load_x(phase_idx, i, slot):
            if phase_idx == 0:
                em.dma("sp", xt[slot][:, :, :], xin_v[:, :, i * TT:(i + 1) * TT], bX[slot], writes=[bX[slot]])
            else:
                em.dma("sp", xt[slot][:, :, :], xout_v[:, :, i * TT:(i + 1) * TT], bX[slot], reads=[bDx[i]],
                       writes=[bX[slot]])

        def store_x(i, slot):
            em.dma("sp", xout_v[:, :, i * TT:(i + 1) * TT], xt[slot][:, :, :], bX[slot], reads=[bX[slot]],
                   writes=[bDx[i]])

        def post(i, slot):
            if out_h:
                stats(slot, slot)
                make_h(slot, slot)
                em.dma("sp", h_out.rearrange("(c p) n -> p c n", p=128)[:, :, i * TT:(i + 1) * TT], hs[slot][:, :, :],
                       bH[slot], reads=[bH[slot]])
            if out_final:
                stats(slot, slot)
                for c in range(KC):
                    em.op("dve", lambda e, c=c: e.scalar_tensor_tensor(out=fo[:, c, :], in0=xt[slot][:, c, :],
                                                                     scalar=gsb[:, 3, c:c + 1], in1=rstd[:, slot, :],
                                                                     op0=ALU.mult, op1=ALU.mult),
                          reads=[bX[slot], bR[slot], bG], writes=[bFo])
                em.dma("sp", o_fin.rearrange("(c p) n -> p c n", p=128)[:, :, i * TT:(i + 1) * TT], fo[:, :, :],
                       bFo, reads=[bFo])

        phases = (["wout"] if has_y else []) + [("ffn", i) for i in range(n_ffn)]
        for pi, phs in enumerate(phases):
            last = (pi == len(phases) - 1)
            if phs == "wout":
                load_wout()
                for i in range(NT):
                    slot = xcnt[0] % 2
                    xcnt[0] += 1
                    load_x(pi, i, slot)
                    if y_load is None:
                        em.dma("sp", yt[:, :, :], y_in.rearrange("(c p) n -> p c n", p=128)[:, :, i * TT:(i + 1) * TT],
                               bY, writes=[bY])
                    else:
                        y_load(em, yt, bY, i)
                    em.op("act", lambda e: e.activation(out=hs[slot][:, :, :], in_=yt[:, 0:8, :], func=AF.Square),
                          reads=[bY], writes=[bH[slot]])
                    for g in range(2):
                        for c in range(4):
                            em.op("pe", lambda e, g=g, c=c: e.matmul(ph(14 + g), lhsT=ones[:, :],
                                                                    rhs=hs[slot][:, 4 * g + c, :], start=(c == 0),
                                                                    stop=(c == 3)),
                                  reads=[bH[slot], bOnes], writes=[bP[14 + g]])
                        em.op("act", lambda e, g=g: e.activation(out=rstd[:, g, :], in_=ph(14 + g), func=AF.Sqrt,
                                                                 bias=EPS, scale=1.0 / 512), reads=[bP[14 + g]],
                              writes=[bR[g]])
                        em.op("dve", lambda e, g=g: e.reciprocal(rstd[:, g, :], rstd[:, g, :]), reads=[bR[g]],
                              writes=[bR[g]])
                        em.op("dve", lambda e, g=g: e.tensor_tensor(
                            yt[:, 4 * g:4 * g + 4, :], yt[:, 4 * g:4 * g + 4, :],
                            rstd[:, g, :].unsqueeze(1).to_broadcast([128, 4, TT]), ALU.mult),
                            reads=[bY, bR[g]], writes=[bY])
                    for d in range(8):
                        for j in range(16):
                            em.op("pe", lambda e, d=d, j=j: e.matmul(ph(d), lhsT=W2[:, j, d * 128:(d + 1) * 128],
                                                                    rhs=yt[:, j, :], start=(j == 0), stop=(j == 15)),
                                  reads=[bY, bW2[j // 8]], writes=[bP[d]])
                        em.op("dve", lambda e, d=d: e.tensor_tensor(xt[slot][:, d, :], xt[slot][:, d, :], ph(d),
                                                                     ALU.add), reads=[bP[d], bX[slot]],
                              writes=[bX[slot]])
                    if last:
                        post(i, slot)
                    store_x(i, slot)
            else:
                fi = phs[1]
                for pc in (0, 2):
                    load_ffn_w1_piece(fi, pc)
                load_ffn_w2_piece(fi, 0)
                for pc in (1, 3):
                    load_ffn_w1_piece(fi, pc)
                load_ffn_w2_piece(fi, 1)

                def prep(i):
                    slot = xcnt[0] % 2
                    xcnt[0] += 1
                    load_x(pi, i, slot)
                    stats(slot, slot)
                    make_h(slot, slot)
                    return slot

                slot_next = prep(0)
                for i in range(NT):
                    slot = slot_next
                    for j in range(FC + 1):
                        if j < FC:
                            pr = 8 + 2 * (j % 3)
                            for half, off in ((0, 0), (1, D_FF)):
                                for c in range(KC):
                                    em.op("pe", lambda e, c=c, half=half, off=off, j=j, pr=pr: e.matmul(
                                        ph(pr + half), lhsT=W1[:, c, off + j * 128:off + (j + 1) * 128],
                                        rhs=hs[slot][:, c, :], start=(c == 0), stop=(c == KC - 1)),
                                        reads=[bH[slot], bW1[2 * half + (j // 11)]], writes=[bP[pr + half]])
                            sg = j % 2
                            em.op("act", lambda e, pr=pr, sg=sg: e.activation(out=sgt[:, sg, :], in_=ph(pr),
                                                                            func=AF.Silu), reads=[bP[pr]],
                                  writes=[bSg[sg]])
                            a = j % NACT
                            em.op("dve", lambda e, pr=pr, sg=sg, a=a: e.tensor_tensor(actb[:, a, :], sgt[:, sg, :],
                                                                                   ph(pr + 1), ALU.mult),
                                  reads=[bSg[sg], bP[pr + 1]], writes=[bAct[a]])
                        if j >= 1:
                            jj = j - 1
                            a = jj % NACT
                            for d in range(8):
                                em.op("pe", lambda e, d=d, jj=jj, a=a: e.matmul(
                                    ph(d), lhsT=W2[:, jj, d * 128:(d + 1) * 128], rhs=actb[:, a, :],
                                    start=(jj == 0 and d % 2 == 0), stop=(jj == FC - 1 and d % 2 == 1)),
                                    reads=[bAct[a], bW2[jj // 11]], writes=[bP[d]])
                        if j == 10 and i + 1 < NT:
                            slot_next = prep(i + 1)
                    for d in range(8):
                        em.op("dve", lambda e, d=d: e.scalar_tensor_tensor(out=xt[slot][:, d, :], in0=ph(d),
                                                                         scalar=0.5, in1=xt[slot][:, d, :],
                                                                         op0=ALU.mult, op1=ALU.add),
                              reads=[bP[d], bX[slot]], writes=[bX[slot]])
                    if last:
                        post(i, slot)
                    store_x(i, slot)
        allb = bX + bH + bDx + ([bFo] if out_final else [])
        em.wait_bufs("sp", allb)
        if ctx is not None:
            em.barrier()
            em.end_phase()
            em.pes = None
    return nc


ST = 512
NCOL = 1542
CHUNKS = [("z0", 0, 128), ("z1", 128, 128), ("x0", 256, 128), ("x1", 384, 128), ("B", 512, 64), ("C", 576, 64),
          ("dt", 640, 4), ("mx", 644, 128), ("mo", 772, 128), ("mi", 900, 1), ("mf", 901, 1),
          ("rr", 902, 128), ("rk", 1030, 128), ("rv", 1158, 128), ("rwl", 1286, 64), ("ral", 1350, 64),
          ("rgl", 1414, 128)]
KDEC = float(np.exp(-0.5))
STAGES = {"ssd", "ml", "rw"}
CUT = 99


class _Cut(Exception):
    pass


def _ck(k):
    if CUT == k:
        raise _Cut()


def _cf_layout():
    items = [("mask128", 128), ("identf", 128), ("onesf", 128), ("sel4", 512), ("m128", ST), ("m64", ST),
             ("rneg", ST), ("maskAB", 512), ("nmasklo", 128), ("signm", 128), ("blkf", 128)]
    off, o = {}, 0
    for n, w in items:
        off[n] = (o, w)
        o += w
    return off, o


def _prm_layout():
    items = [("cwx", 8), ("cbx", 2), ("cwB", 4), ("cbB", 1), ("cwC", 4), ("cbC", 1), ("dtb", 1), ("alog", 1),
             ("Dcol", 2), ("cwm", 4), ("cbm", 1), ("bi", 1), ("bf", 1), ("nm", 1), ("mu3", 3), ("mu2", 2),
             ("mug", 1), ("w0", 1), ("a0", 1), ("kk", 1), ("ka", 1), ("rkc", 1), ("lnw", 1), ("lnb", 1)]
    off, o = {}, 0
    for n, w in items:
        off[n] = (o, w)
        o += w
    return off, o


def mix_consts():
    off, tot = _cf_layout()
    cf = np.zeros((128, tot), np.float32)

    def put(n, a):
        o, w = off[n]
        cf[0:a.shape[0], o:o + w] = a
    i = np.arange(128)
    put("mask128", (i[None, :] >= i[:, None]).astype(np.float32))
    put("identf", np.eye(128, dtype=np.float32))
    put("onesf", np.ones((128, 128), np.float32))
    sel = np.zeros((4, 4, 128), np.float32)
    for h in range(4):
        sel[h, h, :] = 1.0
    put("sel4", sel.reshape(4, 512))
    t = np.arange(ST)
    put("m128", np.tile((t % 128 != 0).astype(np.float32)[None, :], (128, 1)))
    put("m64", np.tile((t % 64 != 0).astype(np.float32)[None, :], (128, 1)))
    put("rneg", np.tile(np.where(t % 128 == 0, -1e30, 0.0).astype(np.float32)[None, :], (128, 1)))
    j = np.arange(64)
    strict = (j[None, :] > j[:, None]).astype(np.float32)
    incl = (j[None, :] >= j[:, None]).astype(np.float32)
    one = np.concatenate([strict, incl, strict, incl], 1)
    put("maskAB", np.concatenate([np.concatenate([one, one], 0), np.zeros((128, 256), np.float32)], 1))
    lo = -(j[None, :] < j[:, None]).astype(np.float32)
    put("nmasklo", np.concatenate([np.concatenate([lo, lo], 0), np.zeros((128, 64), np.float32)], 1))
    put("signm", np.concatenate([np.ones((128, 64), np.float32), -np.ones((128, 64), np.float32)], 1))
    blk = np.zeros((128, 128), np.float32)
    blk[0:64, 0:64] = 1.0
    blk[64:, 64:] = 1.0
    put("blkf", blk)
    return cf


class _Tile:
    def __init__(self, t, b):
        self.t = t
        self.b = b

    def f(self, p=128, n=ST, off=0, p0=0):
        return self.t[p0:p0 + p, off:off + n]

    def h(self, p=128, n=ST, off=0, p0=0):
        return self.t[:, :].bitcast(BF16)[p0:p0 + p, off:off + n]


def build_mix(T, ctx=None):
    NS = T // ST
    cfo, cftot = _cf_layout()
    pro, prtot = _prm_layout()
    if ctx is None:
        nc = bass.Bass("TRN2", target_bir_lowering=False)
        h_in = nc.dram_tensor("h_in", [D_MODEL, T], BF16, kind="ExternalInput").ap()
        w_sel = nc.dram_tensor("w_sel", [D_MODEL, NCOL], F32, kind="ExternalInput").ap()
        g_mix = nc.dram_tensor("g_mix", [128, 8], F32, kind="ExternalInput").ap()
        cf_d = nc.dram_tensor("cf", [128, cftot], F32, kind="ExternalInput").ap()
        prm_d = nc.dram_tensor("prm", [128, prtot], F32, kind="ExternalInput").ap()
        wsm_d = nc.dram_tensor("wsm", [128, 768], F32, kind="ExternalInput").ap()
        y_out = nc.dram_tensor("y_out", [512, T], BF16, kind="ExternalOutput").ap()
        hin_v = h_in.rearrange("(c p) n -> p c n", p=128)

        def h_src(t):
            return hin_v[:, :, t * ST:(t + 1) * ST]

        def y_dst(r0, r1, t):
            return y_out[r0:r1, t * ST:(t + 1) * ST]
    else:
        nc = ctx["nc"]
        w_sel, g_mix, cf_d, prm_d, wsm_d, y_out = (ctx[k] for k in ("w_sel", "g_mix", "cf", "prm", "wsm", "y_out"))
        h_src = ctx["h_src"]
        y_dst = ctx["y_dst"]

    es = ExitStack()
    with es:
        if ctx is None:
            em = Em(nc, es)
        else:
            em = ctx["em"]
            em.pes = es
        V = em.op
        Win = em.sbuf("Win", [128, 8, NCOL], BF16)
        bWin = Buf("Win")
        ht = [em.sbuf("ht%d" % i, [128, 8, ST], BF16) for i in range(2)]
        bHt = [Buf("ht%d" % i) for i in range(2)]
        cf = em.sbuf("cfs", [128, cftot], F32)
        bC = Buf("cf")
        prm = em.sbuf("prms", [128, prtot + 16], F32)
        bPr = Buf("prm")
        wsm = em.sbuf("wsms", [128, 768], BF16)
        bWs = Buf("wsm")
        cb = em.sbuf("cbs", [128, 448], BF16)
        bCb = Buf("cb")
        gm = em.sbuf("gm", [128, 8], F32)
        bGm = Buf("gm")
        halo = em.sbuf("halo", [128, 16, 4], F32)
        bHalo = [Buf("halo%d" % i) for i in range(16)]
        NG = 56
        TW = ST + 8
        gt = [_Tile(em.sbuf("g%d" % i, [128, TW], F32), Buf("g%d" % i)) for i in range(NG)]
        if ctx is None:
            banks = [em.psum("pb%d" % i, [128, 512], F32) for i in range(8)]
            bBk = [Buf("pb%d" % i, True) for i in range(8)]
        else:
            banks, bBk = ctx["banks"], ctx["bbanks"]
        bki = [0]
        reserved = set()

        def bank(keep=False):
            while True:
                i = bki[0] % 8
                bki[0] += 1
                if i not in reserved:
                    break
            if keep:
                reserved.add(i)
            return banks[i], bBk[i]

        def unkeep(bb):
            reserved.discard(bBk.index(bb))
        from collections import deque
        free = deque(gt)
        live = []

        def G():
            g = free.popleft()
            live.append(g)
            return g

        def R(*ts):
            for g in ts:
                live.remove(g)
                free.append(g)

        def C(name, p=128, p0=0):
            o, w = cfo[name]
            return cf[p0:p0 + p, o:o + w]

        def P(name, j=0, p=128, p0=0):
            o, w = pro[name]
            return prm[p0:p0 + p, o + j:o + j + 1]
        identb, onesb, blkb = cb[:, 0:128], cb[:, 128:256], cb[:, 256:384]

        em.dma("sp", cf[:, :], cf_d[:, :], bC, writes=[bC])
        em.dma("sp", prm[:, 0:prtot], prm_d[:, :], bPr, writes=[bPr])
        em.dma("sp", gm[:, :], g_mix[:, :], bGm, writes=[bGm])
        V("dve", lambda e: e.tensor_copy(cb[:, 0:128], C("identf")), [bC], [bCb])
        V("dve", lambda e: e.tensor_copy(cb[:, 128:256], C("onesf")), [bC], [bCb])
        V("dve", lambda e: e.tensor_copy(cb[:, 256:384], C("blkf")), [bC], [bCb])
        em.dma("sp", gt[0].t[:, 0:384], wsm_d[:, 0:384], gt[0].b, writes=[gt[0].b])
        em.dma("sp", gt[1].t[:, 0:384], wsm_d[:, 384:768], gt[1].b, writes=[gt[1].b])
        V("dve", lambda e: e.tensor_copy(wsm[:, 0:384], gt[0].t[:, 0:384]), [gt[0].b], [bWs])
        V("dve", lambda e: e.tensor_copy(wsm[:, 384:768], gt[1].t[:, 0:384]), [gt[1].b], [bWs])
        wq_b, wk_b, wv_b = wsm[:, 0:128], wsm[:, 128:256], wsm[:, 256:384]
        wup_b, aup_b, gup_b = wsm[0:64, 384:512], wsm[0:64, 512:640], wsm[:, 640:768]
        for c in range(8):
            for hf in range(3):
                g = gt[2 + (c * 3 + hf) % 4]
                c0 = hf * 514
                em.dma("sp", g.t[:, 0:514], w_sel[c * 128:(c + 1) * 128, c0:c0 + 514], g.b, writes=[g.b])
                V(["dve", "pool", "act"][hf] if hf < 2 else "dve",
                  lambda e, g=g, c=c, c0=c0: e.tensor_scalar(Win[:, c, c0:c0 + 514], g.t[:, 0:514], gm[:, c:c + 1],
                                                           None, ALU.mult), [g.b, bGm], [bWin])
        o_mu3 = pro["mu3"][0]
        V("dve", lambda e: e.tensor_scalar(prm[:, prtot:prtot + 6], prm[:, o_mu3:o_mu3 + 6], -1.0, 1.0, ALU.mult,
                                          ALU.add), [bPr], [bPr])
        V("act", lambda e: e.activation(out=prm[0:4, prtot + 6:prtot + 7], in_=P("alog", 0, 4), func=AF.Exp), [bPr],
          [bPr])
        V("dve", lambda e: e.tensor_scalar(prm[0:4, prtot + 6:prtot + 7], prm[0:4, prtot + 6:prtot + 7], -1.0, None,
                                          ALU.mult), [bPr], [bPr])
        V("dve", lambda e: e.tensor_scalar(prm[0:1, prtot + 7:prtot + 8], P("bf", 0, 1), -1.0, None, ALU.mult),
          [bPr], [bPr])
        negA = prm[0:4, prtot + 6:prtot + 7]
        nbf = prm[0:1, prtot + 7:prtot + 8]

        def OMU(j, p=128):
            return prm[0:p, prtot + j:prtot + j + 1]

        stt = em.sbuf("stt", [128, 1024], F32)
        bSs, bSm, bSr, bMc = Buf("Sssd"), Buf("Sml"), Buf("Srw"), Buf("mcar")
        S_f = stt[0:64, 0:256]
        Cst = stt[:, 256:385]
        Srw = stt[:, 400:464]
        mcar = stt[0:1, 480:481]
        stb = em.sbuf("stb", [128, 512], BF16)
        bSsb, bSrb = Buf("Sssdb"), Buf("Srwb")
        S_b = stb[0:64, 0:256]
        Srw_b = stb[:, 256:320]
        vtok = em.sbuf("vtok", [128, 4, 256], BF16)
        bVt = Buf("vtok")
        nbc = em.sbuf("nbc", [128, 128], BF16)
        Cst_b = stb[:, 320:448]
        bSmb = Buf("Smlb")
        bNb = Buf("nbc")
        V("dve", lambda e: e.memset(stt[:, :], 0.0), [], [bSs, bSm, bSr, bMc])
        V("dve", lambda e: e.memset(stb[:, :], 0.0), [], [bSsb, bSrb, bSmb])
        V("dve", lambda e: e.memset(vtok[:, :, :], 1.0), [], [bVt])
        V("dve", lambda e: e.memset(nbc[:, :], 0.0), [], [bNb])
        V("dve", lambda e: e.memset(halo[:, :, :], 0.0), [], bHalo)

        def load_h(t, slot):
            em.dma("sp", ht[slot][:, :, :], h_src(t), bHt[slot], writes=[bHt[slot]])

        iden2 = em.sbuf("iden2", [128, 64], F32)
        bI2 = Buf("iden2")
        V("dve", lambda e: e.tensor_tensor(iden2[:, :], C("identf")[:, 0:64], C("identf")[:, 64:128], ALU.add), [bC],
          [bI2])
        V("dve", lambda e: e.tensor_copy(cb[:, 384:448], iden2[:, :]), [bI2], [bCb])

        def mm(out, lhsT, rhs, r, w, start=True, stop=True):
            V("pe", lambda e: e.matmul(out, lhsT=lhsT, rhs=rhs, start=start, stop=stop), r, w)

        def v3(ap, l):
            return ap.rearrange("p (c l) -> p c l", l=l)

        load_h(0, 0)
        for t in range(NS):
            slot = t % 2
            if t + 1 < NS:
                load_h(t + 1, 1 - slot)
            tsl = slice(t * ST, (t + 1) * ST)
            raw = {}
            for ci, (name, c0, M) in enumerate(CHUNKS):
                pb, bb = bank()
                for c in range(8):
                    mm(pb[0:M, :], Win[:, c, c0:c0 + M], ht[slot][:, c, :], [bWin, bHt[slot]], [bb], c == 0, c == 7)
                g = G()
                raw[name] = g
                if name in ("z0", "z1"):
                    V("act", lambda e: e.activation(out=g.f(), in_=pb[:, :], func=AF.Silu), [bb], [g.b])
                elif name == "mo":
                    V("act", lambda e: e.activation(out=g.f(), in_=pb[:, :], func=AF.Sigmoid), [bb], [g.b])
                elif name == "dt":
                    V("act", lambda e: e.activation(out=g.f(4), in_=pb[0:4, :], func=AF.Exp, bias=P("dtb", 0, 4)),
                      [bb, bPr], [g.b])
                    V("act", lambda e: e.activation(out=g.f(4), in_=g.f(4), func=AF.Ln, bias=1.0), [g.b], [g.b])
                elif name == "mi":
                    V("act", lambda e: e.activation(out=g.f(1), in_=pb[0:1, :], func=AF.Identity, bias=P("bi", 0, 1)),
                      [bb, bPr], [g.b])
                elif name == "mf":
                    V("act", lambda e: e.activation(out=g.f(1), in_=pb[0:1, :], func=AF.Exp, scale=-1.0, bias=nbf),
                      [bb, bPr], [g.b])
                    V("act", lambda e: e.activation(out=g.f(1), in_=g.f(1), func=AF.Ln, bias=1.0), [g.b], [g.b])
                else:
                    hw = 3 if name in ("x0", "x1", "B", "C", "mx") else 1
                    hb = bHalo[ci % 16]
                    V("act", lambda e: e.copy(g.f(M, hw, 4 - hw), halo[0:M, ci % 16, 4 - hw:4]), [hb], [g.b])
                    if ci % 2:
                        V("act", lambda e: e.copy(g.f(M, ST, 4), pb[0:M, :]), [bb], [g.b])
                    else:
                        V("dve", lambda e: e.tensor_copy(g.f(M, ST, 4), pb[0:M, :]), [bb], [g.b])
                    V("act", lambda e: e.copy(halo[0:M, ci % 16, 4 - hw:4], g.f(M, hw, 4 + ST - hw)), [g.b],
                      [hb])
            conv_in = [("x0", 128, lambda k: P("cwx", k), P("cbx", 0)),
                       ("x1", 128, lambda k: P("cwx", 4 + k), P("cbx", 1)),
                       ("B", 64, lambda k: P("cwB", k, 64), P("cbB", 0, 64)),
                       ("C", 64, lambda k: P("cwC", k, 64), P("cbC", 0, 64)),
                       ("mx", 128, lambda k: P("cwm", k), P("cbm", 0))]
            conv_out = {}
            for (name, M, wk, bcol) in conv_in:
                r = raw[name]
                acc = G()
                V("act", lambda e: e.activation(out=acc.f(M), in_=r.f(M, ST, 4), func=AF.Identity, bias=bcol,
                                                scale=wk(3)), [r.b, bPr], [acc.b])
                for k in range(3):
                    V("dve", lambda e: e.scalar_tensor_tensor(out=acc.f(M), in0=r.f(M, ST, 1 + k), scalar=wk(k),
                                                              in1=acc.f(M), op0=ALU.mult, op1=ALU.add),
                      [r.b, bPr, acc.b], [acc.b])
                o = G()
                V("act", lambda e: e.activation(out=o.h(M), in_=acc.f(M), func=AF.Silu), [acc.b], [o.b])
                conv_out[name] = o
                R(acc)
                if name != "mx":
                    R(r)
            if "ssd" in STAGES:
                NCH = ST // 128
                sz = [raw["z0"], raw["z1"]]
                xs_b = [conv_out["x0"], conv_out["x1"]]
                B_b, C_b, xc_b = conv_out["B"], conv_out["C"], conv_out["mx"]
                dt = raw["dt"]
                a_t, acum = G(), G()
                V("dve", lambda e: e.tensor_scalar(a_t.f(4), dt.f(4), negA, None, ALU.mult), [dt.b, bPr], [a_t.b])
                V("dve", lambda e: e.tensor_tensor_scan(acum.f(4), C("m128", 4), a_t.f(4), 0.0, ALU.mult, ALU.add),
                  [a_t.b, bC], [acum.b])
                R(a_t)
                pcol, bcol_ = bank()
                for c in range(NCH):
                    mm(pcol[:, c * 4:c * 4 + 4], acum.f(4, 128, c * 128), C("identf", 4)[:, 0:4], [acum.b, bC], [bcol_])
                    mm(pcol[:, 16 + c * 4:16 + c * 4 + 4], dt.f(4, 128, c * 128), C("identf", 4)[:, 0:4], [dt.b, bC],
                       [bcol_])
                R(dt)
                cols = G()
                V("dve", lambda e: e.tensor_copy(cols.f(128, 32), pcol[:, 0:32]), [bcol_], [cols.b])
                V("dve", lambda e: e.tensor_scalar(cols.f(128, 16, 96), cols.f(128, 16, 0), -1.0, None, ALU.mult),
                  [cols.b], [cols.b])
                abc = []
                for h in range(4):
                    pa, ba = bank()
                    mm(pa[:, :], C("sel4", 4)[:, h * 128:(h + 1) * 128], acum.f(4), [acum.b, bC], [ba])
                    ab = G()
                    V("act", lambda e: e.copy(ab.f(), pa[:, :]), [ba], [ab.b])
                    abc.append(ab)
                    V("dve", lambda e: e.tensor_copy(cols.t[:, 32 + h:32 + h + 13:4], ab.t[:, 127:512:128]), [ab.b],
                      [cols.b])
                R(acum)
                V("dve", lambda e: e.tensor_tensor(cols.f(128, 16, 48), cols.f(128, 16, 32), cols.f(128, 16, 0),
                                                  ALU.subtract), [cols.b], [cols.b])
                V("act", lambda e: e.activation(out=cols.f(128, 16, 48), in_=cols.f(128, 16, 48), func=AF.Exp), [cols.b],
                  [cols.b])
                V("act", lambda e: e.activation(out=cols.f(128, 16, 80), in_=cols.f(128, 16, 32), func=AF.Exp), [cols.b],
                  [cols.b])
                V("dve", lambda e: e.tensor_tensor(cols.f(128, 16, 64), cols.f(128, 16, 48), cols.f(128, 16, 16),
                                                  ALU.mult), [cols.b], [cols.b])
                cdec = []
                for h in range(4):
                    ex, cd = G(), G()
                    V("act", lambda e: e.activation(out=ex.f(64), in_=abc[h].f(64), func=AF.Exp), [abc[h].b], [ex.b])
                    V("dve", lambda e: e.tensor_tensor(cd.h(64), ex.f(64), C_b.h(64), ALU.mult), [ex.b, C_b.b], [cd.b])
                    R(ex)
                    cdec.append(cd)
                pcb, bcb = bank()
                for c in range(NCH):
                    mm(pcb[:, c * 128:(c + 1) * 128], B_b.h(64, 128, c * 128), C_b.h(64, 128, c * 128), [B_b.b, C_b.b],
                       [bcb])
                CBm = G()
                V("dve", lambda e: e.tensor_tensor(v3(CBm.f(), 128), v3(pcb[:, :], 128),
                                                  C("mask128").unsqueeze(1).to_broadcast([128, NCH, 128]), ALU.mult),
                  [bcb, bC], [CBm.b])
                xdt, xdte, Btok = G(), G(), G()
                for half in range(2):
                    px, bx = bank()
                    pxb = px[:, :].bitcast(BF16)
                    for cc in range(2):
                        c = half * 2 + cc
                        for i in range(2):
                            V("pe", lambda e: e.transpose(pxb[:, cc * 256 + i * 128:cc * 256 + (i + 1) * 128],
                                                          xs_b[i].h(128, 128, c * 128), identb), [xs_b[i].b, bCb], [bx])
                        V("pe", lambda e: e.transpose(pxb[:, 512 + cc * 64:512 + (cc + 1) * 64],
                                                      B_b.h(64, 128, c * 128), identb[0:64, 0:64]), [B_b.b, bCb], [bx])
                    for (dst, coff) in ((xdt, 16), (xdte, 64)):
                        V("dve", lambda e: e.tensor_tensor(
                            v3(dst.h(128, 512, half * 512), 64), v3(pxb[:, 0:512], 64),
                            cols.f(128, 8, coff + half * 8).unsqueeze(2).to_broadcast([128, 8, 64]), ALU.mult),
                            [bx, cols.b], [dst.b])
                    V("act", lambda e: e.copy(Btok.h(128, 128, half * 128), pxb[:, 512:640]), [bx], [Btok.b])
                R(B_b)
                py = [bank(True), bank(True)]
                for c in range(NCH):
                    mts = []
                    for h in range(4):
                        et, mt = G(), G()
                        V("dve", lambda e: e.tensor_scalar(et.f(128, 128), abc[h].f(128, 128, c * 128),
                                                          cols.f(128, 1, 96 + c * 4 + h), 0.0, ALU.add, ALU.min),
                          [abc[h].b, cols.b], [et.b])
                        V("act", lambda e: e.activation(out=et.f(128, 128), in_=et.f(128, 128), func=AF.Exp), [et.b],
                          [et.b])
                        V("dve", lambda e: e.tensor_tensor(mt.h(128, 128), et.f(128, 128), CBm.f(128, 128, c * 128),
                                                            ALU.mult), [et.b, CBm.b], [mt.b])
                        R(et)
                        mts.append(mt)
                    for h in range(4):
                        pyb, byb = py[h // 2]
                        po = (h % 2) * 64
                        mm(pyb[po:po + 64, c * 128:(c + 1) * 128], xdt.h(128, 64, (c * 4 + h) * 64), mts[h].h(128, 128),
                           [xdt.b, mts[h].b], [byb], True, False)
                        mm(pyb[po:po + 64, c * 128:(c + 1) * 128], S_b[:, h * 64:(h + 1) * 64],
                           cdec[h].h(64, 128, c * 128), [bSsb, cdec[h].b], [byb], False, True)
                    R(*mts)
                    ps_, bs_ = bank()
                    for h in range(4):
                        mm(ps_[0:64, h * 64:(h + 1) * 64], Btok.h(128, 64, c * 64), xdte.h(128, 64, (c * 4 + h) * 64),
                           [Btok.b, xdte.b], [bs_])
                    for h in range(4):
                        V("dve", lambda e: e.scalar_tensor_tensor(
                            out=S_f[:, h * 64:(h + 1) * 64], in0=S_f[:, h * 64:(h + 1) * 64],
                            scalar=cols.f(64, 1, 80 + c * 4 + h), in1=ps_[0:64, h * 64:(h + 1) * 64], op0=ALU.mult,
                            op1=ALU.add), [bSs, cols.b, bs_], [bSs])
                    V("act", lambda e: e.copy(S_b, S_f), [bSs], [bSsb])
                R(xdt, xdte, Btok, CBm, cols, C_b, *abc, *cdec)
                for i in range(2):
                    pyb, byb = py[i]
                    tmp, yo = G(), G()
                    V("dve", lambda e: e.scalar_tensor_tensor(out=tmp.f(), in0=xs_b[i].h(), scalar=P("Dcol", i),
                                                              in1=pyb[:, :], op0=ALU.mult, op1=ALU.add),
                      [xs_b[i].b, bPr, byb], [tmp.b])
                    unkeep(byb)
                    V("dve", lambda e: e.tensor_tensor(yo.h(), tmp.f(), sz[i].f(), ALU.mult), [tmp.b, sz[i].b], [yo.b])
                    em.dma("sp", y_dst(i * 128, (i + 1) * 128, t), yo.h(), yo.b, reads=[yo.b])
                    R(tmp, yo, xs_b[i], sz[i])
            if "ml" in STAGES:
                try:
                    osig, li, sp = raw["mo"], raw["mi"], raw["mf"]
                    xc_b = conv_out["mx"]
                    NCH = ST // 128
                    rmx = raw["mx"]
                    mxb = G()
                    V("act", lambda e: e.copy(mxb.h(), rmx.f(128, ST, 4)), [rmx.b], [mxb.b])
                    R(rmx)
                    pq, bq = bank()
                    mm(pq[:, :], wq_b, xc_b.h(), [bWs, xc_b.b], [bq])
                    q_b = G()
                    V("act", lambda e: e.copy(q_b.h(), pq[:, :]), [bq], [q_b.b])
                    pk, bk = bank()
                    mm(pk[:, :], wk_b, xc_b.h(), [bWs, xc_b.b], [bk])
                    k_b = G()
                    V("act", lambda e: e.activation(out=k_b.h(), in_=pk[:, :], func=AF.Identity, scale=128.0 ** -0.5), [bk],
                      [k_b.b])
                    _ck(1)
                    bcum, g_, cmx, mint, sm, il, mt = G(), G(), G(), G(), G(), G(), G()
                    V("dve", lambda e: e.tensor_tensor_scan(bcum.f(1), C("m128", 1), sp.f(1), 0.0, ALU.mult, ALU.subtract),
                      [sp.b, bC], [bcum.b])
                    V("dve", lambda e: e.tensor_tensor(g_.f(1), li.f(1), bcum.f(1), ALU.subtract), [li.b, bcum.b], [g_.b])
                    V("dve", lambda e: e.tensor_tensor_scan(cmx.f(1), C("rneg", 1), g_.f(1), 0.0, ALU.add, ALU.max),
                      [g_.b, bC], [cmx.b])
                    V("dve", lambda e: e.tensor_tensor(mint.f(1), bcum.f(1), cmx.f(1), ALU.add), [bcum.b, cmx.b], [mint.b])
                    V("dve", lambda e: e.tensor_copy(sm.f(1, 4, 0), bcum.t[0:1, 127:512:128]), [bcum.b], [sm.b])
                    V("dve", lambda e: e.tensor_copy(sm.f(1, 4, 4), cmx.t[0:1, 127:512:128]), [cmx.b], [sm.b])
                    V("dve", lambda e: e.tensor_tensor(sm.f(1, 4, 8), sm.f(1, 4, 0), sm.f(1, 4, 4), ALU.add), [sm.b], [sm.b])
                    V("dve", lambda e: e.tensor_copy(sm.f(1, 1, 16), mcar), [bMc, sm.b], [sm.b])
                    for c in range(4):
                        V("dve", lambda e: e.tensor_tensor(sm.f(1, 1, 12 + c), sm.f(1, 1, 16 + c), sm.f(1, 1, c), ALU.add),
                          [sm.b], [sm.b])
                        V("dve", lambda e: e.tensor_tensor(sm.f(1, 1, 12 + c), sm.f(1, 1, 12 + c), sm.f(1, 1, 8 + c),
                                                          ALU.max), [sm.b], [sm.b])
                        if c < 3:
                            V("dve", lambda e: e.tensor_copy(sm.f(1, 1, 17 + c), sm.f(1, 1, 12 + c)), [sm.b], [sm.b])
                    V("dve", lambda e: e.tensor_copy(mcar, sm.f(1, 1, 15)), [sm.b], [bMc])
                    V("dve", lambda e: e.tensor_tensor(sm.f(1, 4, 20), sm.f(1, 4, 0), sm.f(1, 4, 16), ALU.add), [sm.b], [sm.b])
                    V("dve", lambda e: e.tensor_tensor(sm.f(1, 4, 20), sm.f(1, 4, 20), sm.f(1, 4, 12), ALU.subtract), [sm.b],
                      [sm.b])
                    V("dve", lambda e: e.tensor_tensor(sm.f(1, 4, 24), sm.f(1, 4, 8), sm.f(1, 4, 12), ALU.subtract), [sm.b],
                      [sm.b])
                    V("act", lambda e: e.activation(out=sm.f(1, 8, 20), in_=sm.f(1, 8, 20), func=AF.Exp), [sm.b], [sm.b])
                    V("dve", lambda e: e.tensor_tensor(v3(il.f(1), 128), v3(bcum.f(1), 128),
                                                      sm.f(1, 4, 16).unsqueeze(2).to_broadcast([1, 4, 128]), ALU.add),
                      [bcum.b, sm.b], [il.b])
                    V("dve", lambda e: e.tensor_tensor(mt.f(1), il.f(1), mint.f(1), ALU.max), [il.b, mint.b], [mt.b])
                    V("dve", lambda e: e.tensor_tensor(mint.f(1), bcum.f(1), mt.f(1), ALU.subtract), [bcum.b, mt.b], [mint.b])
                    V("dve", lambda e: e.tensor_tensor(il.f(1), il.f(1), mt.f(1), ALU.subtract), [il.b, mt.b], [il.b])
                    V("dve", lambda e: e.tensor_scalar(mt.f(1), mt.f(1), -1.0, None, ALU.mult), [mt.b], [mt.b])
                    V("dve", lambda e: e.tensor_tensor(v3(cmx.f(1), 128), v3(g_.f(1), 128),
                                                      sm.f(1, 4, 4).unsqueeze(2).to_broadcast([1, 4, 128]), ALU.subtract),
                      [g_.b, sm.b], [cmx.b])
                    R(bcum, li, sp)
                    _ck(2)
                    onerow = C("onesf", 1)
                    pc_, bc_ = bank()
                    for c in range(NCH):
                        mm(pc_[:, 32 + 2 * c:34 + 2 * c], g_.f(1, 128, c * 128), onerow[:, 0:2], [g_.b, bC], [bc_])
                        mm(pc_[:, 40 + 2 * c:42 + 2 * c], cmx.f(1, 128, c * 128), onerow[:, 0:2], [cmx.b, bC], [bc_])
                    mm(pc_[:, 8:16], onerow[:, 0:128], sm.f(1, 8, 20), [sm.b, bC], [bc_])
                    mcol = G()
                    V("dve", lambda e: e.tensor_copy(mcol.f(128, 8, 8), pc_[:, 8:16]), [bc_], [mcol.b])
                    V("dve", lambda e: e.tensor_copy(mcol.f(128, 8, 0), pc_[:, 32:48:2]), [bc_], [mcol.b])
                    V("act", lambda e: e.activation(out=mcol.f(128, 4, 4), in_=mcol.f(128, 4, 4), func=AF.Exp), [mcol.b],
                      [mcol.b])
                    V("dve", lambda e: e.tensor_scalar(mcol.f(128, 4, 16), mcol.f(128, 4, 4), 128.0 ** -0.5, None, ALU.mult),
                      [mcol.b], [mcol.b])
                    R(g_, cmx, sm)
                    _ck(3)
                    pR1, bR1 = bank(True)
                    mm(pR1[:, :], onerow[:, 0:128], mint.f(1), [mint.b, bC], [bR1])
                    pR2, bR2 = bank()
                    mm(pR2[:, :], onerow[:, 0:128], il.f(1), [il.b, bC], [bR2])
                    wint, qw, emt = G(), G(), G()
                    V("act", lambda e: e.activation(out=wint.f(), in_=pR2[:, :], func=AF.Exp), [bR2], [wint.b])
                    V("dve", lambda e: e.tensor_tensor(qw.h(), q_b.h(), wint.f(), ALU.mult), [q_b.b, wint.b], [qw.b])
                    pR3, bR3 = bank()
                    mm(pR3[:, :], onerow[:, 0:128], mt.f(1), [mt.b, bC], [bR3])
                    V("act", lambda e: e.activation(out=emt.f(), in_=pR3[:, :], func=AF.Exp), [bR3], [emt.b])
                    R(wint, mint, il, mt)
                    _ck(4)
                    kwt = G()
                    pnum, bnum = bank(True)
                    pden, bden = bank(True)
                    for c in range(NCH):
                        cs = slice(c * 128, (c + 1) * 128)
                        pv, bv = bank()
                        mm(pv[:, 0:128], mxb.h(128, 128, c * 128), wv_b, [mxb.b, bWs], [bv])
                        mm(pv[:, 128:256], xc_b.h(128, 128, c * 128), wk_b, [xc_b.b, bWs], [bv])
                        V("act", lambda e: e.copy(vtok[:, c, 0:128], pv[:, 0:128]), [bv], [bVt])
                        V("dve", lambda e: e.tensor_scalar(kwt.h(128, 128, c * 128), pv[:, 128:256],
                                                          mcol.f(128, 1, 16 + c), None, ALU.mult), [bv, mcol.b],
                          [kwt.b])
                        _ck(6)
                        et, wm, qkw, tmpc = G(), G(), G(), G()
                        V("dve", lambda e: e.tensor_scalar(et.f(128, 128), pR1[:, cs], mcol.f(128, 1, c), 0.0, ALU.add,
                                                          ALU.min), [bR1, mcol.b], [et.b])
                        V("act", lambda e: e.activation(out=et.f(128, 128), in_=et.f(128, 128), func=AF.Exp), [et.b], [et.b])
                        V("dve", lambda e: e.tensor_tensor(wm.f(128, 128), et.f(128, 128), C("mask128"), ALU.mult),
                          [et.b, bC], [wm.b])
                        _ck(7)
                        pqk, bqk = bank()
                        mm(pqk[:, 0:128], k_b.h(128, 128, c * 128), q_b.h(128, 128, c * 128), [k_b.b, q_b.b], [bqk])
                        V("dve", lambda e: e.tensor_tensor(qkw.h(128, 128), pqk[:, 0:128], wm.f(128, 128), ALU.mult),
                          [bqk, wm.b], [qkw.b])
                        _ck(8)
                        mm(pnum[:, cs], vtok[:, c, 0:128], qkw.h(128, 128), [bVt, qkw.b], [bnum], True, False)
                        mm(pnum[:, cs], Cst_b[:, 0:128], qw.h(128, 128, c * 128), [bSmb, qw.b], [bnum], False, True)
                        mm(pden[:, cs], onesb, qkw.h(128, 128), [bCb, qkw.b], [bden], True, False)
                        mm(pden[:, cs], nbc[:, :], qw.h(128, 128, c * 128), [bNb, qw.b], [bden], False, True)
                        _ck(9)
                        pcl, bcl = bank()
                        mm(pcl[:, 0:160], kwt.h(128, 128, c * 128), vtok[:, c, 0:160], [kwt.b, bVt], [bcl])
                        V("dve", lambda e: e.tensor_scalar(tmpc.f(128, 129), pcl[:, 0:129], mcol.f(128, 1, 12 + c), None,
                                                          ALU.mult), [bcl, mcol.b], [tmpc.b])
                        V("dve", lambda e: e.scalar_tensor_tensor(out=Cst, in0=Cst, scalar=mcol.f(128, 1, 8 + c),
                                                                  in1=tmpc.f(128, 129), op0=ALU.mult, op1=ALU.add),
                          [bSm, mcol.b, tmpc.b], [bSm])
                        V("dve", lambda e: e.tensor_copy(nbc[:, :], Cst[:, 128:129].to_broadcast([128, 128])), [bSm],
                          [bNb])
                        V("act", lambda e: e.copy(Cst_b, Cst[:, 0:128]), [bSm], [bSmb])
                        R(et, wm, qkw, tmpc)
                    unkeep(bR1)
                    _ck(5)
                    R(kwt, mxb, xc_b, q_b, k_b, qw, mcol)
                    dsb, hh, sq, rs, yo = G(), G(), G(), G(), G()
                    V("act", lambda e: e.copy(dsb.f(), pden[:, :]), [bden], [dsb.b])
                    unkeep(bden)
                    V("dve", lambda e: e.scalar_tensor_tensor(out=dsb.f(), in0=dsb.f(), scalar=-1.0, in1=dsb.f(),
                                                              op0=ALU.mult, op1=ALU.max), [dsb.b], [dsb.b])
                    V("dve", lambda e: e.tensor_tensor(dsb.f(), dsb.f(), emt.f(), ALU.max), [dsb.b, emt.b], [dsb.b])
                    V("dve", lambda e: e.reciprocal(dsb.f(), dsb.f()), [dsb.b], [dsb.b])
                    V("dve", lambda e: e.tensor_tensor(hh.f(), pnum[:, :], dsb.f(), ALU.mult), [bnum, dsb.b], [hh.b])
                    unkeep(bnum)
                    V("dve", lambda e: e.tensor_tensor(hh.f(), hh.f(), osig.f(), ALU.mult), [hh.b, osig.b], [hh.b])
                    V("act", lambda e: e.activation(out=sq.h(), in_=hh.f(), func=AF.Square), [hh.b], [sq.b])
                    pss, bss = bank()
                    mm(pss[:, :], onesb, sq.h(), [bCb, sq.b], [bss])
                    V("act", lambda e: e.activation(out=rs.f(), in_=pss[:, :], func=AF.Sqrt, bias=EPS, scale=1.0 / 128),
                      [bss], [rs.b])
                    V("dve", lambda e: e.reciprocal(rs.f(), rs.f()), [rs.b], [rs.b])
                    V("dve", lambda e: e.scalar_tensor_tensor(out=yo.h(), in0=hh.f(), scalar=P("nm", 0), in1=rs.f(),
                                                              op0=ALU.mult, op1=ALU.mult), [hh.b, bPr, rs.b], [yo.b])
                    em.dma("sp", y_dst(256, 384, t), yo.h(), yo.b, reads=[yo.b])
                    R(dsb, hh, sq, rs, yo, emt, osig)
                except _Cut:
                    reserved.clear()

            if "rw" in STAGES:
                try:
                    NC8 = ST // 64

                    def shiftmix(r, M, mu_ap, omu_ap, eng="dve"):
                        o = G()
                        V(eng, lambda e: e.tensor_scalar(o.f(M), r.f(M, ST, 4), omu_ap, None, ALU.mult), [r.b, bPr], [o.b])
                        if eng == "dve":
                            V(eng, lambda e: e.scalar_tensor_tensor(out=o.f(M), in0=r.f(M, ST, 3), scalar=mu_ap, in1=o.f(M),
                                                                    op0=ALU.mult, op1=ALU.add), [r.b, bPr, o.b], [o.b])
                        else:
                            tm_ = G()
                            V(eng, lambda e: e.tensor_scalar(tm_.f(M), r.f(M, ST, 3), mu_ap, None, ALU.mult), [r.b, bPr],
                              [tm_.b])
                            V(eng, lambda e: e.tensor_tensor(o.f(M), o.f(M), tm_.f(M), ALU.add), [o.b, tm_.b], [o.b])
                            R(tm_)
                        R(r)
                        return o
                    r_s = shiftmix(raw["rr"], 128, P("mu3", 0), OMU(0))
                    k_s = shiftmix(raw["rk"], 128, P("mu3", 1), OMU(1), "dve")
                    v_s = shiftmix(raw["rv"], 128, P("mu3", 2), OMU(2))
                    wl_s = shiftmix(raw["rwl"], 64, P("mu2", 0, 64), OMU(3, 64), "dve")
                    al_s = shiftmix(raw["ral"], 64, P("mu2", 1, 64), OMU(4, 64))
                    gl_s = shiftmix(raw["rgl"], 128, P("mug", 0), OMU(5), "dve")
                    tw_b, al_b, sg_b = G(), G(), G()
                    V("act", lambda e: e.activation(out=tw_b.h(64), in_=wl_s.f(64), func=AF.Tanh), [wl_s.b], [tw_b.b])
                    V("act", lambda e: e.copy(al_b.h(64), al_s.f(64)), [al_s.b], [al_b.b])
                    V("act", lambda e: e.activation(out=sg_b.h(), in_=gl_s.f(), func=AF.Sigmoid), [gl_s.b], [sg_b.b])
                    R(wl_s, al_s, gl_s)
                    _ck(11)
                    sgm, a_f, g_f = G(), G(), G()
                    pw, bw = bank()
                    mm(pw[:, :], wup_b, tw_b.h(64), [bWs, tw_b.b], [bw])
                    V("act", lambda e: e.activation(out=sgm.f(), in_=pw[:, :], func=AF.Sigmoid, bias=P("w0", 0)), [bw, bPr],
                      [sgm.b])
                    pa_, ba_ = bank()
                    mm(pa_[:, :], aup_b, al_b.h(64), [bWs, al_b.b], [ba_])
                    V("act", lambda e: e.activation(out=a_f.f(), in_=pa_[:, :], func=AF.Sigmoid, bias=P("a0", 0)), [ba_, bPr],
                      [a_f.b])
                    pg, bg = bank()
                    mm(pg[:, :], gup_b, sg_b.h(), [bWs, sg_b.b], [bg])
                    V("act", lambda e: e.copy(g_f.f(), pg[:, :]), [bg], [g_f.b])
                    R(tw_b, al_b, sg_b)
                    _ck(12)
                    kkf, sq, nrm = G(), G(), G()
                    V("dve", lambda e: e.tensor_scalar(kkf.f(), k_s.f(), P("kk", 0), None, ALU.mult), [k_s.b, bPr], [kkf.b])
                    V("act", lambda e: e.activation(out=sq.h(), in_=kkf.f(), func=AF.Square), [kkf.b], [sq.b])
                    pss, bss = bank()
                    mm(pss[:, :], blkb, sq.h(), [bCb, sq.b], [bss])
                    V("act", lambda e: e.activation(out=nrm.f(), in_=pss[:, :], func=AF.Sqrt), [bss], [nrm.b])
                    V("dve", lambda e: e.tensor_scalar(nrm.f(), nrm.f(), 1e-6, None, ALU.max), [nrm.b], [nrm.b])
                    V("dve", lambda e: e.reciprocal(nrm.f(), nrm.f()), [nrm.b], [nrm.b])
                    V("dve", lambda e: e.tensor_tensor(kkf.f(), kkf.f(), nrm.f(), ALU.mult), [kkf.b, nrm.b], [kkf.b])
                    R(sq, nrm)
                    _ck(13)
                    kp, beta, csg, cx = G(), G(), G(), G()
                    V("dve", lambda e: e.tensor_scalar(kp.f(), a_f.f(), -1.0, P("ka", 0), ALU.add, ALU.mult), [a_f.b, bPr],
                      [kp.b])
                    V("dve", lambda e: e.scalar_tensor_tensor(out=kp.f(), in0=kp.f(), scalar=1.0, in1=k_s.f(), op0=ALU.add,
                                                              op1=ALU.mult), [kp.b, k_s.b], [kp.b])
                    V("dve", lambda e: e.tensor_tensor(beta.f(), kkf.f(), a_f.f(), ALU.mult), [kkf.b, a_f.b], [beta.b])
                    V("dve", lambda e: e.tensor_tensor_scan(csg.f(), C("m64"), sgm.f(), 0.0, ALU.mult, ALU.add), [sgm.b, bC],
                      [csg.b])
                    V("dve", lambda e: e.tensor_tensor(cx.f(), csg.f(), sgm.f(), ALU.subtract), [csg.b, sgm.b], [cx.b])
                    eP, eN, ePx = G(), G(), G()
                    V("act", lambda e: e.activation(out=eP.f(), in_=csg.f(), func=AF.Exp, scale=-KDEC), [csg.b], [eP.b])
                    V("act", lambda e: e.activation(out=eN.f(), in_=csg.f(), func=AF.Exp, scale=KDEC), [csg.b], [eN.b])
                    V("act", lambda e: e.activation(out=ePx.f(), in_=cx.f(), func=AF.Exp, scale=-KDEC), [cx.b], [ePx.b])
                    KR, Bc_b, Kc_b, v_b, pl = G(), G(), G(), G(), G()
                    KRv = KR.h(128, 1024).rearrange("p (c two l) -> p c two l", two=2, l=64)
                    V("dve", lambda e: e.tensor_tensor(KRv[:, :, 0, :], v3(kkf.f(), 64), v3(ePx.f(), 64), ALU.mult),
                      [kkf.b, ePx.b], [KR.b])
                    V("dve", lambda e: e.tensor_tensor(KRv[:, :, 1, :], v3(r_s.f(), 64), v3(eP.f(), 64), ALU.mult),
                      [r_s.b, eP.b], [KR.b])
                    V("dve", lambda e: e.tensor_tensor(Bc_b.h(), beta.f(), eN.f(), ALU.mult), [beta.b, eN.b], [Bc_b.b])
                    V("dve", lambda e: e.tensor_tensor(Kc_b.h(), kp.f(), eN.f(), ALU.mult), [kp.b, eN.b], [Kc_b.b])
                    V("act", lambda e: e.copy(v_b.h(), v_s.f()), [v_s.b], [v_b.b])
                    V("dve", lambda e: e.tensor_copy(pl.f(128, 8), eP.t[:, 63:512:64]), [eP.b], [pl.b])
                    R(kkf, a_f, k_s, beta, csg, cx, sgm, eP, eN, ePx)
                    _ck(14)
                    TK = [G(), G()]
                    for j2 in range(2):
                        px, bx = bank()
                        pxb = px[:, :].bitcast(BF16)
                        for cc in range(4):
                            c = 4 * j2 + cc
                            srcs = [(KR, c * 128), (Bc_b, c * 64), (Kc_b, c * 64), (v_b, c * 64)]
                            for q in range(4):
                                for hd in range(2):
                                    p0 = 64 * hd
                                    V("pe", lambda e: e.transpose(
                                        pxb[p0:p0 + 64, cc * 256 + q * 64:cc * 256 + (q + 1) * 64],
                                        srcs[q][0].h(64, 64, srcs[q][1], p0), identb[p0:p0 + 64, p0:p0 + 64]),
                                        [srcs[q][0].b, bCb], [bx])
                        if j2 % 2:
                            V("act", lambda e: e.copy(TK[j2].h(128, 1024), pxb[:, :]), [bx], [TK[j2].b])
                        else:
                            V("dve", lambda e: e.tensor_copy(TK[j2].h(128, 1024), pxb[:, :]), [bx], [TK[j2].b])
                    R(v_b)
                    _ck(15)

                    def tk(c, q, p0):
                        return TK[c // 4].h(64, 64, (c % 4) * 256 + q * 64, p0)
                    AB = [G(), G()]
                    Xa, XTa, Xb, XTb, TTm = G(), G(), G(), G(), G()
                    pA2, bA2 = bank(True)
                    for j2 in range(4):
                        pA, bA = bank()
                        for cc in range(2):
                            c = 2 * j2 + cc
                            for hd in range(2):
                                p0 = 64 * hd
                                krhs = KR.h(64, 128, c * 128, p0)
                                mm(pA[p0:p0 + 64, cc * 256:cc * 256 + 128], Bc_b.h(64, 64, c * 64, p0), krhs,
                                   [Bc_b.b, KR.b], [bA])
                                mm(pA[p0:p0 + 64, cc * 256 + 128:cc * 256 + 256], Kc_b.h(64, 64, c * 64, p0), krhs,
                                   [Kc_b.b, KR.b], [bA])
                                mm(pA2[p0:p0 + 64, c * 64:(c + 1) * 64], KR.h(64, 64, c * 128, p0),
                                   Bc_b.h(64, 64, c * 64, p0), [KR.b, Bc_b.b], [bA2])
                        abt = AB[j2 // 2]
                        V("dve", lambda e: e.tensor_tensor(
                            v3(abt.h(128, 512, (j2 % 2) * 512), 256), v3(pA[:, :], 256),
                            C("maskAB")[:, 0:256].unsqueeze(1).to_broadcast([128, 2, 256]), ALU.mult), [bA, bC],
                            [abt.b])
                    V("dve", lambda e: e.tensor_tensor(v3(Xa.h(128, 512), 64), v3(pA2[:, :], 64),
                                                      C("nmasklo")[:, 0:64].unsqueeze(1).to_broadcast([128, 8, 64]),
                                                      ALU.mult), [bA2, bC], [Xa.b])
                    unkeep(bA2)
                    for j2 in range(2):
                        V("dve", lambda e: e.tensor_scalar(
                            v3(XTa.h(128, 256, j2 * 256), 64),
                            AB[j2].h(128, 1024).rearrange("p (m w) -> p m w", w=256)[:, :, 0:64], -1.0, None,
                            ALU.mult), [AB[j2].b], [XTa.b])
                    V("dve", lambda e: e.tensor_tensor(
                        v3(TTm.h(128, 512), 64), v3(XTa.h(128, 512), 64),
                        cb[:, 384:448].unsqueeze(1).to_broadcast([128, 8, 64]), ALU.add), [XTa.b, bCb], [TTm.b])

                    def ab(c, w, p0):
                        return AB[c // 4].h(64, 64, (c % 4) * 256 + w * 64, p0)
                    _ck(16)
                    Xc, XTc, Xn, XTn = Xa, XTa, Xb, XTb
                    for lvl in range(1, 6):
                        for b4 in range(2):
                            pX, bX_ = bank()
                            for m in range(4):
                                mo_ = (b4 * 4 + m) * 64
                                for hd in range(2):
                                    p0 = 64 * hd
                                    mm(pX[p0:p0 + 64, m * 128:m * 128 + 64], XTc.h(64, 64, mo_, p0),
                                       Xc.h(64, 64, mo_, p0), [XTc.b, Xc.b], [bX_])
                                    if lvl < 5:
                                        mm(pX[p0:p0 + 64, m * 128 + 64:m * 128 + 128], Xc.h(64, 64, mo_, p0),
                                           XTc.h(64, 64, mo_, p0), [XTc.b, Xc.b], [bX_])
                            pXv = v3(pX[:, :], 128)
                            V("act", lambda e: e.copy(v3(Xn.h(128, 256, b4 * 256), 64), pXv[:, :, 0:64]), [bX_],
                              [Xn.b])
                            if lvl < 5:
                                V("dve", lambda e: e.tensor_copy(v3(XTn.h(128, 256, b4 * 256), 64), pXv[:, :, 64:128]),
                                  [bX_], [XTn.b])
                            pT, bT = bank()
                            for m in range(4):
                                mo_ = (b4 * 4 + m) * 64
                                for hd in range(2):
                                    p0 = 64 * hd
                                    mm(pT[p0:p0 + 64, m * 64:(m + 1) * 64], Xn.h(64, 64, mo_, p0),
                                       TTm.h(64, 64, mo_, p0), [Xn.b, TTm.b], [bT])
                            V("dve", lambda e: e.tensor_tensor(TTm.h(128, 256, b4 * 256), TTm.h(128, 256, b4 * 256),
                                                              pT[:, 0:256], ALU.add), [bT, TTm.b], [TTm.b])
                        Xc, XTc, Xn, XTn = Xn, XTn, Xc, XTc
                    R(Xa, XTa, Xb, XTb)
                    _ck(17)
                    AkV, WU = G(), G()
                    pK, bK = bank()
                    for c in range(NC8):
                        for hd in range(2):
                            p0 = 64 * hd
                            mm(pK[p0:p0 + 64, c * 64:(c + 1) * 64], ab(c, 2, p0), tk(c, 3, p0),
                               [AB[c // 4].b, TK[c // 4].b], [bK])
                    V("act", lambda e: e.copy(AkV.h(128, 512), pK[:, :]), [bK], [AkV.b])
                    for b4 in range(2):
                        pW, bW_ = bank()
                        for cc in range(4):
                            c = b4 * 4 + cc
                            for hd in range(2):
                                p0 = 64 * hd
                                mm(pW[p0:p0 + 64, cc * 128:cc * 128 + 64], TTm.h(64, 64, c * 64, p0), tk(c, 0, p0),
                                   [TTm.b, TK[c // 4].b], [bW_])
                                mm(pW[p0:p0 + 64, cc * 128 + 64:cc * 128 + 128], TTm.h(64, 64, c * 64, p0),
                                   AkV.h(64, 64, c * 64, p0), [TTm.b, AkV.b], [bW_])
                        V("dve", lambda e: e.tensor_tensor(v3(WU.h(128, 512, b4 * 512), 128), v3(pW[:, :], 128),
                                                          C("signm").unsqueeze(1).to_broadcast([128, 4, 128]),
                                                          ALU.mult), [bW_, bC], [WU.b])
                    R(AkV, TTm)
                    _ck(18)

                    def wu(c, w, p0):
                        return WU.h(64, 64, c * 128 + w * 64, p0)
                    G0T, Hpp, QT = G(), G(), G()
                    pG, bG_ = bank()
                    pH, bH_ = bank()
                    pQ, bQ_ = bank()
                    for c in range(NC8):
                        cs = slice(c * 64, (c + 1) * 64)
                        for hd in range(2):
                            p0 = 64 * hd
                            mm(pG[p0:p0 + 64, cs], wu(c, 0, p0), tk(c, 1, p0), [WU.b, TK[c // 4].b], [bG_])
                            mm(pH[p0:p0 + 64, cs], tk(c, 1, p0), wu(c, 1, p0), [WU.b, TK[c // 4].b], [bH_], True,
                               False)
                            mm(pH[p0:p0 + 64, cs], tk(c, 2, p0), tk(c, 3, p0), [TK[c // 4].b], [bH_], False, True)
                            mm(pQ[p0:p0 + 64, cs], wu(c, 0, p0), ab(c, 1, p0), [WU.b, AB[c // 4].b], [bQ_])
                    V("dve", lambda e: e.tensor_tensor(v3(G0T.f(), 64),
                                                      iden2[:, :].unsqueeze(1).to_broadcast([128, NC8, 64]),
                                                      v3(pG[:, :], 64), ALU.subtract), [bG_, bI2], [G0T.b])
                    V("dve", lambda e: e.tensor_tensor(v3(Hpp.f(), 64), v3(pH[:, :], 64),
                                                      pl.f(128, 8).unsqueeze(2).to_broadcast([128, NC8, 64]),
                                                      ALU.mult), [bH_, pl.b], [Hpp.b])
                    V("dve", lambda e: e.tensor_tensor(v3(QT.h(), 64), KRv[:, :, 1, :], v3(pQ[:, :], 64),
                                                      ALU.subtract), [bQ_, KR.b], [QT.b])
                    _ck(19)
                    pY, bY_ = bank(True)
                    for c in range(NC8):
                        cs = slice(c * 64, (c + 1) * 64)
                        for hd in range(2):
                            p0 = 64 * hd
                            mm(pY[p0:p0 + 64, cs], Srw_b[p0:p0 + 64, :], QT.h(64, 64, c * 64, p0), [bSrb, QT.b],
                               [bY_], True, False)
                            mm(pY[p0:p0 + 64, cs], wu(c, 1, p0), ab(c, 1, p0), [WU.b, AB[c // 4].b], [bY_], False,
                               False)
                            mm(pY[p0:p0 + 64, cs], tk(c, 3, p0), ab(c, 3, p0), [TK[c // 4].b, AB[c // 4].b], [bY_],
                               False, True)
                        pS, bS_ = bank()
                        for hd in range(2):
                            p0 = 64 * hd
                            mm(pS[p0:p0 + 64, 0:64], G0T.f(64, 64, c * 64, p0), Srw[p0:p0 + 64, :], [G0T.b, bSr],
                               [bS_])
                        V("dve", lambda e: e.scalar_tensor_tensor(out=Srw, in0=pS[:, 0:64], scalar=pl.f(128, 1, c),
                                                                  in1=Hpp.f(128, 64, c * 64), op0=ALU.mult,
                                                                  op1=ALU.add), [bS_, pl.b, Hpp.b, bSr], [bSr])
                        V("act", lambda e: e.copy(Srw_b, Srw), [bSr], [bSrb])
                    R(G0T, Hpp, QT, KR, Bc_b, Kc_b, pl, WU, *TK, *AB)
                    _ck(20)
                    y_f, y_b, sq, rs, rk_, rkr, yo = G(), G(), G(), G(), G(), G(), G()
                    V("dve", lambda e: e.tensor_copy(y_f.f(), pY[:, :]), [bY_], [y_f.b])
                    V("act", lambda e: e.copy(y_b.h(), pY[:, :]), [bY_], [y_b.b])
                    unkeep(bY_)
                    pm, bm = bank()
                    mm(pm[:, :], blkb, y_b.h(), [bCb, y_b.b], [bm])
                    V("dve", lambda e: e.scalar_tensor_tensor(out=y_f.f(), in0=pm[:, :], scalar=-1.0 / 64, in1=y_f.f(),
                                                              op0=ALU.mult, op1=ALU.add), [bm, y_f.b], [y_f.b])
                    V("act", lambda e: e.activation(out=sq.h(), in_=y_f.f(), func=AF.Square), [y_f.b], [sq.b])
                    pv_, bv_ = bank()
                    mm(pv_[:, :], blkb, sq.h(), [bCb, sq.b], [bv_])
                    V("act", lambda e: e.activation(out=rs.f(), in_=pv_[:, :], func=AF.Sqrt, bias=64e-5, scale=1.0 / 64),
                      [bv_], [rs.b])
                    V("dve", lambda e: e.reciprocal(rs.f(), rs.f()), [rs.b], [rs.b])
                    V("dve", lambda e: e.tensor_tensor(y_f.f(), y_f.f(), rs.f(), ALU.mult), [y_f.b, rs.b], [y_f.b])
                    V("dve", lambda e: e.tensor_scalar(y_f.f(), y_f.f(), P("lnw", 0), P("lnb", 0), ALU.mult, ALU.add),
                      [y_f.b, bPr], [y_f.b])
                    V("dve", lambda e: e.tensor_tensor(rk_.f(), r_s.f(), kp.f(), ALU.mult), [r_s.b, kp.b], [rk_.b])
                    V("dve", lambda e: e.tensor_scalar(rkr.h(), rk_.f(), P("rkc", 0), None, ALU.mult), [rk_.b, bPr], [rkr.b])
                    pb_, bb_ = bank()
                    mm(pb_[:, :], blkb, rkr.h(), [bCb, rkr.b], [bb_])
                    V("dve", lambda e: e.tensor_tensor(rk_.f(), pb_[:, :], v_s.f(), ALU.mult), [bb_, v_s.b], [rk_.b])
                    V("dve", lambda e: e.tensor_tensor(y_f.f(), y_f.f(), rk_.f(), ALU.add), [y_f.b, rk_.b], [y_f.b])
                    V("dve", lambda e: e.tensor_tensor(yo.h(), y_f.f(), g_f.f(), ALU.mult), [y_f.b, g_f.b], [yo.b])
                    em.dma("sp", y_dst(384, 512, t), yo.h(), yo.b, reads=[yo.b])
                    R(y_f, y_b, sq, rs, rk_, rkr, yo, r_s, v_s, kp, g_f)
                except _Cut:
                    reserved.clear()

            if len(STAGES) == 3:
                assert not live, [g.b.name for g in live]
            R(*list(live))
        em.wait_bufs("sp", [g.b for g in gt])
        if ctx is not None:
            em.barrier()
            em.end_phase()
            em.pes = None
    return nc


def _lay8(g):
    return np.ascontiguousarray(np.asarray(g, np.float32).reshape(8, 128).T)


def _mix_core_inputs(p, l, q):
    g = q // 2
    hs = [4 * q + i for i in range(4)]
    cols = []
    for h in hs:
        cols += list(range(64 * h, 64 * h + 64))
    for h in hs:
        cols += list(range(1024 + 64 * h, 1024 + 64 * h + 64))
    cols += list(range(2048 + 64 * g, 2048 + 64 * g + 64))
    cols += list(range(2176 + 64 * g, 2176 + 64 * g + 64))
    cols += [2304 + h for h in hs]
    cols += list(range(2320 + 128 * q, 2320 + 128 * q + 128))
    cols += list(range(2832 + 128 * q, 2832 + 128 * q + 128))
    cols += [3344 + q, 3348 + q]
    for base in (3352, 3864, 4376):
        cols += list(range(base + 128 * q, base + 128 * q + 128))
    cols += list(range(4888, 5144))
    assert len(cols) == NCOL
    w_sel = np.ascontiguousarray(p["w_in"][l][:, cols])
    pro, prtot = _prm_layout()
    prm = np.zeros((128, prtot), np.float32)

    def put(n, a, j=0):
        a = np.asarray(a, np.float32)
        if a.ndim == 1:
            a = a[:, None]
        o, w = pro[n]
        prm[0:a.shape[0], o + j:o + j + a.shape[1]] = a
    cw, cbv = p["ssd_conv_w"][l], p["ssd_conv_b"][l]
    for i in range(2):
        ch = list(range(64 * hs[2 * i], 64 * hs[2 * i] + 64)) + list(range(64 * hs[2 * i + 1], 64 * hs[2 * i + 1] + 64))
        put("cwx", cw[:, ch].T, 4 * i)
        put("cbx", cbv[ch], i)
        put("Dcol", np.repeat(p["ssd_d"][l][[hs[2 * i], hs[2 * i + 1]]], 64), i)
    chB = list(range(1024 + 64 * g, 1024 + 64 * g + 64))
    chC = list(range(1152 + 64 * g, 1152 + 64 * g + 64))
    put("cwB", cw[:, chB].T)
    put("cbB", cbv[chB])
    put("cwC", cw[:, chC].T)
    put("cbC", cbv[chC])
    put("dtb", p["ssd_dt_bias"][l][hs])
    put("alog", p["ssd_a_log"][l][hs])
    sl = slice(128 * q, 128 * q + 128)
    put("cwm", p["mlstm_conv_w"][l][:, sl].T)
    put("cbm", p["mlstm_conv_b"][l][sl])
    put("bi", p["mlstm_gate_bias"][l][q:q + 1])
    put("bf", p["mlstm_gate_bias"][l][4 + q:5 + q])
    put("nm", p["mlstm_norm"][l][sl])
    mu = p["rwkv_shift_mu"][l]
    for j, base in enumerate((0, 512, 1024)):
        put("mu3", mu[base + 128 * q:base + 128 * q + 128], j)
    put("mu2", mu[1536:1600], 0)
    put("mu2", mu[1600:1664], 1)
    put("mug", mu[1664:1792])
    put("w0", p["rwkv_w0"][l][sl])
    put("a0", p["rwkv_a0"][l][sl])
    put("kk", p["rwkv_k_k"][l][sl])
    put("ka", p["rwkv_k_a"][l][sl])
    put("rkc", p["rwkv_r_k"][l].reshape(-1)[sl])
    put("lnw", p["rwkv_ln_w"][l][sl])
    put("lnb", p["rwkv_ln_b"][l][sl])
    wsm = np.zeros((128, 768), np.float32)
    wsm[:, 0:128] = p["mlstm_wq"][l][q]
    wsm[:, 128:256] = p["mlstm_wk"][l][q]
    wsm[:, 256:384] = p["mlstm_wv"][l][q]
    wsm[0:64, 384:512] = p["rwkv_w_up"][l][:, sl]
    wsm[0:64, 512:640] = p["rwkv_a_up"][l][:, sl]
    wsm[:, 640:768] = p["rwkv_g_up"][l][:, sl]
    return {"w_sel": w_sel, "g_mix": _lay8(p["mix_norm"][l]), "prm": prm, "wsm": wsm}


def build_all(T, NTOK, TTK=256):
    nc = bass.Bass("TRN2", target_bir_lowering=False)
    D = D_MODEL
    L = DEPTH
    cfo, cftot = _cf_layout()
    pro, prtot = _prm_layout()

    def din(name, shape, dt=F32):
        return nc.dram_tensor(name, list(shape), dt, kind="ExternalInput").ap()
    x_in = din("x_in", [D, NTOK])
    wgu1 = din("wgu1", [L, D, 2 * D_FF])
    wd1 = din("wd1", [L, D_FF, D])
    gn1 = din("gn1", [L, 128, 8])
    wgu2 = din("wgu2", [L, D, 2 * D_FF])
    wd2 = din("wd2", [L, D_FF, D])
    gn2 = din("gn2", [L, 128, 8])
    w_o = din("w_o", [L, 2048, D])
    g_y = din("g_y", [L, 128, 8])
    g_f = din("g_f", [128, 8])
    w_sel = din("w_sel", [L, D, NCOL])
    g_mix = din("g_mix", [L, 128, 8])
    prm = din("prm", [L, 128, prtot])
    wsm = din("wsm", [L, 128, 768])
    cf = din("cf", [128, cftot])
    o_fin = nc.dram_tensor("o_fin", [D, NTOK], F32, kind="ExternalOutput").ap()
    x_res = nc.dram_tensor("x_res", [D, NTOK], F32).ap()
    h_loc = nc.dram_tensor("h_loc", [D, NTOK], BF16).ap()
    h_all = nc.dram_tensor("h_all", [8 * 512, NTOK], BF16).ap()
    y_loc = nc.dram_tensor("y_loc", [4 * 512, NTOK], BF16).ap()
    y_all = nc.dram_tensor("y_all", [16 * 512, NTOK], BF16).ap()
    y_mine = nc.dram_tensor("y_mine", [2048, NTOK], BF16).ap()
    groups = [[0, 1, 2, 3], [4, 5, 6, 7]]
    SPQ = NTOK // ST

    es = ExitStack()
    with es:
        em = Em(nc, es)
        banks = [em.psum("pb%d" % i, [128, 512], F32) for i in range(8)]
        bbanks = [Buf("pb%d" % i, True) for i in range(8)]
        cc_sem = es.enter_context(nc.semaphore("cc_sem"))
        cc = [cc_sem, 0]
        em.extra.append(cc)
        base = {"nc": nc, "em": em, "banks": banks, "bbanks": bbanks}

        def gather(src, dst, nblk):
            em.barrier()
            for j in range(nblk):
                nc.gpsimd.collective_compute("AllGather", ALU.bypass, replica_groups=groups,
                                             ins=[src[j * 128:(j + 1) * 128, :]],
                                             outs=[dst[j * 512:(j + 1) * 512, :]]).then_inc(cc_sem, 1)
                cc[1] += 1
            em.extra[0] = (cc_sem, cc[1])
            em.barrier()

        sqv = nc.sync.snap(nc.sync.partition_id() % 4, min_val=0, max_val=3)
        bYm = Buf("y_mine")

        def select_y():
            src = y_all.rearrange("(s m) n -> s m n", s=4)[bass.ds(sqv, 1), :, :]
            em.dma("sp", y_mine.rearrange("(o m) n -> o m n", o=1), src, bYm, writes=[bYm])

        def y_load(em_, yt, bY, i):
            cs_ = slice(i * TTK, (i + 1) * TTK)
            for kc, dst in ((0, yt[:, 0:8:2, :]), (1, yt[:, 1:8:2, :]), (2, yt[:, 8:12, :]), (3, yt[:, 12:16, :])):
                em_.dma("sp", dst, y_mine[kc * 512:(kc + 1) * 512, cs_].rearrange("(r p) n -> p r n", p=128), bY,
                        reads=[bYm], writes=[bY])

        def y_dst(r0, r1, t):
            sq_, tt = t // SPQ, t % SPQ
            return y_loc[sq_ * 512 + r0:sq_ * 512 + r1, tt * ST:(tt + 1) * ST]

        def h_src(t):
            r, tt = t // SPQ, t % SPQ
            return h_all.rearrange("(c r p) n -> p c r n", c=8, r=4)[:, :, r, tt * ST:(tt + 1) * ST]

        em.extra[0] = (cc_sem, 0)
        em.prefix = "t0_"
        build_tok(NTOK, TTK, False, 1, True, False,
                  dict(base, x_in=x_in, x_out=x_res, wgu=[wgu1[0]], wd=[wd1[0]], gn=[gn1[0]], h_out=h_loc))
        for l in range(L):
            gather(h_loc, h_all, 8)
            em.prefix = "m%d_" % l
            build_mix(T, dict(base, w_sel=w_sel[l], g_mix=g_mix[l], cf=cf, prm=prm[l], wsm=wsm[l], y_out=y_loc,
                              h_src=h_src, y_dst=y_dst))
            gather(y_loc, y_all, 16)
            select_y()
            em.prefix = "t%d_" % (l + 1)
            if l < L - 1:
                build_tok(NTOK, TTK, True, 2, True, False,
                          dict(base, x_in=x_res, x_out=x_res, y_load=y_load, w_o=w_o[l], g_y=g_y[l],
                               wgu=[wgu2[l], wgu1[l + 1]], wd=[wd2[l], wd1[l + 1]], gn=[gn2[l], gn1[l + 1]],
                               h_out=h_loc))
            else:
                build_tok(NTOK, TTK, True, 1, False, True,
                          dict(base, x_in=x_res, x_out=x_res, y_load=y_load, w_o=w_o[l], g_y=g_y[l],
                               wgu=[wgu2[l]], wd=[wd2[l]], gn=[gn2[l]], g_f=g_f, o_fin=o_fin))
        print("[build_all] instructions:", em.nins, "dma sems:", em.nsem)
    return nc


_PROGS = {}


def _prog(key, fn):
    if key not in _PROGS:
        _PROGS[key] = fn()
    return _PROGS[key]


def _run(nc, in_maps):
    return run_bass_kernel_spmd(nc, in_maps, core_ids=list(range(8))).results


def kernel(**inp):
    p = {k: np.asarray(v) for k, v in inp.items()}
    x = p["x"].astype(np.float32, copy=False)
    B, T, D = x.shape
    NTOK = B * T // 8
    xflat = x.reshape(B * T, D)
    nc = _prog(("all", T, NTOK), lambda: build_all(T, NTOK))
    f32 = lambda a: np.ascontiguousarray(a, dtype=np.float32)
    lay = lambda a: np.ascontiguousarray(np.stack([_lay8(a[l]) for l in range(DEPTH)]))
    shared = {"wgu1": f32(p["ffn1_w_gate_up"]), "wd1": f32(p["ffn1_w_down"]), "gn1": lay(p["ffn1_norm"]),
              "wgu2": f32(p["ffn2_w_gate_up"]), "wd2": f32(p["ffn2_w_down"]), "gn2": lay(p["ffn2_norm"]),
              "w_o": f32(p["w_out"]), "g_y": lay(p["ssd_norm"]), "g_f": _lay8(p["final_norm"]),
              "g_mix": lay(p["mix_norm"]), "cf": mix_consts()}
    perq = []
    for q in range(4):
        ds_ = [_mix_core_inputs(p, l, q) for l in range(DEPTH)]
        perq.append({k: np.ascontiguousarray(np.stack([d[k] for d in ds_])) for k in ("w_sel", "prm", "wsm")})
    im = []
    for c in range(8):
        d = dict(shared)
        d.update(perq[c % 4])
        d["x_in"] = np.ascontiguousarray(xflat[c * NTOK:(c + 1) * NTOK].T)
        im.append(d)
    res = _run(nc, im)
    out = np.concatenate([np.asarray(r["o_fin"]).T for r in res], axis=0).reshape(B, T, D)
    return np.ascontiguousarray(out, dtype=np.float32)
```

```python
import numpy as np
import ml_dtypes
from contextlib import ExitStack
import concourse.bass as bass
import concourse.mybir as mybir
from concourse.bass_utils import run_bass_kernel_spmd

F32 = mybir.dt.float32
BF16 = mybir.dt.bfloat16
AF = mybir.ActivationFunctionType
ALU = mybir.AluOpType

D_MODEL = 1024
DEPTH = 4
D_FF = 2816
EPS = 1e-6
WQ = "sp"


class Buf:
    __slots__ = ("name", "w", "r", "dsem", "dcnt", "excl")

    def __init__(self, name, excl=False):
        self.name = name
        self.excl = excl
        self.w = None
        self.r = {}
        self.dsem = None
        self.dcnt = 0


class Em:
    def __init__(self, nc, es):
        self.nc = nc
        self.es = es
        self.eng = {"pe": nc.tensor, "act": nc.scalar, "dve": nc.vector, "pool": nc.gpsimd, "sp": nc.sync}
        self.sem = {k: es.enter_context(nc.semaphore("s_" + k)) for k in ["pe", "act", "dve", "pool"]}
        self.cnt = {k: 0 for k in self.sem}
        self.known = {}
        self.nsem = 0
        self.nins = 0
        self.pes = None
        self.prefix = ""
        self.sempool = []
        self.dma_bufs = []
        self.extra = []

    def sbuf(self, name, shape, dt):
        st = self.pes if self.pes is not None else self.es
        return st.enter_context(self.nc.sbuf_tensor(self.prefix + name, list(shape), dt))

    def barrier(self):
        evs = [(self.sem[k], self.cnt[k]) for k in self.sem if self.cnt[k] > 0]
        evs += [(b.dsem, b.dcnt) for b in self.dma_bufs]
        evs += [(s_, c_) for (s_, c_) in self.extra if c_ > 0]
        for e in self.eng:
            self._wait(e, evs)

    def end_phase(self):
        for b in self.dma_bufs:
            self.sempool.append((b.dsem, b.dcnt))
            b.dsem = None
        self.dma_bufs = []

    def psum(self, name, shape, dt=F32):
        return self.es.enter_context(self.nc.psum_tensor(name, list(shape), dt))

    def _deps(self, reads, writes):
        evs = []
        for b in reads:
            if b.w is not None:
                evs.append(b.w)
            if b.excl:
                evs.extend(b.r.values())
        for b in writes:
            if b.w is not None:
                evs.append(b.w)
            evs.extend(b.r.values())
        return evs

    def _wait(self, e, evs):
        need = {}
        for (s, v) in evs:
            k = id(s)
            if v > self.known.get((e, k), 0):
                if k not in need or need[k][1] < v:
                    need[k] = (s, v)
        for k, (s, v) in need.items():
            self.eng[e].wait_ge(s, v)
            self.known[(e, k)] = v

    def _mark(self, ev, reads, writes):
        k = id(ev[0])
        for b in reads:
            b.r[k] = ev
        for b in writes:
            b.w = ev
            b.r = {}

    def op(self, e, fn, reads=(), writes=()):
        evs = self._deps(reads, writes)
        if e == "pe":
            evs = [ev for ev in evs if ev[0] is not self.sem["pe"]]
        self._wait(e, evs)
        ins = fn(self.eng[e])
        self.cnt[e] += 1
        ins.then_inc(self.sem[e], 1)
        self._mark((self.sem[e], self.cnt[e]), reads, writes)
        self.nins += 1

    def dma(self, q, out, in_, prim, reads=(), writes=()):
        evs = self._deps(reads, writes)
        self._wait(q, evs)
        if prim.dsem is None:
            if self.sempool:
                prim.dsem, prim.dcnt = self.sempool.pop()
            else:
                prim.dsem = self.es.enter_context(self.nc.semaphore("d%d" % self.nsem))
                prim.dcnt = 0
                self.nsem += 1
            self.dma_bufs.append(prim)
        prim.dcnt += 16
        self.eng[q].dma_start(out=out, in_=in_).then_inc(prim.dsem, 16)
        self._mark((prim.dsem, prim.dcnt), reads, writes)
        self.nins += 1

    def wait_bufs(self, q, bufs):
        evs = []
        for b in bufs:
            if b.w is not None:
                evs.append(b.w)
            evs.extend(b.r.values())
        self._wait(q, evs)


def build_tok(ntok, TT, has_y, n_ffn, out_h, out_final, ctx=None):
    NT = ntok // TT
    D, KC, FC = D_MODEL, 8, D_FF // 128
    if ctx is None:
        nc = bass.Bass("TRN2", target_bir_lowering=False)
        x_in = nc.dram_tensor("x_in", [D, ntok], F32, kind="ExternalInput").ap()
        x_out = nc.dram_tensor("x_out", [D, ntok], F32, kind="ExternalOutput").ap()
        y_load = None
        if has_y:
            y_in = nc.dram_tensor("y_in", [2048, ntok], BF16, kind="ExternalInput").ap()
            w_o = nc.dram_tensor("w_o", [2048, D], F32, kind="ExternalInput").ap()
            g_y = nc.dram_tensor("g_y", [128, 8], F32, kind="ExternalInput").ap()
        wgu, wd, gn = [], [], []
        for i in range(n_ffn):
            wgu.append(nc.dram_tensor("wgu%d" % i, [D, 2 * D_FF], F32, kind="ExternalInput").ap())
            wd.append(nc.dram_tensor("wd%d" % i, [D_FF, D], F32, kind="ExternalInput").ap())
            gn.append(nc.dram_tensor("gn%d" % i, [128, 8], F32, kind="ExternalInput").ap())
        if out_h:
            h_out = nc.dram_tensor("h_out", [D, ntok], BF16, kind="ExternalOutput").ap()
        if out_final:
            g_f = nc.dram_tensor("g_f", [128, 8], F32, kind="ExternalInput").ap()
            o_fin = nc.dram_tensor("o_fin", [D, ntok], F32, kind="ExternalOutput").ap()
    else:
        nc = ctx["nc"]
        x_in, x_out = ctx["x_in"], ctx["x_out"]
        y_load = ctx.get("y_load")
        w_o, g_y = ctx.get("w_o"), ctx.get("g_y")
        wgu, wd, gn = ctx["wgu"], ctx["wd"], ctx["gn"]
        h_out, g_f, o_fin = ctx.get("h_out"), ctx.get("g_f"), ctx.get("o_fin")

    xin_v = x_in.rearrange("(c p) n -> p c n", p=128)
    xout_v = x_out.rearrange("(c p) n -> p c n", p=128)

    es = ExitStack()
    with es:
        if ctx is None:
            em = Em(nc, es)
        else:
            em = ctx["em"]
            em.pes = es
        W1 = em.sbuf("W1", [128, KC, 2 * D_FF], BF16)
        W2 = em.sbuf("W2", [128, FC, D], BF16)
        SC = 1408
        NSTG = 3
        stg = [em.sbuf("stg%d" % i, [128, SC], F32) for i in range(NSTG)]
        bStg = [Buf("stg%d" % i) for i in range(NSTG)]
        xt = [em.sbuf("xt%d" % i, [128, KC, TT], F32) for i in range(2)]
        bX = [Buf("xt%d" % i) for i in range(2)]
        hs = [em.sbuf("hs%d" % i, [128, KC, TT], BF16) for i in range(2)]
        bH = [Buf("hs%d" % i) for i in range(2)]
        NACT = 4
        actb = em.sbuf("actb", [128, NACT, TT], BF16)
        bAct = [Buf("act%d" % i) for i in range(NACT)]
        sgt = em.sbuf("sgt", [128, 2, TT], F32)
        bSg = [Buf("sg%d" % i) for i in range(2)]
        rstd = em.sbuf("rstd", [128, 2, TT], F32)
        bR = [Buf("rstd%d" % i) for i in range(2)]
        ones = em.sbuf("ones", [128, 128], BF16)
        bOnes = Buf("ones")
        gsb = em.sbuf("gsb", [128, 4, 8], F32)
        bG = Buf("gsb")
        if has_y:
            yt = em.sbuf("yt", [128, 16, TT], BF16)
            bY = Buf("yt")
        if out_final:
            fo = em.sbuf("fo", [128, KC, TT], F32)
            bFo = Buf("fo")
        assert TT <= 256
        if ctx is None:
            pbank = [em.psum("pb%d" % i, [128, 512], F32) for i in range(8)]
        else:
            pbank = ctx["banks"]

        def ph(i):
            return pbank[i // 2][:, (i % 2) * 256:(i % 2) * 256 + TT]
        bPb = [Buf("pb%d" % i, True) for i in range(8)] if ctx is None else ctx["bbanks"]
        bP = [bPb[i // 2] for i in range(16)]
        bW1 = [Buf("W1p%d" % i) for i in range(4)]
        bW2 = [Buf("W2p%d" % i) for i in range(2)]
        bDx = [Buf("dx%d" % i) for i in range(NT)]

        em.op("dve", lambda e: e.memset(ones[:], 1.0), writes=[bOnes])
        em.dma("sp", gsb[:, 0, :], (g_y if has_y else gn[0])[:, :], bG, writes=[bG])
        for i in range(n_ffn):
            em.dma("sp", gsb[:, 1 + i, :], gn[i][:, :], bG, writes=[bG])
        if out_final:
            em.dma("sp", gsb[:, 3, :], g_f[:, :], bG, writes=[bG])

        stg_i = [0]

        def load_piece(dst_ap, src_ap, ncols, scale_ap, wbuf):
            s = stg_i[0] % NSTG
            stg_i[0] += 1
            q, ce = (("sp", "dve"), ("act", "act"), ("pool", "dve"))[s]
            em.dma(q, stg[s][:, 0:ncols], src_ap, bStg[s], writes=[bStg[s]])
            if ce == "act":
                if scale_ap is None:
                    em.op("act", lambda e: e.copy(dst_ap, stg[s][:, 0:ncols]), reads=[bStg[s]], writes=[wbuf])
                else:
                    em.op("act", lambda e: e.activation(out=dst_ap, in_=stg[s][:, 0:ncols], func=AF.Identity,
                                                        scale=scale_ap), reads=[bStg[s], bG], writes=[wbuf])
            elif scale_ap is None:
                em.op("dve", lambda e: e.tensor_copy(dst_ap, stg[s][:, 0:ncols]), reads=[bStg[s]], writes=[wbuf])
            else:
                em.op("dve", lambda e: e.tensor_scalar(dst_ap, stg[s][:, 0:ncols], scale_ap, None, ALU.mult),
                      reads=[bStg[s], bG], writes=[wbuf])

        def load_ffn_w1_piece(i, pc):
            for c in range(KC):
                load_piece(W1[:, c, pc * SC:(pc + 1) * SC], wgu[i][c * 128:(c + 1) * 128, pc * SC:(pc + 1) * SC], SC,
                           gsb[:, 1 + i, c:c + 1], bW1[pc])

        def load_ffn_w2_piece(i, pc):
            for j in range(pc * 11, pc * 11 + 11):
                load_piece(W2[:, j, :], wd[i][j * 128:(j + 1) * 128, :], D, None, bW2[pc])

        def load_wout():
            for j in range(16):
                load_piece(W2[:, j, :], w_o[j * 128:(j + 1) * 128, :], D,
                           gsb[:, 0, j:j + 1] if j < 8 else None, bW2[j // 8])

        xcnt = [0]

        def stats(slot, rs, nparts=1, src=None, srcbuf=None):
            src = xt[slot] if src is None else src
            srcbuf = bX[slot] if srcbuf is None else srcbuf
            hslot = slot
            em.op("act", lambda e: e.activation(out=hs[hslot][:, :, :], in_=src[:, 0:KC, :], func=AF.Square),
                  reads=[srcbuf], writes=[bH[hslot]])
            for c in range(KC):
                em.op("pe", lambda e, c=c: e.matmul(ph(14 + rs), lhsT=ones[:, :], rhs=hs[hslot][:, c, :],
                                                  start=(c == 0), stop=(c == KC - 1)),
                      reads=[bH[hslot], bOnes], writes=[bP[14 + rs]])
            em.op("act", lambda e: e.activation(out=rstd[:, rs, :], in_=ph(14 + rs), func=AF.Sqrt, bias=EPS,
                                                scale=1.0 / D), reads=[bP[14 + rs]], writes=[bR[rs]])
            em.op("dve", lambda e: e.reciprocal(rstd[:, rs, :], rstd[:, rs, :]), reads=[bR[rs]], writes=[bR[rs]])

        def make_h(slot, rs):
            em.op("dve", lambda e: e.tensor_tensor(hs[slot][:, :, :], xt[slot][:, :, :],
                                                    rstd[:, rs, :].unsqueeze(1).to_broadcast([128, KC, TT]), ALU.mult),
                  reads=[bX[slot], bR[rs]], writes=[bH[slot]])

        def load_x(phase_idx, i, slot):
            if phase_idx == 0:
                em.dma("sp", xt[slot][:, :, :], xin_v[:, :, i * TT:(i + 1) * TT], bX[slot], writes=[bX[slot]])
            else:
                em.dma("sp", xt[slot][:, :, :], xout_v[:, :, i * TT:(i + 1) * TT], bX[slot], reads=[bDx[i]],
                       writes=[bX[slot]])

        def store_x(i, slot):
            em.dma("sp", xout_v[:, :, i * TT:(i + 1) * TT], xt[slot][:, :, :], bX[slot], reads=[bX[slot]],
                   writes=[bDx[i]])

        def post(i, slot):
            if out_h:
                stats(slot, slot)
                make_h(slot, slot)
                em.dma("sp", h_out.rearrange("(c p) n -> p c n", p=128)[:, :, i * TT:(i + 1) * TT], hs[slot][:, :, :],
                       bH[slot], reads=[bH[slot]])
            if out_final:
                stats(slot, slot)
                for c in range(KC):
                    em.op("dve", lambda e, c=c: e.scalar_tensor_tensor(out=fo[:, c, :], in0=xt[slot][:, c, :],
                                                                     scalar=gsb[:, 3, c:c + 1], in1=rstd[:, slot, :],
                                                                     op0=ALU.mult, op1=ALU.mult),
                          reads=[bX[slot], bR[slot], bG], writes=[bFo])
                em.dma("sp", o_fin.rearrange("(c p) n -> p c n", p=128)[:, :, i * TT:(i + 1) * TT], fo[:, :, :],
                       bFo, reads=[bFo])

        phases = (["wout"] if has_y else []) + [("ffn", i) for i in range(n_ffn)]
        for pi, phs in enumerate(phases):
            last = (pi == len(phases) - 1)
            if phs == "wout":
                load_wout()
                for i in range(NT):
                    slot = xcnt[0] % 2
                    xcnt[0] += 1
                    load_x(pi, i, slot)
                    if y_load is None:
                        em.dma("sp", yt[:, :, :], y_in.rearrange("(c p) n -> p c n", p=128)[:, :, i * TT:(i + 1) * TT],
                               bY, writes=[bY])
                    else:
                        y_load(em, yt, bY, i)
                    em.op("act", lambda e: e.activation(out=hs[slot][:, :, :], in_=yt[:, 0:8, :], func=AF.Square),
                          reads=[bY], writes=[bH[slot]])
                    for g in range(2):
                        for c in range(4):
                            em.op("pe", lambda e, g=g, c=c: e.matmul(ph(14 + g), lhsT=ones[:, :],
                                                                    rhs=hs[slot][:, 4 * g + c, :], start=(c == 0),
                                                                    stop=(c == 3)),
                                  reads=[bH[slot], bOnes], writes=[bP[14 + g]])
                        em.op("act", lambda e, g=g: e.activation(out=rstd[:, g, :], in_=ph(14 + g), func=AF.Sqrt,
                                                                 bias=EPS, scale=1.0 / 512), reads=[bP[14 + g]],
                              writes=[bR[g]])
                        em.op("dve", lambda e, g=g: e.reciprocal(rstd[:, g, :], rstd[:, g, :]), reads=[bR[g]],
                              writes=[bR[g]])
                        em.op("dve", lambda e, g=g: e.tensor_tensor(
                            yt[:, 4 * g:4 * g + 4, :], yt[:, 4 * g:4 * g + 4, :],
                            rstd[:, g, :].unsqueeze(1).to_broadcast([128, 4, TT]), ALU.mult),
                            reads=[bY, bR[g]], writes=[bY])
                    for d in range(8):
                        for j in range(16):
                            em.op("pe", lambda e, d=d, j=j: e.matmul(ph(d), lhsT=W2[:, j, d * 128:(d + 1) * 128],
                                                                    rhs=yt[:, j, :], start=(j == 0), stop=(j == 15)),
                                  reads=[bY, bW2[j // 8]], writes=[bP[d]])
                        em.op("dve", lambda e, d=d: e.tensor_tensor(xt[slot][:, d, :], xt[slot][:, d, :], ph(d),
                                                                     ALU.add), reads=[bP[d], bX[slot]],
                              writes=[bX[slot]])
                    if last:
                        post(i, slot)
                    store_x(i, slot)
            else:
                fi = phs[1]
                for pc in (0, 2):
                    load_ffn_w1_piece(fi, pc)
                load_ffn_w2_piece(fi, 0)
                for pc in (1, 3):
                    load_ffn_w1_piece(fi, pc)
                load_ffn_w2_piece(fi, 1)

                def prep(i):
                    slot = xcnt[0] % 2
                    xcnt[0] += 1
                    load_x(pi, i, slot)
                    stats(slot, slot)
                    make_h(slot, slot)
                    return slot

                slot_next = prep(0)
                for i in range(NT):
                    slot = slot_next
                    for j in range(FC + 1):
                        if j < FC:
                            pr = 8 + 2 * (j % 3)
                            for half, off in ((0, 0), (1, D_FF)):
                                for c in range(KC):
                                    em.op("pe", lambda e, c=c, half=half, off=off, j=j, pr=pr: e.matmul(
                                        ph(pr + half), lhsT=W1[:, c, off + j * 128:off + (j + 1) * 128],
                                        rhs=hs[slot][:, c, :], start=(c == 0), stop=(c == KC - 1)),
                                        reads=[bH[slot], bW1[2 * half + (j // 11)]], writes=[bP[pr + half]])
                            sg = j % 2
                            em.op("act", lambda e, pr=pr, sg=sg: e.activation(out=sgt[:, sg, :], in_=ph(pr),
                                                                            func=AF.Silu), reads=[bP[pr]],
                                  writes=[bSg[sg]])
                            a = j % NACT
                            em.op("dve", lambda e, pr=pr, sg=sg, a=a: e.tensor_tensor(actb[:, a, :], sgt[:, sg, :],
                                                                                   ph(pr + 1), ALU.mult),
                                  reads=[bSg[sg], bP[pr + 1]], writes=[bAct[a]])
                        if j >= 1:
                            jj = j - 1
                            a = jj % NACT
                            for d in range(8):
                                em.op("pe", lambda e, d=d, jj=jj, a=a: e.matmul(
                                    ph(d), lhsT=W2[:, jj, d * 128:(d + 1) * 128], rhs=actb[:, a, :],
                                    start=(jj == 0 and d % 2 == 0), stop=(jj == FC - 1 and d % 2 == 1)),
                                    reads=[bAct[a], bW2[jj // 11]], writes=[bP[d]])
                        if j == 10 and i + 1 < NT:
                            slot_next = prep(i + 1)
                    for d in range(8):
                        em.op("dve", lambda e, d=d: e.scalar_tensor_tensor(out=xt[slot][:, d, :], in0=ph(d),
                                                                         scalar=0.5, in1=xt[slot][:, d, :],
                                                                         op0=ALU.mult, op1=ALU.add),
                              reads=[bP[d], bX[slot]], writes=[bX[slot]])
                    if last:
                        post(i, slot)
                    store_x(i, slot)
        allb = bX + bH + bDx + ([bFo] if out_final else [])
        em.wait_bufs("sp", allb)
        if ctx is not None:
            em.barrier()
            em.end_phase()
            em.pes = None
    return nc


ST = 512
NCOL = 1542
CHUNKS = [("z0", 0, 128), ("z1", 128, 128), ("x0", 256, 128), ("x1", 384, 128), ("B", 512, 64), ("C", 576, 64),
          ("dt", 640, 4), ("mx", 644, 128), ("mo", 772, 128), ("mi", 900, 1), ("mf", 901, 1),
          ("rr", 902, 128), ("rk", 1030, 128), ("rv", 1158, 128), ("rwl", 1286, 64), ("ral", 1350, 64),
          ("rgl", 1414, 128)]
KDEC = float(np.exp(-0.5))
STAGES = {"ssd", "ml", "rw"}
CUT = 99


class _Cut(Exception):
    pass


def _ck(k):
    if CUT == k:
        raise _Cut()


def _cf_layout():
    items = [("mask128", 128), ("identf", 128), ("onesf", 128), ("sel4", 512), ("m128", ST), ("m64", ST),
             ("rneg", ST), ("maskAB", 512), ("nmasklo", 128), ("signm", 128), ("blkf", 128)]
    off, o = {}, 0
    for n, w in items:
        off[n] = (o, w)
        o += w
    return off, o


def _prm_layout():
    items = [("cwx", 8), ("cbx", 2), ("cwB", 4), ("cbB", 1), ("cwC", 4), ("cbC", 1), ("dtb", 1), ("alog", 1),
             ("Dcol", 2), ("cwm", 4), ("cbm", 1), ("bi", 1), ("bf", 1), ("nm", 1), ("mu3", 3), ("mu2", 2),
             ("mug", 1), ("w0", 1), ("a0", 1), ("kk", 1), ("ka", 1), ("rkc", 1), ("lnw", 1), ("lnb", 1)]
    off, o = {}, 0
    for n, w in items:
        off[n] = (o, w)
        o += w
    return off, o


def mix_consts():
    off, tot = _cf_layout()
    cf = np.zeros((128, tot), np.float32)

    def put(n, a):
        o, w = off[n]
        cf[0:a.shape[0], o:o + w] = a
    i = np.arange(128)
    put("mask128", (i[None, :] >= i[:, None]).astype(np.float32))
    put("identf", np.eye(128, dtype=np.float32))
    put("onesf", np.ones((128, 128), np.float32))
    sel = np.zeros((4, 4, 128), np.float32)
    for h in range(4):
        sel[h, h, :] = 1.0
    put("sel4", sel.reshape(4, 512))
    t = np.arange(ST)
    put("m128", np.tile((t % 128 != 0).astype(np.float32)[None, :], (128, 1)))
    put("m64", np.tile((t % 64 != 0).astype(np.float32)[None, :], (128, 1)))
    put("rneg", np.tile(np.where(t % 128 == 0, -1e30, 0.0).astype(np.float32)[None, :], (128, 1)))
    j = np.arange(64)
    strict = (j[None, :] > j[:, None]).astype(np.float32)
    incl = (j[None, :] >= j[:, None]).astype(np.float32)
    one = np.concatenate([strict, incl, strict, incl], 1)
    put("maskAB", np.concatenate([np.concatenate([one, one], 0), np.zeros((128, 256), np.float32)], 1))
    lo = -(j[None, :] < j[:, None]).astype(np.float32)
    put("nmasklo", np.concatenate([np.concatenate([lo, lo], 0), np.zeros((128, 64), np.float32)], 1))
    put("signm", np.concatenate([np.ones((128, 64), np.float32), -np.ones((128, 64), np.float32)], 1))
    blk = np.zeros((128, 128), np.float32)
    blk[0:64, 0:64] = 1.0
    blk[64:, 64:] = 1.0
    put("blkf", blk)
    return cf


class _Tile:
    def __init__(self, t, b):
        self.t = t
        self.b = b

    def f(self, p=128, n=ST, off=0, p0=0):
        return self.t[p0:p0 + p, off:off + n]

    def h(self, p=128, n=ST, off=0, p0=0):
        return self.t[:, :].bitcast(BF16)[p0:p0 + p, off:off + n]


def build_mix(T, ctx=None):
    NS = T // ST
    cfo, cftot = _cf_layout()
    pro, prtot = _prm_layout()
    if ctx is None:
        nc = bass.Bass("TRN2", target_bir_lowering=False)
        h_in = nc.dram_tensor("h_in", [D_MODEL, T], BF16, kind="ExternalInput").ap()
        w_sel = nc.dram_tensor("w_sel", [D_MODEL, NCOL], F32, kind="ExternalInput").ap()
        g_mix = nc.dram_tensor("g_mix", [128, 8], F32, kind="ExternalInput").ap()
        cf_d = nc.dram_tensor("cf", [128, cftot], F32, kind="ExternalInput").ap()
        prm_d = nc.dram_tensor("prm", [128, prtot], F32, kind="ExternalInput").ap()
        wsm_d = nc.dram_tensor("wsm", [128, 768], F32, kind="ExternalInput").ap()
        y_out = nc.dram_tensor("y_out", [512, T], BF16, kind="ExternalOutput").ap()
        hin_v = h_in.rearrange("(c p) n -> p c n", p=128)

        def h_src(t):
            return hin_v[:, :, t * ST:(t + 1) * ST]

        def y_dst(r0, r1, t):
            return y_out[r0:r1, t * ST:(t + 1) * ST]
    else:
        nc = ctx["nc"]
        w_sel, g_mix, cf_d, prm_d, wsm_d, y_out = (ctx[k] for k in ("w_sel", "g_mix", "cf", "prm", "wsm", "y_out"))
        h_src = ctx["h_src"]
        y_dst = ctx["y_dst"]

    es = ExitStack()
    with es:
        if ctx is None:
            em = Em(nc, es)
        else:
            em = ctx["em"]
            em.pes = es
        V = em.op
        Win = em.sbuf("Win", [128, 8, NCOL], BF16)
        bWin = Buf("Win")
        ht = [em.sbuf("ht%d" % i, [128, 8, ST], BF16) for i in range(2)]
        bHt = [Buf("ht%d" % i) for i in range(2)]
        cf = em.sbuf("cfs", [128, cftot], F32)
        bC = Buf("cf")
        prm = em.sbuf("prms", [128, prtot + 16], F32)
        bPr = Buf("prm")
        wsm = em.sbuf("wsms", [128, 768], BF16)
        bWs = Buf("wsm")
        cb = em.sbuf("cbs", [128, 448], BF16)
        bCb = Buf("cb")
        gm = em.sbuf("gm", [128, 8], F32)
        bGm = Buf("gm")
        halo = em.sbuf("halo", [128, 16, 4], F32)
        bHalo = [Buf("halo%d" % i) for i in range(16)]
        NG = 56
        TW = ST + 8
        gt = [_Tile(em.sbuf("g%d" % i, [128, TW], F32), Buf("g%d" % i)) for i in range(NG)]
        if ctx is None:
            banks = [em.psum("pb%d" % i, [128, 512], F32) for i in range(8)]
            bBk = [Buf("pb%d" % i, True) for i in range(8)]
        else:
            banks, bBk = ctx["banks"], ctx["bbanks"]
        bki = [0]
        reserved = set()

        def bank(keep=False):
            while True:
                i = bki[0] % 8
                bki[0] += 1
                if i not in reserved:
                    break
            if keep:
                reserved.add(i)
            return banks[i], bBk[i]

        def unkeep(bb):
            reserved.discard(bBk.index(bb))
        from collections import deque
        free = deque(gt)
        live = []

        def G():
            g = free.popleft()
            live.append(g)
            return g

        def R(*ts):
            for g in ts:
                live.remove(g)
                free.append(g)

        def C(name, p=128, p0=0):
            o, w = cfo[name]
            return cf[p0:p0 + p, o:o + w]

        def P(name, j=0, p=128, p0=0):
            o, w = pro[name]
            return prm[p0:p0 + p, o + j:o + j + 1]
        identb, onesb, blkb = cb[:, 0:128], cb[:, 128:256], cb[:, 256:384]

        em.dma("sp", cf[:, :], cf_d[:, :], bC, writes=[bC])
        em.dma("sp", prm[:, 0:prtot], prm_d[:, :], bPr, writes=[bPr])
        em.dma("sp", gm[:, :], g_mix[:, :], bGm, writes=[bGm])
        V("dve", lambda e: e.tensor_copy(cb[:, 0:128], C("identf")), [bC], [bCb])
        V("dve", lambda e: e.tensor_copy(cb[:, 128:256], C("onesf")), [bC], [bCb])
        V("dve", lambda e: e.tensor_copy(cb[:, 256:384], C("blkf")), [bC], [bCb])
        em.dma("sp", gt[0].t[:, 0:384], wsm_d[:, 0:384], gt[0].b, writes=[gt[0].b])
        em.dma("sp", gt[1].t[:, 0:384], wsm_d[:, 384:768], gt[1].b, writes=[gt[1].b])
        V("dve", lambda e: e.tensor_copy(wsm[:, 0:384], gt[0].t[:, 0:384]), [gt[0].b], [bWs])
        V("dve", lambda e: e.tensor_copy(wsm[:, 384:768], gt[1].t[:, 0:384]), [gt[1].b], [bWs])
        wq_b, wk_b, wv_b = wsm[:, 0:128], wsm[:, 128:256], wsm[:, 256:384]
        wup_b, aup_b, gup_b = wsm[0:64, 384:512], wsm[0:64, 512:640], wsm[:, 640:768]
        for c in range(8):
            for hf in range(3):
                g = gt[2 + (c * 3 + hf) % 4]
                c0 = hf * 514
                em.dma("sp", g.t[:, 0:514], w_sel[c * 128:(c + 1) * 128, c0:c0 + 514], g.b, writes=[g.b])
                V(["dve", "pool", "act"][hf] if hf < 2 else "dve",
                  lambda e, g=g, c=c, c0=c0: e.tensor_scalar(Win[:, c, c0:c0 + 514], g.t[:, 0:514], gm[:, c:c + 1],
                                                           None, ALU.mult), [g.b, bGm], [bWin])
        o_mu3 = pro["mu3"][0]
        V("dve", lambda e: e.tensor_scalar(prm[:, prtot:prtot + 6], prm[:, o_mu3:o_mu3 + 6], -1.0, 1.0, ALU.mult,
                                          ALU.add), [bPr], [bPr])
        V("act", lambda e: e.activation(out=prm[0:4, prtot + 6:prtot + 7], in_=P("alog", 0, 4), func=AF.Exp), [bPr],
          [bPr])
        V("dve", lambda e: e.tensor_scalar(prm[0:4, prtot + 6:prtot + 7], prm[0:4, prtot + 6:prtot + 7], -1.0, None,
                                          ALU.mult), [bPr], [bPr])
        V("dve", lambda e: e.tensor_scalar(prm[0:1, prtot + 7:prtot + 8], P("bf", 0, 1), -1.0, None, ALU.mult),
          [bPr], [bPr])
        negA = prm[0:4, prtot + 6:prtot + 7]
        nbf = prm[0:1, prtot + 7:prtot + 8]

        def OMU(j, p=128):
            return prm[0:p, prtot + j:prtot + j + 1]

        stt = em.sbuf("stt", [128, 1024], F32)
        bSs, bSm, bSr, bMc = Buf("Sssd"), Buf("Sml"), Buf("Srw"), Buf("mcar")
        S_f = stt[0:64, 0:256]
        Cst = stt[:, 256:385]
        Srw = stt[:, 400:464]
        mcar = stt[0:1, 480:481]
        stb = em.sbuf("stb", [128, 512], BF16)
        bSsb, bSrb = Buf("Sssdb"), Buf("Srwb")
        S_b = stb[0:64, 0:256]
        Srw_b = stb[:, 256:320]
        vtok = em.sbuf("vtok", [128, 4, 256], BF16)
        bVt = Buf("vtok")
        nbc = em.sbuf("nbc", [128, 128], BF16)
        Cst_b = stb[:, 320:448]
        bSmb = Buf("Smlb")
        bNb = Buf("nbc")
        V("dve", lambda e: e.memset(stt[:, :], 0.0), [], [bSs, bSm, bSr, bMc])
        V("dve", lambda e: e.memset(stb[:, :], 0.0), [], [bSsb, bSrb, bSmb])
        V("dve", lambda e: e.memset(vtok[:, :, :], 1.0), [], [bVt])
        V("dve", lambda e: e.memset(nbc[:, :], 0.0), [], [bNb])
        V("dve", lambda e: e.memset(halo[:, :, :], 0.0), [], bHalo)

        def load_h(t, slot):
            em.dma("sp", ht[slot][:, :, :], h_src(t), bHt[slot], writes=[bHt[slot]])

        iden2 = em.sbuf("iden2", [128, 64], F32)
        bI2 = Buf("iden2")
        V("dve", lambda e: e.tensor_tensor(iden2[:, :], C("identf")[:, 0:64], C("identf")[:, 64:128], ALU.add), [bC],
          [bI2])
        V("dve", lambda e: e.tensor_copy(cb[:, 384:448], iden2[:, :]), [bI2], [bCb])

        def mm(out, lhsT, rhs, r, w, start=True, stop=True):
            V("pe", lambda e: e.matmul(out, lhsT=lhsT, rhs=rhs, start=start, stop=stop), r, w)

        def v3(ap, l):
            return ap.rearrange("p (c l) -> p c l", l=l)

        def proj(t):
            slot = t % 2
            if t + 1 < NS:
                load_h(t + 1, 1 - slot)
            raw = {}
            for ci, (name, c0, M) in enumerate(CHUNKS):
                pb, bb = bank()
                for c in range(8):
                    mm(pb[0:M, :], Win[:, c, c0:c0 + M], ht[slot][:, c, :], [bWin, bHt[slot]], [bb], c == 0, c == 7)
                g = G()
                raw[name] = g
                if name in ("z0", "z1"):
                    V("act", lambda e: e.activation(out=g.f(), in_=pb[:, :], func=AF.Silu), [bb], [g.b])
                elif name == "mo":
                    V("act", lambda e: e.activation(out=g.f(), in_=pb[:, :], func=AF.Sigmoid), [bb], [g.b])
                elif name == "dt":
                    V("act", lambda e: e.activation(out=g.f(4), in_=pb[0:4, :], func=AF.Exp, bias=P("dtb", 0, 4)),
                      [bb, bPr], [g.b])
                    V("act", lambda e: e.activation(out=g.f(4), in_=g.f(4), func=AF.Ln, bias=1.0), [g.b], [g.b])
                elif name == "mi":
                    V("act", lambda e: e.activation(out=g.f(1), in_=pb[0:1, :], func=AF.Identity, bias=P("bi", 0, 1)),
                      [bb, bPr], [g.b])
                elif name == "mf":
                    V("act", lambda e: e.activation(out=g.f(1), in_=pb[0:1, :], func=AF.Exp, scale=-1.0, bias=nbf),
                      [bb, bPr], [g.b])
                    V("act", lambda e: e.activation(out=g.f(1), in_=g.f(1), func=AF.Ln, bias=1.0), [g.b], [g.b])
                else:
                    hw = 3 if name in ("x0", "x1", "B", "C", "mx") else 1
                    hb = bHalo[ci % 16]
                    V("act", lambda e: e.copy(g.f(M, hw, 4 - hw), halo[0:M, ci % 16, 4 - hw:4]), [hb], [g.b])
                    if ci % 2:
                        V("act", lambda e: e.copy(g.f(M, ST, 4), pb[0:M, :]), [bb], [g.b])
                    else:
                        V("dve", lambda e: e.tensor_copy(g.f(M, ST, 4), pb[0:M, :]), [bb], [g.b])
                    V("act", lambda e: e.copy(halo[0:M, ci % 16, 4 - hw:4], g.f(M, hw, 4 + ST - hw)), [g.b],
                      [hb])
            return raw

        load_h(0, 0)
        raw_next = proj(0)
        for t in range(NS):
            raw = raw_next
            raw_next = None
            tsl = slice(t * ST, (t + 1) * ST)
            conv_in = [("x0", 128, lambda k: P("cwx", k), P("cbx", 0)),
                       ("x1", 128, lambda k: P("cwx", 4 + k), P("cbx", 1)),
                       ("B", 64, lambda k: P("cwB", k, 64), P("cbB", 0, 64)),
                       ("C", 64, lambda k: P("cwC", k, 64), P("cbC", 0, 64)),
                       ("mx", 128, lambda k: P("cwm", k), P("cbm", 0))]
            conv_out = {}
            for (name, M, wk, bcol) in conv_in:
                r = raw[name]
                acc = G()
                V("act", lambda e: e.activation(out=acc.f(M), in_=r.f(M, ST, 4), func=AF.Identity, bias=bcol,
                                                scale=wk(3)), [r.b, bPr], [acc.b])
                for k in range(3):
                    V("dve", lambda e: e.scalar_tensor_tensor(out=acc.f(M), in0=r.f(M, ST, 1 + k), scalar=wk(k),
                                                              in1=acc.f(M), op0=ALU.mult, op1=ALU.add),
                      [r.b, bPr, acc.b], [acc.b])
                o = G()
                V("act", lambda e: e.activation(out=o.h(M), in_=acc.f(M), func=AF.Silu), [acc.b], [o.b])
                conv_out[name] = o
                R(acc)
                if name != "mx":
                    R(r)
            if "ssd" in STAGES:
                NCH = ST // 128
                sz = [raw["z0"], raw["z1"]]
                xs_b = [conv_out["x0"], conv_out["x1"]]
                B_b, C_b, xc_b = conv_out["B"], conv_out["C"], conv_out["mx"]
                dt = raw["dt"]
                a_t, acum = G(), G()
                V("dve", lambda e: e.tensor_scalar(a_t.f(4), dt.f(4), negA, None, ALU.mult), [dt.b, bPr], [a_t.b])
                V("dve", lambda e: e.tensor_tensor_scan(acum.f(4), C("m128", 4), a_t.f(4), 0.0, ALU.mult, ALU.add),
                  [a_t.b, bC], [acum.b])
                R(a_t)
                pcol, bcol_ = bank()
                for c in range(NCH):
                    mm(pcol[:, c * 4:c * 4 + 4], acum.f(4, 128, c * 128), C("identf", 4)[:, 0:4], [acum.b, bC], [bcol_])
                    mm(pcol[:, 16 + c * 4:16 + c * 4 + 4], dt.f(4, 128, c * 128), C("identf", 4)[:, 0:4], [dt.b, bC],
                       [bcol_])
                R(dt)
                cols = G()
                V("dve", lambda e: e.tensor_copy(cols.f(128, 32), pcol[:, 0:32]), [bcol_], [cols.b])
                V("dve", lambda e: e.tensor_scalar(cols.f(128, 16, 96), cols.f(128, 16, 0), -1.0, None, ALU.mult),
                  [cols.b], [cols.b])
                abc = []
                for h in range(4):
                    pa, ba = bank()
                    mm(pa[:, :], C("sel4", 4)[:, h * 128:(h + 1) * 128], acum.f(4), [acum.b, bC], [ba])
                    ab = G()
                    V("act", lambda e: e.copy(ab.f(), pa[:, :]), [ba], [ab.b])
                    abc.append(ab)
                    V("dve", lambda e: e.tensor_copy(cols.t[:, 32 + h:32 + h + 13:4], ab.t[:, 127:512:128]), [ab.b],
                      [cols.b])
                R(acum)
                V("dve", lambda e: e.tensor_tensor(cols.f(128, 16, 48), cols.f(128, 16, 32), cols.f(128, 16, 0),
                                                  ALU.subtract), [cols.b], [cols.b])
                V("act", lambda e: e.activation(out=cols.f(128, 16, 48), in_=cols.f(128, 16, 48), func=AF.Exp), [cols.b],
                  [cols.b])
                V("act", lambda e: e.activation(out=cols.f(128, 16, 80), in_=cols.f(128, 16, 32), func=AF.Exp), [cols.b],
                  [cols.b])
                V("dve", lambda e: e.tensor_tensor(cols.f(128, 16, 64), cols.f(128, 16, 48), cols.f(128, 16, 16),
                                                  ALU.mult), [cols.b], [cols.b])
                cdec = []
                for h in range(4):
                    ex, cd = G(), G()
                    V("act", lambda e: e.activation(out=ex.f(64), in_=abc[h].f(64), func=AF.Exp), [abc[h].b], [ex.b])
                    V("dve", lambda e: e.tensor_tensor(cd.h(64), ex.f(64), C_b.h(64), ALU.mult), [ex.b, C_b.b], [cd.b])
                    R(ex)
                    cdec.append(cd)
                pcb, bcb = bank()
                for c in range(NCH):
                    mm(pcb[:, c * 128:(c + 1) * 128], B_b.h(64, 128, c * 128), C_b.h(64, 128, c * 128), [B_b.b, C_b.b],
                       [bcb])
                CBm = G()
                V("dve", lambda e: e.tensor_tensor(v3(CBm.f(), 128), v3(pcb[:, :], 128),
                                                  C("mask128").unsqueeze(1).to_broadcast([128, NCH, 128]), ALU.mult),
                  [bcb, bC], [CBm.b])
                xdt, xdte, Btok = G(), G(), G()
                for half in range(2):
                    px, bx = bank()
                    pxb = px[:, :].bitcast(BF16)
                    for cc in range(2):
                        c = half * 2 + cc
                        for i in range(2):
                            V("pe", lambda e: e.transpose(pxb[:, cc * 256 + i * 128:cc * 256 + (i + 1) * 128],
                                                          xs_b[i].h(128, 128, c * 128), identb), [xs_b[i].b, bCb], [bx])
                        V("pe", lambda e: e.transpose(pxb[:, 512 + cc * 64:512 + (cc + 1) * 64],
                                                      B_b.h(64, 128, c * 128), identb[0:64, 0:64]), [B_b.b, bCb], [bx])
                    for (dst, coff) in ((xdt, 16), (xdte, 64)):
                        V("dve", lambda e: e.tensor_tensor(
                            v3(dst.h(128, 512, half * 512), 64), v3(pxb[:, 0:512], 64),
                            cols.f(128, 8, coff + half * 8).unsqueeze(2).to_broadcast([128, 8, 64]), ALU.mult),
                            [bx, cols.b], [dst.b])
                    V("act", lambda e: e.copy(Btok.h(128, 128, half * 128), pxb[:, 512:640]), [bx], [Btok.b])
                R(B_b)
                py = [bank(True), bank(True)]
                for c in range(NCH):
                    mts = []
                    for h in range(4):
                        et, mt = G(), G()
                        V("dve", lambda e: e.tensor_scalar(et.f(128, 128), abc[h].f(128, 128, c * 128),
                                                          cols.f(128, 1, 96 + c * 4 + h), 0.0, ALU.add, ALU.min),
                          [abc[h].b, cols.b], [et.b])
                        V("act", lambda e: e.activation(out=et.f(128, 128), in_=et.f(128, 128), func=AF.Exp), [et.b],
                          [et.b])
                        V("dve", lambda e: e.tensor_tensor(mt.h(128, 128), et.f(128, 128), CBm.f(128, 128, c * 128),
                                                            ALU.mult), [et.b, CBm.b], [mt.b])
                        R(et)
                        mts.append(mt)
                    for h in range(4):
                        pyb, byb = py[h // 2]
                        po = (h % 2) * 64
                        mm(pyb[po:po + 64, c * 128:(c + 1) * 128], xdt.h(128, 64, (c * 4 + h) * 64), mts[h].h(128, 128),
                           [xdt.b, mts[h].b], [byb], True, False)
                        mm(pyb[po:po + 64, c * 128:(c + 1) * 128], S_b[:, h * 64:(h + 1) * 64],
                           cdec[h].h(64, 128, c * 128), [bSsb, cdec[h].b], [byb], False, True)
                    R(*mts)
                    ps_, bs_ = bank()
                    for h in range(4):
                        mm(ps_[0:64, h * 64:(h + 1) * 64], Btok.h(128, 64, c * 64), xdte.h(128, 64, (c * 4 + h) * 64),
                           [Btok.b, xdte.b], [bs_])
                    for h in range(4):
                        V("dve", lambda e: e.scalar_tensor_tensor(
                            out=S_f[:, h * 64:(h + 1) * 64], in0=S_f[:, h * 64:(h + 1) * 64],
                            scalar=cols.f(64, 1, 80 + c * 4 + h), in1=ps_[0:64, h * 64:(h + 1) * 64], op0=ALU.mult,
                            op1=ALU.add), [bSs, cols.b, bs_], [bSs])
                    V("act", lambda e: e.copy(S_b, S_f), [bSs], [bSsb])
                R(xdt, xdte, Btok, CBm, cols, C_b, *abc, *cdec)
                for i in range(2):
                    pyb, byb = py[i]
                    tmp, yo = G(), G()
                    V("dve", lambda e: e.scalar_tensor_tensor(out=tmp.f(), in0=xs_b[i].h(), scalar=P("Dcol", i),
                                                              in1=pyb[:, :], op0=ALU.mult, op1=ALU.add),
                      [xs_b[i].b, bPr, byb], [tmp.b])
                    unkeep(byb)
                    V("dve", lambda e: e.tensor_tensor(yo.h(), tmp.f(), sz[i].f(), ALU.mult), [tmp.b, sz[i].b], [yo.b])
                    em.dma("sp", y_dst(i * 128, (i + 1) * 128, t), yo.h(), yo.b, reads=[yo.b])
                    R(tmp, yo, xs_b[i], sz[i])
            if "ml" in STAGES:
                try:
                    osig, li, sp = raw["mo"], raw["mi"], raw["mf"]
                    xc_b = conv_out["mx"]
                    NCH = ST // 128
                    rmx = raw["mx"]
                    mxb = G()
                    V("act", lambda e: e.copy(mxb.h(), rmx.f(128, ST, 4)), [rmx.b], [mxb.b])
                    R(rmx)
                    pq, bq = bank()
                    mm(pq[:, :], wq_b, xc_b.h(), [bWs, xc_b.b], [bq])
                    q_b = G()
                    V("act", lambda e: e.copy(q_b.h(), pq[:, :]), [bq], [q_b.b])
                    pk, bk = bank()
                    mm(pk[:, :], wk_b, xc_b.h(), [bWs, xc_b.b], [bk])
                    k_b = G()
                    V("act", lambda e: e.activation(out=k_b.h(), in_=pk[:, :], func=AF.Identity, scale=128.0 ** -0.5), [bk],
                      [k_b.b])
                    _ck(1)
                    bcum, g_, cmx, mint, sm, il, mt = G(), G(), G(), G(), G(), G(), G()
                    V("dve", lambda e: e.tensor_tensor_scan(bcum.f(1), C("m128", 1), sp.f(1), 0.0, ALU.mult, ALU.subtract),
                      [sp.b, bC], [bcum.b])
                    V("dve", lambda e: e.tensor_tensor(g_.f(1), li.f(1), bcum.f(1), ALU.subtract), [li.b, bcum.b], [g_.b])
                    V("dve", lambda e: e.tensor_tensor_scan(cmx.f(1), C("rneg", 1), g_.f(1), 0.0, ALU.add, ALU.max),
                      [g_.b, bC], [cmx.b])
                    V("dve", lambda e: e.tensor_tensor(mint.f(1), bcum.f(1), cmx.f(1), ALU.add), [bcum.b, cmx.b], [mint.b])
                    V("dve", lambda e: e.tensor_copy(sm.f(1, 4, 0), bcum.t[0:1, 127:512:128]), [bcum.b], [sm.b])
                    V("dve", lambda e: e.tensor_copy(sm.f(1, 4, 4), cmx.t[0:1, 127:512:128]), [cmx.b], [sm.b])
                    V("dve", lambda e: e.tensor_tensor(sm.f(1, 4, 8), sm.f(1, 4, 0), sm.f(1, 4, 4), ALU.add), [sm.b], [sm.b])
                    V("dve", lambda e: e.tensor_copy(sm.f(1, 1, 16), mcar), [bMc, sm.b], [sm.b])
                    for c in range(4):
                        V("dve", lambda e: e.tensor_tensor(sm.f(1, 1, 12 + c), sm.f(1, 1, 16 + c), sm.f(1, 1, c), ALU.add),
                          [sm.b], [sm.b])
                        V("dve", lambda e: e.tensor_tensor(sm.f(1, 1, 12 + c), sm.f(1, 1, 12 + c), sm.f(1, 1, 8 + c),
                                                          ALU.max), [sm.b], [sm.b])
                        if c < 3:
                            V("dve", lambda e: e.tensor_copy(sm.f(1, 1, 17 + c), sm.f(1, 1, 12 + c)), [sm.b], [sm.b])
                    V("dve", lambda e: e.tensor_copy(mcar, sm.f(1, 1, 15)), [sm.b], [bMc])
                    V("dve", lambda e: e.tensor_tensor(sm.f(1, 4, 20), sm.f(1, 4, 0), sm.f(1, 4, 16), ALU.add), [sm.b], [sm.b])
                    V("dve", lambda e: e.tensor_tensor(sm.f(1, 4, 20), sm.f(1, 4, 20), sm.f(1, 4, 12), ALU.subtract), [sm.b],
                      [sm.b])
                    V("dve", lambda e: e.tensor_tensor(sm.f(1, 4, 24), sm.f(1, 4, 8), sm.f(1, 4, 12), ALU.subtract), [sm.b],
                      [sm.b])
                    V("act", lambda e: e.activation(out=sm.f(1, 8, 20), in_=sm.f(1, 8, 20), func=AF.Exp), [sm.b], [sm.b])
                    V("dve", lambda e: e.tensor_tensor(v3(il.f(1), 128), v3(bcum.f(1), 128),
                                                      sm.f(1, 4, 16).unsqueeze(2).to_broadcast([1, 4, 128]), ALU.add),
                      [bcum.b, sm.b], [il.b])
                    V("dve", lambda e: e.tensor_tensor(mt.f(1), il.f(1), mint.f(1), ALU.max), [il.b, mint.b], [mt.b])
                    V("dve", lambda e: e.tensor_tensor(mint.f(1), bcum.f(1), mt.f(1), ALU.subtract), [bcum.b, mt.b], [mint.b])
                    V("dve", lambda e: e.tensor_tensor(il.f(1), il.f(1), mt.f(1), ALU.subtract), [il.b, mt.b], [il.b])
                    V("dve", lambda e: e.tensor_scalar(mt.f(1), mt.f(1), -1.0, None, ALU.mult), [mt.b], [mt.b])
                    V("dve", lambda e: e.tensor_tensor(v3(cmx.f(1), 128), v3(g_.f(1), 128),
                                                      sm.f(1, 4, 4).unsqueeze(2).to_broadcast([1, 4, 128]), ALU.subtract),
                      [g_.b, sm.b], [cmx.b])
                    R(bcum, li, sp)
                    _ck(2)
                    onerow = C("onesf", 1)
                    pc_, bc_ = bank()
                    for c in range(NCH):
                        mm(pc_[:, 32 + 2 * c:34 + 2 * c], g_.f(1, 128, c * 128), onerow[:, 0:2], [g_.b, bC], [bc_])
                        mm(pc_[:, 40 + 2 * c:42 + 2 * c], cmx.f(1, 128, c * 128), onerow[:, 0:2], [cmx.b, bC], [bc_])
                    mm(pc_[:, 8:16], onerow[:, 0:128], sm.f(1, 8, 20), [sm.b, bC], [bc_])
                    mcol = G()
                    V("dve", lambda e: e.tensor_copy(mcol.f(128, 8, 8), pc_[:, 8:16]), [bc_], [mcol.b])
                    V("dve", lambda e: e.tensor_copy(mcol.f(128, 8, 0), pc_[:, 32:48:2]), [bc_], [mcol.b])
                    V("act", lambda e: e.activation(out=mcol.f(128, 4, 4), in_=mcol.f(128, 4, 4), func=AF.Exp), [mcol.b],
                      [mcol.b])
                    V("dve", lambda e: e.tensor_scalar(mcol.f(128, 4, 16), mcol.f(128, 4, 4), 128.0 ** -0.5, None, ALU.mult),
                      [mcol.b], [mcol.b])
                    R(g_, cmx, sm)
                    _ck(3)
                    pR1, bR1 = bank(True)
                    mm(pR1[:, :], onerow[:, 0:128], mint.f(1), [mint.b, bC], [bR1])
                    pR2, bR2 = bank()
                    mm(pR2[:, :], onerow[:, 0:128], il.f(1), [il.b, bC], [bR2])
                    wint, qw, emt = G(), G(), G()
                    V("act", lambda e: e.activation(out=wint.f(), in_=pR2[:, :], func=AF.Exp), [bR2], [wint.b])
                    V("dve", lambda e: e.tensor_tensor(qw.h(), q_b.h(), wint.f(), ALU.mult), [q_b.b, wint.b], [qw.b])
                    pR3, bR3 = bank()
                    mm(pR3[:, :], onerow[:, 0:128], mt.f(1), [mt.b, bC], [bR3])
                    V("act", lambda e: e.activation(out=emt.f(), in_=pR3[:, :], func=AF.Exp), [bR3], [emt.b])
                    R(wint, mint, il, mt)
                    _ck(4)
                    kwt = G()
                    pnum, bnum = bank(True)
                    pden, bden = bank(True)
                    for c in range(NCH):
                        cs = slice(c * 128, (c + 1) * 128)
                        pv, bv = bank()
                        mm(pv[:, 0:128], mxb.h(128, 128, c * 128), wv_b, [mxb.b, bWs], [bv])
                        mm(pv[:, 128:256], xc_b.h(128, 128, c * 128), wk_b, [xc_b.b, bWs], [bv])
                        V("act", lambda e: e.copy(vtok[:, c, 0:128], pv[:, 0:128]), [bv], [bVt])
                        V("dve", lambda e: e.tensor_scalar(kwt.h(128, 128, c * 128), pv[:, 128:256],
                                                          mcol.f(128, 1, 16 + c), None, ALU.mult), [bv, mcol.b],
                          [kwt.b])
                        _ck(6)
                        et, wm, qkw, tmpc = G(), G(), G(), G()
                        V("dve", lambda e: e.tensor_scalar(et.f(128, 128), pR1[:, cs], mcol.f(128, 1, c), 0.0, ALU.add,
                                                          ALU.min), [bR1, mcol.b], [et.b])
                        V("act", lambda e: e.activation(out=et.f(128, 128), in_=et.f(128, 128), func=AF.Exp), [et.b], [et.b])
                        V("dve", lambda e: e.tensor_tensor(wm.f(128, 128), et.f(128, 128), C("mask128"), ALU.mult),
                          [et.b, bC], [wm.b])
                        _ck(7)
                        pqk, bqk = bank()
                        mm(pqk[:, 0:128], k_b.h(128, 128, c * 128), q_b.h(128, 128, c * 128), [k_b.b, q_b.b], [bqk])
                        V("dve", lambda e: e.tensor_tensor(qkw.h(128, 128), pqk[:, 0:128], wm.f(128, 128), ALU.mult),
                          [bqk, wm.b], [qkw.b])
                        _ck(8)
                        mm(pnum[:, cs], vtok[:, c, 0:128], qkw.h(128, 128), [bVt, qkw.b], [bnum], True, False)
                        mm(pnum[:, cs], Cst_b[:, 0:128], qw.h(128, 128, c * 128), [bSmb, qw.b], [bnum], False, True)
                        mm(pden[:, cs], onesb, qkw.h(128, 128), [bCb, qkw.b], [bden], True, False)
                        mm(pden[:, cs], nbc[:, :], qw.h(128, 128, c * 128), [bNb, qw.b], [bden], False, True)
                        _ck(9)
                        pcl, bcl = bank()
                        mm(pcl[:, 0:160], kwt.h(128, 128, c * 128), vtok[:, c, 0:160], [kwt.b, bVt], [bcl])
                        V("dve", lambda e: e.tensor_scalar(tmpc.f(128, 129), pcl[:, 0:129], mcol.f(128, 1, 12 + c), None,
                                                          ALU.mult), [bcl, mcol.b], [tmpc.b])
                        V("dve", lambda e: e.scalar_tensor_tensor(out=Cst, in0=Cst, scalar=mcol.f(128, 1, 8 + c),
                                                                  in1=tmpc.f(128, 129), op0=ALU.mult, op1=ALU.add),
                          [bSm, mcol.b, tmpc.b], [bSm])
                        V("dve", lambda e: e.tensor_copy(nbc[:, :], Cst[:, 128:129].to_broadcast([128, 128])), [bSm],
                          [bNb])
                        V("act", lambda e: e.copy(Cst_b, Cst[:, 0:128]), [bSm], [bSmb])
                        R(et, wm, qkw, tmpc)
                    unkeep(bR1)
                    _ck(5)
                    R(kwt, mxb, xc_b, q_b, k_b, qw, mcol)
                    dsb, hh, sq, rs, yo = G(), G(), G(), G(), G()
                    V("act", lambda e: e.copy(dsb.f(), pden[:, :]), [bden], [dsb.b])
                    unkeep(bden)
                    V("dve", lambda e: e.scalar_tensor_tensor(out=dsb.f(), in0=dsb.f(), scalar=-1.0, in1=dsb.f(),
                                                              op0=ALU.mult, op1=ALU.max), [dsb.b], [dsb.b])
                    V("dve", lambda e: e.tensor_tensor(dsb.f(), dsb.f(), emt.f(), ALU.max), [dsb.b, emt.b], [dsb.b])
                    V("dve", lambda e: e.reciprocal(dsb.f(), dsb.f()), [dsb.b], [dsb.b])
                    V("dve", lambda e: e.tensor_tensor(hh.f(), pnum[:, :], dsb.f(), ALU.mult), [bnum, dsb.b], [hh.b])
                    unkeep(bnum)
                    V("dve", lambda e: e.tensor_tensor(hh.f(), hh.f(), osig.f(), ALU.mult), [hh.b, osig.b], [hh.b])
                    V("act", lambda e: e.activation(out=sq.h(), in_=hh.f(), func=AF.Square), [hh.b], [sq.b])
                    pss, bss = bank()
                    mm(pss[:, :], onesb, sq.h(), [bCb, sq.b], [bss])
                    V("act", lambda e: e.activation(out=rs.f(), in_=pss[:, :], func=AF.Sqrt, bias=EPS, scale=1.0 / 128),
                      [bss], [rs.b])
                    V("dve", lambda e: e.reciprocal(rs.f(), rs.f()), [rs.b], [rs.b])
                    V("dve", lambda e: e.scalar_tensor_tensor(out=yo.h(), in0=hh.f(), scalar=P("nm", 0), in1=rs.f(),
                                                              op0=ALU.mult, op1=ALU.mult), [hh.b, bPr, rs.b], [yo.b])
                    em.dma("sp", y_dst(256, 384, t), yo.h(), yo.b, reads=[yo.b])
                    R(dsb, hh, sq, rs, yo, emt, osig)
                except _Cut:
                    reserved.clear()

            if t + 1 < NS:
                raw_next = proj(t + 1)
            if "rw" in STAGES:
                try:
                    NC8 = ST // 64

                    def shiftmix(r, M, mu_ap, omu_ap, eng="dve"):
                        o = G()
                        V(eng, lambda e: e.tensor_scalar(o.f(M), r.f(M, ST, 4), omu_ap, None, ALU.mult), [r.b, bPr], [o.b])
                        if eng == "dve":
                            V(eng, lambda e: e.scalar_tensor_tensor(out=o.f(M), in0=r.f(M, ST, 3), scalar=mu_ap, in1=o.f(M),
                                                                    op0=ALU.mult, op1=ALU.add), [r.b, bPr, o.b], [o.b])
                        else:
                            tm_ = G()
                            V(eng, lambda e: e.tensor_scalar(tm_.f(M), r.f(M, ST, 3), mu_ap, None, ALU.mult), [r.b, bPr],
                              [tm_.b])
                            V(eng, lambda e: e.tensor_tensor(o.f(M), o.f(M), tm_.f(M), ALU.add), [o.b, tm_.b], [o.b])
                            R(tm_)
                        R(r)
                        return o
                    r_s = shiftmix(raw["rr"], 128, P("mu3", 0), OMU(0))
                    k_s = shiftmix(raw["rk"], 128, P("mu3", 1), OMU(1), "dve")
                    v_s = shiftmix(raw["rv"], 128, P("mu3", 2), OMU(2))
                    wl_s = shiftmix(raw["rwl"], 64, P("mu2", 0, 64), OMU(3, 64), "dve")
                    al_s = shiftmix(raw["ral"], 64, P("mu2", 1, 64), OMU(4, 64))
                    gl_s = shiftmix(raw["rgl"], 128, P("mug", 0), OMU(5), "dve")
                    tw_b, al_b, sg_b = G(), G(), G()
                    V("act", lambda e: e.activation(out=tw_b.h(64), in_=wl_s.f(64), func=AF.Tanh), [wl_s.b], [tw_b.b])
                    V("act", lambda e: e.copy(al_b.h(64), al_s.f(64)), [al_s.b], [al_b.b])
                    V("act", lambda e: e.activation(out=sg_b.h(), in_=gl_s.f(), func=AF.Sigmoid), [gl_s.b], [sg_b.b])
                    R(wl_s, al_s, gl_s)
                    _ck(11)
                    sgm, a_f, g_f = G(), G(), G()
                    pw, bw = bank()
                    mm(pw[:, :], wup_b, tw_b.h(64), [bWs, tw_b.b], [bw])
                    V("act", lambda e: e.activation(out=sgm.f(), in_=pw[:, :], func=AF.Sigmoid, bias=P("w0", 0)), [bw, bPr],
                      [sgm.b])
                    pa_, ba_ = bank()
                    mm(pa_[:, :], aup_b, al_b.h(64), [bWs, al_b.b], [ba_])
                    V("act", lambda e: e.activation(out=a_f.f(), in_=pa_[:, :], func=AF.Sigmoid, bias=P("a0", 0)), [ba_, bPr],
                      [a_f.b])
                    pg, bg = bank()
                    mm(pg[:, :], gup_b, sg_b.h(), [bWs, sg_b.b], [bg])
                    V("act", lambda e: e.copy(g_f.f(), pg[:, :]), [bg], [g_f.b])
                    R(tw_b, al_b, sg_b)
                    _ck(12)
                    kkf, sq, nrm = G(), G(), G()
                    V("dve", lambda e: e.tensor_scalar(kkf.f(), k_s.f(), P("kk", 0), None, ALU.mult), [k_s.b, bPr], [kkf.b])
                    V("act", lambda e: e.activation(out=sq.h(), in_=kkf.f(), func=AF.Square), [kkf.b], [sq.b])
                    pss, bss = bank()
                    mm(pss[:, :], blkb, sq.h(), [bCb, sq.b], [bss])
                    V("act", lambda e: e.activation(out=nrm.f(), in_=pss[:, :], func=AF.Sqrt), [bss], [nrm.b])
                    V("dve", lambda e: e.tensor_scalar(nrm.f(), nrm.f(), 1e-6, None, ALU.max), [nrm.b], [nrm.b])
                    V("dve", lambda e: e.reciprocal(nrm.f(), nrm.f()), [nrm.b], [nrm.b])
                    V("dve", lambda e: e.tensor_tensor(kkf.f(), kkf.f(), nrm.f(), ALU.mult), [kkf.b, nrm.b], [kkf.b])
                    R(sq, nrm)
                    _ck(13)
                    kp, beta, csg, cx = G(), G(), G(), G()
                    V("dve", lambda e: e.tensor_scalar(kp.f(), a_f.f(), -1.0, P("ka", 0), ALU.add, ALU.mult), [a_f.b, bPr],
                      [kp.b])
                    V("dve", lambda e: e.scalar_tensor_tensor(out=kp.f(), in0=kp.f(), scalar=1.0, in1=k_s.f(), op0=ALU.add,
                                                              op1=ALU.mult), [kp.b, k_s.b], [kp.b])
                    V("dve", lambda e: e.tensor_tensor(beta.f(), kkf.f(), a_f.f(), ALU.mult), [kkf.b, a_f.b], [beta.b])
                    V("dve", lambda e: e.tensor_tensor_scan(csg.f(), C("m64"), sgm.f(), 0.0, ALU.mult, ALU.add), [sgm.b, bC],
                      [csg.b])
                    V("dve", lambda e: e.tensor_tensor(cx.f(), csg.f(), sgm.f(), ALU.subtract), [csg.b, sgm.b], [cx.b])
                    eP, eN, ePx = G(), G(), G()
                    V("act", lambda e: e.activation(out=eP.f(), in_=csg.f(), func=AF.Exp, scale=-KDEC), [csg.b], [eP.b])
                    V("act", lambda e: e.activation(out=eN.f(), in_=csg.f(), func=AF.Exp, scale=KDEC), [csg.b], [eN.b])
                    V("act", lambda e: e.activation(out=ePx.f(), in_=cx.f(), func=AF.Exp, scale=-KDEC), [cx.b], [ePx.b])
                    KR, Bc_b, Kc_b, v_b, pl = G(), G(), G(), G(), G()
                    KRv = KR.h(128, 1024).rearrange("p (c two l) -> p c two l", two=2, l=64)
                    V("dve", lambda e: e.tensor_tensor(KRv[:, :, 0, :], v3(kkf.f(), 64), v3(ePx.f(), 64), ALU.mult),
                      [kkf.b, ePx.b], [KR.b])
                    V("dve", lambda e: e.tensor_tensor(KRv[:, :, 1, :], v3(r_s.f(), 64), v3(eP.f(), 64), ALU.mult),
                      [r_s.b, eP.b], [KR.b])
                    V("dve", lambda e: e.tensor_tensor(Bc_b.h(), beta.f(), eN.f(), ALU.mult), [beta.b, eN.b], [Bc_b.b])
                    V("dve", lambda e: e.tensor_tensor(Kc_b.h(), kp.f(), eN.f(), ALU.mult), [kp.b, eN.b], [Kc_b.b])
                    V("act", lambda e: e.copy(v_b.h(), v_s.f()), [v_s.b], [v_b.b])
                    V("dve", lambda e: e.tensor_copy(pl.f(128, 8), eP.t[:, 63:512:64]), [eP.b], [pl.b])
                    R(kkf, a_f, k_s, beta, csg, cx, sgm, eP, eN, ePx)
                    _ck(14)
                    TK = [G(), G()]
                    for j2 in range(2):
                        px, bx = bank()
                        pxb = px[:, :].bitcast(BF16)
                        for cc in range(4):
                            c = 4 * j2 + cc
                            srcs = [(KR, c * 128), (Bc_b, c * 64), (Kc_b, c * 64), (v_b, c * 64)]
                            for q in range(4):
                                for hd in range(2):
                                    p0 = 64 * hd
                                    V("pe", lambda e: e.transpose(
                                        pxb[p0:p0 + 64, cc * 256 + q * 64:cc * 256 + (q + 1) * 64],
                                        srcs[q][0].h(64, 64, srcs[q][1], p0), identb[p0:p0 + 64, p0:p0 + 64]),
                                        [srcs[q][0].b, bCb], [bx])
                        if j2 % 2:
                            V("act", lambda e: e.copy(TK[j2].h(128, 1024), pxb[:, :]), [bx], [TK[j2].b])
                        else:
                            V("dve", lambda e: e.tensor_copy(TK[j2].h(128, 1024), pxb[:, :]), [bx], [TK[j2].b])
                    R(v_b)
                    _ck(15)

                    def tk(c, q, p0):
                        return TK[c // 4].h(64, 64, (c % 4) * 256 + q * 64, p0)
                    AB = [G(), G()]
                    Xa, XTa, Xb, XTb, TTm = G(), G(), G(), G(), G()
                    pA2, bA2 = bank(True)
                    for j2 in range(4):
                        pA, bA = bank()
                        for cc in range(2):
                            c = 2 * j2 + cc
                            for hd in range(2):
                                p0 = 64 * hd
                                krhs = KR.h(64, 128, c * 128, p0)
                                mm(pA[p0:p0 + 64, cc * 256:cc * 256 + 128], Bc_b.h(64, 64, c * 64, p0), krhs,
                                   [Bc_b.b, KR.b], [bA])
                                mm(pA[p0:p0 + 64, cc * 256 + 128:cc * 256 + 256], Kc_b.h(64, 64, c * 64, p0), krhs,
                                   [Kc_b.b, KR.b], [bA])
                                mm(pA2[p0:p0 + 64, c * 64:(c + 1) * 64], KR.h(64, 64, c * 128, p0),
                                   Bc_b.h(64, 64, c * 64, p0), [KR.b, Bc_b.b], [bA2])
                        abt = AB[j2 // 2]
                        V("dve", lambda e: e.tensor_tensor(
                            v3(abt.h(128, 512, (j2 % 2) * 512), 256), v3(pA[:, :], 256),
                            C("maskAB")[:, 0:256].unsqueeze(1).to_broadcast([128, 2, 256]), ALU.mult), [bA, bC],
                            [abt.b])
                    V("dve", lambda e: e.tensor_tensor(v3(Xa.h(128, 512), 64), v3(pA2[:, :], 64),
                                                      C("nmasklo")[:, 0:64].unsqueeze(1).to_broadcast([128, 8, 64]),
                                                      ALU.mult), [bA2, bC], [Xa.b])
                    unkeep(bA2)
                    for j2 in range(2):
                        V("dve", lambda e: e.tensor_scalar(
                            v3(XTa.h(128, 256, j2 * 256), 64),
                            AB[j2].h(128, 1024).rearrange("p (m w) -> p m w", w=256)[:, :, 0:64], -1.0, None,
                            ALU.mult), [AB[j2].b], [XTa.b])
                    V("dve", lambda e: e.tensor_tensor(
                        v3(TTm.h(128, 512), 64), v3(XTa.h(128, 512), 64),
                        cb[:, 384:448].unsqueeze(1).to_broadcast([128, 8, 64]), ALU.add), [XTa.b, bCb], [TTm.b])

                    def ab(c, w, p0):
                        return AB[c // 4].h(64, 64, (c % 4) * 256 + w * 64, p0)
                    _ck(16)
                    Xc, XTc, Xn, XTn = Xa, XTa, Xb, XTb
                    for lvl in range(1, 6):
                        for b4 in range(2):
                            pX, bX_ = bank()
                            for m in range(4):
                                mo_ = (b4 * 4 + m) * 64
                                for hd in range(2):
                                    p0 = 64 * hd
                                    mm(pX[p0:p0 + 64, m * 128:m * 128 + 64], XTc.h(64, 64, mo_, p0),
                                       Xc.h(64, 64, mo_, p0), [XTc.b, Xc.b], [bX_])
                                    if lvl < 5:
                                        mm(pX[p0:p0 + 64, m * 128 + 64:m * 128 + 128], Xc.h(64, 64, mo_, p0),
                                           XTc.h(64, 64, mo_, p0), [XTc.b, Xc.b], [bX_])
                            pXv = v3(pX[:, :], 128)
                            V("act", lambda e: e.copy(v3(Xn.h(128, 256, b4 * 256), 64), pXv[:, :, 0:64]), [bX_],
                              [Xn.b])
                            if lvl < 5:
                                V("dve", lambda e: e.tensor_copy(v3(XTn.h(128, 256, b4 * 256), 64), pXv[:, :, 64:128]),
                                  [bX_], [XTn.b])
                            pT, bT = bank()
                            for m in range(4):
                                mo_ = (b4 * 4 + m) * 64
                                for hd in range(2):
                                    p0 = 64 * hd
                                    mm(pT[p0:p0 + 64, m * 64:(m + 1) * 64], Xn.h(64, 64, mo_, p0),
                                       TTm.h(64, 64, mo_, p0), [Xn.b, TTm.b], [bT])
                            V("dve", lambda e: e.tensor_tensor(TTm.h(128, 256, b4 * 256), TTm.h(128, 256, b4 * 256),
                                                              pT[:, 0:256], ALU.add), [bT, TTm.b], [TTm.b])
                        Xc, XTc, Xn, XTn = Xn, XTn, Xc, XTc
                    R(Xa, XTa, Xb, XTb)
                    _ck(17)
                    AkV, WU = G(), G()
                    pK, bK = bank()
                    for c in range(NC8):
                        for hd in range(2):
                            p0 = 64 * hd
                            mm(pK[p0:p0 + 64, c * 64:(c + 1) * 64], ab(c, 2, p0), tk(c, 3, p0),
                               [AB[c // 4].b, TK[c // 4].b], [bK])
                    V("act", lambda e: e.copy(AkV.h(128, 512), pK[:, :]), [bK], [AkV.b])
                    for b4 in range(2):
                        pW, bW_ = bank()
                        for cc in range(4):
                            c = b4 * 4 + cc
                            for hd in range(2):
                                p0 = 64 * hd
                                mm(pW[p0:p0 + 64, cc * 128:cc * 128 + 64], TTm.h(64, 64, c * 64, p0), tk(c, 0, p0),
                                   [TTm.b, TK[c // 4].b], [bW_])
                                mm(pW[p0:p0 + 64, cc * 128 + 64:cc * 128 + 128], TTm.h(64, 64, c * 64, p0),
                                   AkV.h(64, 64, c * 64, p0), [TTm.b, AkV.b], [bW_])
                        V("dve", lambda e: e.tensor_tensor(v3(WU.h(128, 512, b4 * 512), 128), v3(pW[:, :], 128),
                                                          C("signm").unsqueeze(1).to_broadcast([128, 4, 128]),
                                                          ALU.mult), [bW_, bC], [WU.b])
                    R(AkV, TTm)
                    _ck(18)

                    def wu(c, w, p0):
                        return WU.h(64, 64, c * 128 + w * 64, p0)
                    G0T, Hpp, QT = G(), G(), G()
                    pG, bG_ = bank()
                    pH, bH_ = bank()
                    pQ, bQ_ = bank()
                    for c in range(NC8):
                        cs = slice(c * 64, (c + 1) * 64)
                        for hd in range(2):
                            p0 = 64 * hd
                            mm(pG[p0:p0 + 64, cs], wu(c, 0, p0), tk(c, 1, p0), [WU.b, TK[c // 4].b], [bG_])
                            mm(pH[p0:p0 + 64, cs], tk(c, 1, p0), wu(c, 1, p0), [WU.b, TK[c // 4].b], [bH_], True,
                               False)
                            mm(pH[p0:p0 + 64, cs], tk(c, 2, p0), tk(c, 3, p0), [TK[c // 4].b], [bH_], False, True)
                            mm(pQ[p0:p0 + 64, cs], wu(c, 0, p0), ab(c, 1, p0), [WU.b, AB[c // 4].b], [bQ_])
                    V("dve", lambda e: e.tensor_tensor(v3(G0T.f(), 64),
                                                      iden2[:, :].unsqueeze(1).to_broadcast([128, NC8, 64]),
                                                      v3(pG[:, :], 64), ALU.subtract), [bG_, bI2], [G0T.b])
                    V("dve", lambda e: e.tensor_tensor(v3(Hpp.f(), 64), v3(pH[:, :], 64),
                                                      pl.f(128, 8).unsqueeze(2).to_broadcast([128, NC8, 64]),
                                                      ALU.mult), [bH_, pl.b], [Hpp.b])
                    V("dve", lambda e: e.tensor_tensor(v3(QT.h(), 64), KRv[:, :, 1, :], v3(pQ[:, :], 64),
                                                      ALU.subtract), [bQ_, KR.b], [QT.b])
                    _ck(19)
                    pY, bY_ = bank(True)
                    for c in range(NC8):
                        cs = slice(c * 64, (c + 1) * 64)
                        for hd in range(2):
                            p0 = 64 * hd
                            mm(pY[p0:p0 + 64, cs], Srw_b[p0:p0 + 64, :], QT.h(64, 64, c * 64, p0), [bSrb, QT.b],
                               [bY_], True, False)
                            mm(pY[p0:p0 + 64, cs], wu(c, 1, p0), ab(c, 1, p0), [WU.b, AB[c // 4].b], [bY_], False,
                               False)
                            mm(pY[p0:p0 + 64, cs], tk(c, 3, p0), ab(c, 3, p0), [TK[c // 4].b, AB[c // 4].b], [bY_],
                               False, True)
                        pS, bS_ = bank()
                        for hd in range(2):
                            p0 = 64 * hd
                            mm(pS[p0:p0 + 64, 0:64], G0T.f(64, 64, c * 64, p0), Srw[p0:p0 + 64, :], [G0T.b, bSr],
                               [bS_])
                        V("dve", lambda e: e.scalar_tensor_tensor(out=Srw, in0=pS[:, 0:64], scalar=pl.f(128, 1, c),
                                                                  in1=Hpp.f(128, 64, c * 64), op0=ALU.mult,
                                                                  op1=ALU.add), [bS_, pl.b, Hpp.b, bSr], [bSr])
                        V("act", lambda e: e.copy(Srw_b, Srw), [bSr], [bSrb])
                    R(G0T, Hpp, QT, KR, Bc_b, Kc_b, pl, WU, *TK, *AB)
                    _ck(20)
                    y_f, y_b, sq, rs, rk_, rkr, yo = G(), G(), G(), G(), G(), G(), G()
                    V("dve", lambda e: e.tensor_copy(y_f.f(), pY[:, :]), [bY_], [y_f.b])
                    V("act", lambda e: e.copy(y_b.h(), pY[:, :]), [bY_], [y_b.b])
                    unkeep(bY_)
                    pm, bm = bank()
                    mm(pm[:, :], blkb, y_b.h(), [bCb, y_b.b], [bm])
                    V("dve", lambda e: e.scalar_tensor_tensor(out=y_f.f(), in0=pm[:, :], scalar=-1.0 / 64, in1=y_f.f(),
                                                              op0=ALU.mult, op1=ALU.add), [bm, y_f.b], [y_f.b])
                    V("act", lambda e: e.activation(out=sq.h(), in_=y_f.f(), func=AF.Square), [y_f.b], [sq.b])
                    pv_, bv_ = bank()
                    mm(pv_[:, :], blkb, sq.h(), [bCb, sq.b], [bv_])
                    V("act", lambda e: e.activation(out=rs.f(), in_=pv_[:, :], func=AF.Sqrt, bias=64e-5, scale=1.0 / 64),
                      [bv_], [rs.b])
                    V("dve", lambda e: e.reciprocal(rs.f(), rs.f()), [rs.b], [rs.b])
                    V("dve", lambda e: e.tensor_tensor(y_f.f(), y_f.f(), rs.f(), ALU.mult), [y_f.b, rs.b], [y_f.b])
                    V("dve", lambda e: e.tensor_scalar(y_f.f(), y_f.f(), P("lnw", 0), P("lnb", 0), ALU.mult, ALU.add),
                      [y_f.b, bPr], [y_f.b])
                    V("dve", lambda e: e.tensor_tensor(rk_.f(), r_s.f(), kp.f(), ALU.mult), [r_s.b, kp.b], [rk_.b])
                    V("dve", lambda e: e.tensor_scalar(rkr.h(), rk_.f(), P("rkc", 0), None, ALU.mult), [rk_.b, bPr], [rkr.b])
                    pb_, bb_ = bank()
                    mm(pb_[:, :], blkb, rkr.h(), [bCb, rkr.b], [bb_])
                    V("dve", lambda e: e.tensor_tensor(rk_.f(), pb_[:, :], v_s.f(), ALU.mult), [bb_, v_s.b], [rk_.b])
                    V("dve", lambda e: e.tensor_tensor(y_f.f(), y_f.f(), rk_.f(), ALU.add), [y_f.b, rk_.b], [y_f.b])
                    V("dve", lambda e: e.tensor_tensor(yo.h(), y_f.f(), g_f.f(), ALU.mult), [y_f.b, g_f.b], [yo.b])
                    em.dma("sp", y_dst(384, 512, t), yo.h(), yo.b, reads=[yo.b])
                    R(y_f, y_b, sq, rs, rk_, rkr, yo, r_s, v_s, kp, g_f)
                except _Cut:
                    reserved.clear()

            keep_ = set(id(g) for g in raw_next.values()) if raw_next else set()
            if len(STAGES) == 3:
                assert all(id(g) in keep_ for g in live), [g.b.name for g in live]
            R(*[g for g in live if id(g) not in keep_])
        em.wait_bufs("sp", [g.b for g in gt])
        if ctx is not None:
            em.barrier()
            em.end_phase()
            em.pes = None
    return nc


def _lay8(g):
    return np.ascontiguousarray(np.asarray(g, np.float32).reshape(8, 128).T)


def _mix_core_inputs(p, l, q):
    g = q // 2
    hs = [4 * q + i for i in range(4)]
    cols = []
    for h in hs:
        cols += list(range(64 * h, 64 * h + 64))
    for h in hs:
        cols += list(range(1024 + 64 * h, 1024 + 64 * h + 64))
    cols += list(range(2048 + 64 * g, 2048 + 64 * g + 64))
    cols += list(range(2176 + 64 * g, 2176 + 64 * g + 64))
    cols += [2304 + h for h in hs]
    cols += list(range(2320 + 128 * q, 2320 + 128 * q + 128))
    cols += list(range(2832 + 128 * q, 2832 + 128 * q + 128))
    cols += [3344 + q, 3348 + q]
    for base in (3352, 3864, 4376):
        cols += list(range(base + 128 * q, base + 128 * q + 128))
    cols += list(range(4888, 5144))
    assert len(cols) == NCOL
    w_sel = np.ascontiguousarray(p["w_in"][l][:, cols])
    pro, prtot = _prm_layout()
    prm = np.zeros((128, prtot), np.float32)

    def put(n, a, j=0):
        a = np.asarray(a, np.float32)
        if a.ndim == 1:
            a = a[:, None]
        o, w = pro[n]
        prm[0:a.shape[0], o + j:o + j + a.shape[1]] = a
    cw, cbv = p["ssd_conv_w"][l], p["ssd_conv_b"][l]
    for i in range(2):
        ch = list(range(64 * hs[2 * i], 64 * hs[2 * i] + 64)) + list(range(64 * hs[2 * i + 1], 64 * hs[2 * i + 1] + 64))
        put("cwx", cw[:, ch].T, 4 * i)
        put("cbx", cbv[ch], i)
        put("Dcol", np.repeat(p["ssd_d"][l][[hs[2 * i], hs[2 * i + 1]]], 64), i)
    chB = list(range(1024 + 64 * g, 1024 + 64 * g + 64))
    chC = list(range(1152 + 64 * g, 1152 + 64 * g + 64))
    put("cwB", cw[:, chB].T)
    put("cbB", cbv[chB])
    put("cwC", cw[:, chC].T)
    put("cbC", cbv[chC])
    put("dtb", p["ssd_dt_bias"][l][hs])
    put("alog", p["ssd_a_log"][l][hs])
    sl = slice(128 * q, 128 * q + 128)
    put("cwm", p["mlstm_conv_w"][l][:, sl].T)
    put("cbm", p["mlstm_conv_b"][l][sl])
    put("bi", p["mlstm_gate_bias"][l][q:q + 1])
    put("bf", p["mlstm_gate_bias"][l][4 + q:5 + q])
    put("nm", p["mlstm_norm"][l][sl])
    mu = p["rwkv_shift_mu"][l]
    for j, base in enumerate((0, 512, 1024)):
        put("mu3", mu[base + 128 * q:base + 128 * q + 128], j)
    put("mu2", mu[1536:1600], 0)
    put("mu2", mu[1600:1664], 1)
    put("mug", mu[1664:1792])
    put("w0", p["rwkv_w0"][l][sl])
    put("a0", p["rwkv_a0"][l][sl])
    put("kk", p["rwkv_k_k"][l][sl])
    put("ka", p["rwkv_k_a"][l][sl])
    put("rkc", p["rwkv_r_k"][l].reshape(-1)[sl])
    put("lnw", p["rwkv_ln_w"][l][sl])
    put("lnb", p["rwkv_ln_b"][l][sl])
    wsm = np.zeros((128, 768), np.float32)
    wsm[:, 0:128] = p["mlstm_wq"][l][q]
    wsm[:, 128:256] = p["mlstm_wk"][l][q]
    wsm[:, 256:384] = p["mlstm_wv"][l][q]
    wsm[0:64, 384:512] = p["rwkv_w_up"][l][:, sl]
    wsm[0:64, 512:640] = p["rwkv_a_up"][l][:, sl]
    wsm[:, 640:768] = p["rwkv_g_up"][l][:, sl]
    return {"w_sel": w_sel, "g_mix": _lay8(p["mix_norm"][l]), "prm": prm, "wsm": wsm}


def build_all(T, NTOK, TTK=256):
    nc = bass.Bass("TRN2", target_bir_lowering=False)
    D = D_MODEL
    L = DEPTH
    cfo, cftot = _cf_layout()
    pro, prtot = _prm_layout()

    def din(name, shape, dt=F32):
        return nc.dram_tensor(name, list(shape), dt, kind="ExternalInput").ap()
    x_in = din("x_in", [D, NTOK])
    wgu1 = din("wgu1", [L, D, 2 * D_FF])
    wd1 = din("wd1", [L, D_FF, D])
    gn1 = din("gn1", [L, 128, 8])
    wgu2 = din("wgu2", [L, D, 2 * D_FF])
    wd2 = din("wd2", [L, D_FF, D])
    gn2 = din("gn2", [L, 128, 8])
    w_o = din("w_o", [L, 2048, D])
    g_y = din("g_y", [L, 128, 8])
    g_f = din("g_f", [128, 8])
    w_sel = din("w_sel", [L, D, NCOL])
    g_mix = din("g_mix", [L, 128, 8])
    prm = din("prm", [L, 128, prtot])
    wsm = din("wsm", [L, 128, 768])
    cf = din("cf", [128, cftot])
    o_fin = nc.dram_tensor("o_fin", [D, NTOK], F32, kind="ExternalOutput").ap()
    x_res = nc.dram_tensor("x_res", [D, NTOK], F32).ap()
    h_loc = nc.dram_tensor("h_loc", [D, NTOK], BF16).ap()
    h_all = nc.dram_tensor("h_all", [8 * 512, NTOK], BF16).ap()
    y_loc = nc.dram_tensor("y_loc", [4 * 512, NTOK], BF16).ap()
    y_all = nc.dram_tensor("y_all", [16 * 512, NTOK], BF16).ap()
    y_mine = nc.dram_tensor("y_mine", [2048, NTOK], BF16).ap()
    groups = [[0, 1, 2, 3], [4, 5, 6, 7]]
    SPQ = NTOK // ST

    es = ExitStack()
    with es:
        em = Em(nc, es)
        banks = [em.psum("pb%d" % i, [128, 512], F32) for i in range(8)]
        bbanks = [Buf("pb%d" % i, True) for i in range(8)]
        cc_sem = es.enter_context(nc.semaphore("cc_sem"))
        cc = [cc_sem, 0]
        em.extra.append(cc)
        base = {"nc": nc, "em": em, "banks": banks, "bbanks": bbanks}

        def gather(src, dst, nblk):
            em.barrier()
            for j in range(nblk):
                nc.gpsimd.collective_compute("AllGather", ALU.bypass, replica_groups=groups,
                                             ins=[src[j * 128:(j + 1) * 128, :]],
                                             outs=[dst[j * 512:(j + 1) * 512, :]]).then_inc(cc_sem, 1)
                cc[1] += 1
            em.extra[0] = (cc_sem, cc[1])
            em.barrier()

        sqv = nc.sync.snap(nc.sync.partition_id() % 4, min_val=0, max_val=3)
        bYm = Buf("y_mine")

        def select_y():
            src = y_all.rearrange("(s m) n -> s m n", s=4)[bass.ds(sqv, 1), :, :]
            em.dma("sp", y_mine.rearrange("(o m) n -> o m n", o=1), src, bYm, writes=[bYm])

        def y_load(em_, yt, bY, i):
            cs_ = slice(i * TTK, (i + 1) * TTK)
            for kc, dst in ((0, yt[:, 0:8:2, :]), (1, yt[:, 1:8:2, :]), (2, yt[:, 8:12, :]), (3, yt[:, 12:16, :])):
                em_.dma("sp", dst, y_mine[kc * 512:(kc + 1) * 512, cs_].rearrange("(r p) n -> p r n", p=128), bY,
                        reads=[bYm], writes=[bY])

        def y_dst(r0, r1, t):
            sq_, tt = t // SPQ, t % SPQ
            return y_loc[sq_ * 512 + r0:sq_ * 512 + r1, tt * ST:(tt + 1) * ST]

        def h_src(t):
            r, tt = t // SPQ, t % SPQ
            return h_all.rearrange("(c r p) n -> p c r n", c=8, r=4)[:, :, r, tt * ST:(tt + 1) * ST]

        em.extra[0] = (cc_sem, 0)
        em.prefix = "t0_"
        build_tok(NTOK, TTK, False, 1, True, False,
                  dict(base, x_in=x_in, x_out=x_res, wgu=[wgu1[0]], wd=[wd1[0]], gn=[gn1[0]], h_out=h_loc))
        for l in range(L):
            gather(h_loc, h_all, 8)
            em.prefix = "m%d_" % l
            build_mix(T, dict(base, w_sel=w_sel[l], g_mix=g_mix[l], cf=cf, prm=prm[l], wsm=wsm[l], y_out=y_loc,
                              h_src=h_src, y_dst=y_dst))
            gather(y_loc, y_all, 16)
            select_y()
            em.prefix = "t%d_" % (l + 1)
            if l < L - 1:
                build_tok(NTOK, TTK, True, 2, True, False,
                          dict(base, x_in=x_res, x_out=x_res, y_load=y_load, w_o=w_o[l], g_y=g_y[l],
                               wgu=[wgu2[l], wgu1[l + 1]], wd=[wd2[l], wd1[l + 1]], gn=[gn2[l], gn1[l + 1]],
                               h_out=h_loc))
            else:
                build_tok(NTOK, TTK, True, 1, False, True,
                          dict(base, x_in=x_res, x_out=x_res, y_load=y_load, w_o=w_o[l], g_y=g_y[l],
                               wgu=[wgu2[l]], wd=[wd2[l]], gn=[gn2[l]], g_f=g_f, o_fin=o_fin))
        print("[build_all] instructions:", em.nins, "dma sems:", em.nsem)
    return nc


_PROGS = {}


def _prog(key, fn):
    if key not in _PROGS:
        _PROGS[key] = fn()
    return _PROGS[key]


def _run(nc, in_maps):
    return run_bass_kernel_spmd(nc, in_maps, core_ids=list(range(8))).results


def kernel(**inp):
    p = {k: np.asarray(v) for k, v in inp.items()}
    x = p["x"].astype(np.float32, copy=False)
    B, T, D = x.shape
    NTOK = B * T // 8
    xflat = x.reshape(B * T, D)
    nc = _prog(("all", T, NTOK), lambda: build_all(T, NTOK))
    f32 = lambda a: np.ascontiguousarray(a, dtype=np.float32)
    lay = lambda a: np.ascontiguousarray(np.stack([_lay8(a[l]) for l in range(DEPTH)]))
    shared = {"wgu1": f32(p["ffn1_w_gate_up"]), "wd1": f32(p["ffn1_w_down"]), "gn1": lay(p["ffn1_norm"]),
              "wgu2": f32(p["ffn2_w_gate_up"]), "wd2": f32(p["ffn2_w_down"]), "gn2": lay(p["ffn2_norm"]),
              "w_o": f32(p["w_out"]), "g_y": lay(p["ssd_norm"]), "g_f": _lay8(p["final_norm"]),
              "g_mix": lay(p["mix_norm"]), "cf": mix_consts()}
    perq = []
    for q in range(4):
        ds_ = [_mix_core_inputs(p, l, q) for l in range(DEPTH)]
        perq.append({k: np.ascontiguousarray(np.stack([d[k] for d in ds_])) for k in ("w_sel", "prm", "wsm")})
    im = []
    for c in range(8):
        d = dict(shared)
        d.update(perq[c % 4])
        d["x_in"] = np.ascontiguousarray(xflat[c * NTOK:(c + 1) * NTOK].T)
        im.append(d)
    res = _run(nc, im)
    out = np.concatenate([np.asarray(r["o_fin"]).T for r in res], axis=0).reshape(B, T, D)
    return np.ascontiguousarray(out, dtype=np.float32)
```
